# Optimizing a Trainium2 kernel written in Bass

```python
import jax, jax.numpy as jnp
from jax import lax
import numpy as np

D_MODEL = 2048
BATCH = 4
SEQ = 2048
DEPTH = 2

GRID_W = 64
CTX_LEN = 256
N_HEADS = 16
N_KV_HEADS = 4
GROUP = N_HEADS // N_KV_HEADS
HEAD_DIM = 128
WINDOW = 128
BLOCK = 128
ROPE_THETA = 10000.0
CONV_WIDTH = 3
D_CONV = D_MODEL
D_FF = 5632
N_EXPERTS = 8
TOP_K = 2
D_FF_EXPERT = 7168
RMS_EPS = 1e-6
NEG_INF = -1e30
N_MOD = 6
N_DENSE = (DEPTH + 1) // 2
N_MOE = DEPTH // 2
Q_W = N_HEADS * HEAD_DIM
KV_W = N_KV_HEADS * HEAD_DIM
SPLIT_POINTS = (Q_W, Q_W + KV_W, Q_W + 2 * KV_W, Q_W + 2 * KV_W + D_CONV, Q_W + 2 * KV_W + 2 * D_CONV, Q_W + 2 * KV_W + 3 * D_CONV, Q_W + 2 * KV_W + 3 * D_CONV + D_MODEL)
P_WIDTH = Q_W + 2 * KV_W + 3 * D_CONV + 2 * D_MODEL

kernel_name = "hybrid_gated_swa_shortconv_moe_dit"


def rmsnorm(t, g):
    tf = t.astype(jnp.float32)
    tf = tf * lax.rsqrt(jnp.mean(tf * tf, axis=-1, keepdims=True) + RMS_EPS)
    return tf.astype(t.dtype) * g


def rope_1d(t, pos):
    half = t.shape[-1] // 2
    inv_freq = jnp.power(ROPE_THETA, -jnp.arange(half, dtype=jnp.float32) / half)
    ang = pos.astype(jnp.float32)[:, None] * inv_freq[None, :]
    cos = jnp.cos(ang)[:, None, :]
    sin = jnp.sin(ang)[:, None, :]
    t1 = t[..., :half].astype(jnp.float32)
    t2 = t[..., half:].astype(jnp.float32)
    return jnp.concatenate([t1 * cos - t2 * sin, t2 * cos + t1 * sin], axis=-1).astype(t.dtype)


def rope_2d(t, rows, cols):
    half = t.shape[-1] // 2
    return jnp.concatenate([rope_1d(t[..., :half], rows), rope_1d(t[..., half:], cols)], axis=-1)


def latent_attention(q, k, v, kc, vc, sink):
    b, s = q.shape[0], q.shape[1]
    nb = s // BLOCK
    scale = HEAD_DIM ** -0.5
    pad = ((0, 0), (BLOCK, BLOCK), (0, 0), (0, 0))
    kp = jnp.pad(k, pad).reshape(b, nb + 2, BLOCK, N_KV_HEADS, HEAD_DIM)
    vp = jnp.pad(v, pad).reshape(b, nb + 2, BLOCK, N_KV_HEADS, HEAD_DIM)
    kband = jnp.concatenate([kp[:, :-2], kp[:, 1:-1], kp[:, 2:]], axis=2)
    vband = jnp.concatenate([vp[:, :-2], vp[:, 1:-1], vp[:, 2:]], axis=2)
    qb = q.reshape(b, nb, BLOCK, N_KV_HEADS, GROUP, HEAD_DIM)
    s_loc = jnp.einsum('bnqhgd,bnkhd->bhgnqk', qb, kband).astype(jnp.float32) * scale
    blk = jnp.arange(nb)[:, None, None] * BLOCK
    qpos = blk + jnp.arange(BLOCK)[None, :, None]
    kpos = blk - BLOCK + jnp.arange(3 * BLOCK)[None, None, :]
    valid = (jnp.abs(qpos - kpos) <= WINDOW) & (kpos >= 0) & (kpos < s)
    s_loc = jnp.where(valid, s_loc, NEG_INF)
    s_ctx = jnp.einsum('bnqhgd,bchd->bhgnqc', qb, kc).astype(jnp.float32) * scale
    sink_col = jnp.broadcast_to(sink.astype(jnp.float32).reshape(N_KV_HEADS, GROUP)[None, :, :, None, None, None], s_loc.shape[:-1] + (1,))
    probs = jax.nn.softmax(jnp.concatenate([s_loc, s_ctx, sink_col], axis=-1), axis=-1).astype(v.dtype)
    n_ctx = kc.shape[1]
    p_loc = probs[..., :3 * BLOCK]
    p_ctx = probs[..., 3 * BLOCK:3 * BLOCK + n_ctx]
    o = jnp.einsum('bhgnqk,bnkhd->bnqhgd', p_loc, vband) + jnp.einsum('bhgnqc,bchd->bnqhgd', p_ctx, vc)
    return o.reshape(b, s, N_HEADS * HEAD_DIM)


def context_attention(qc, kc, vc, sink):
    b, n = qc.shape[0], qc.shape[1]
    scale = HEAD_DIM ** -0.5
    qg = qc.reshape(b, n, N_KV_HEADS, GROUP, HEAD_DIM)
    sc = jnp.einsum('bqhgd,bkhd->bhgqk', qg, kc).astype(jnp.float32) * scale
    sink_col = jnp.broadcast_to(sink.astype(jnp.float32).reshape(N_KV_HEADS, GROUP)[None, :, :, None, None], sc.shape[:-1] + (1,))
    probs = jax.nn.softmax(jnp.concatenate([sc, sink_col], axis=-1), axis=-1).astype(vc.dtype)
    o = jnp.einsum('bhgqk,bkhd->bqhgd', probs[..., :n], vc)
    return o.reshape(b, n, N_HEADS * HEAD_DIM)


def short_conv(u, gate_b, gate_c, w):
    z = gate_c * u
    n = z.shape[1]
    zp = jnp.pad(z, ((0, 0), (CONV_WIDTH // 2, CONV_WIDTH // 2), (0, 0)))
    y = zp[:, 0:n] * w[0]
    for i in range(1, CONV_WIDTH):
        y = y + zp[:, i:i + n] * w[i]
    return gate_b * y


def merge_branches(y_attn, y_conv, g_attn, g_conv, w_oa, w_oc, w_o):
    return (jax.nn.sigmoid(g_attn) * (y_attn @ w_oa) + jax.nn.sigmoid(g_conv) * (y_conv @ w_oc)) @ w_o


def swiglu(t, w1, w3, w2):
    return (jax.nn.silu(t @ w1) * (t @ w3)) @ w2


def moe_swiglu(t, w_router, w1, w3, w2):
    shp = t.shape
    tf = t.reshape(-1, shp[-1])
    logits = (tf @ w_router).astype(jnp.float32)
    top_vals, top_idx = lax.top_k(logits, TOP_K)
    top_p = jax.nn.softmax(top_vals, axis=-1)
    combine = jnp.sum(jax.nn.one_hot(top_idx, N_EXPERTS, dtype=jnp.float32) * top_p[..., None], axis=1).astype(t.dtype)
    out = jnp.zeros_like(tf)
    for e in range(N_EXPERTS):
        out = out + combine[:, e:e + 1] * swiglu(tf, w1[e], w3[e], w2[e])
    return out.reshape(shp)


def setup_inputs(seed: int = 0) -> dict:
    key = jax.random.key(seed)
    ks = jax.random.split(key, 24)
    f32 = jnp.float32

    def dense(k, shape, fan_in, gain=1.0):
        return jax.random.normal(k, shape, f32) * (gain * fan_in ** -0.5)

    return {
        "x": jax.random.normal(ks[0], (BATCH, SEQ, D_MODEL), f32),
        "c": jax.random.normal(ks[1], (BATCH, D_MODEL), f32),
        "ctx": jax.random.normal(ks[2], (BATCH, CTX_LEN, D_MODEL), f32),
        "c_ctx": jax.random.normal(ks[3], (D_MODEL,), f32),
        "w_mod": dense(ks[4], (DEPTH, D_MODEL, N_MOD * D_MODEL), D_MODEL, 0.5),
        "b_mod": 0.02 * jax.random.normal(ks[5], (DEPTH, N_MOD * D_MODEL), f32),
        "norm1": 1.0 + 0.02 * jax.random.normal(ks[6], (DEPTH, D_MODEL), f32),
        "w_in": dense(ks[7], (DEPTH, D_MODEL, P_WIDTH), D_MODEL),
        "sink": 0.5 * jax.random.normal(ks[8], (DEPTH, N_HEADS), f32),
        "conv_w": dense(ks[9], (DEPTH, CONV_WIDTH, D_CONV), CONV_WIDTH),
        "w_o_attn": dense(ks[10], (DEPTH, Q_W, D_MODEL), Q_W),
        "w_o_conv": dense(ks[11], (DEPTH, D_CONV, D_MODEL), D_CONV),
        "w_out": dense(ks[12], (DEPTH, D_MODEL, D_MODEL), D_MODEL),
        "norm2": 1.0 + 0.02 * jax.random.normal(ks[13], (DEPTH, D_MODEL), f32),
        "ffn_w1": dense(ks[14], (N_DENSE, D_MODEL, D_FF), D_MODEL),
        "ffn_w3": dense(ks[15], (N_DENSE, D_MODEL, D_FF), D_MODEL),
        "ffn_w2": dense(ks[16], (N_DENSE, D_FF, D_MODEL), D_FF),
        "router": dense(ks[17], (N_MOE, D_MODEL, N_EXPERTS), D_MODEL),
        "moe_w1": dense(ks[18], (N_MOE, N_EXPERTS, D_MODEL, D_FF_EXPERT), D_MODEL),
        "moe_w3": dense(ks[19], (N_MOE, N_EXPERTS, D_MODEL, D_FF_EXPERT), D_MODEL),
        "moe_w2": dense(ks[20], (N_MOE, N_EXPERTS, D_FF_EXPERT, D_MODEL), D_FF_EXPERT),
        "norm_f": 1.0 + 0.02 * jax.random.normal(ks[21], (D_MODEL,), f32),
    }


def reference(x, c, ctx, c_ctx, w_mod, b_mod, norm1, w_in, sink, conv_w, w_o_attn, w_o_conv, w_out, norm2, ffn_w1, ffn_w3, ffn_w2, router, moe_w1, moe_w3, moe_w2, norm_f):
    b, s, _ = x.shape
    n_ctx = ctx.shape[1]
    n_rows = s // GRID_W
    rows = jnp.broadcast_to(jnp.arange(n_rows)[:, None], (n_rows, GRID_W)).reshape(s)
    cols = jnp.broadcast_to(jnp.arange(GRID_W)[None, :], (n_rows, GRID_W)).reshape(s)
    c_act = jax.nn.silu(c)
    cc_act = jax.nn.silu(c_ctx)
    xc = ctx
    for layer in range(DEPTH):
        ctx_out = layer < DEPTH - 1
        mod = c_act @ w_mod[layer] + b_mod[layer]
        mod_c = cc_act @ w_mod[layer] + b_mod[layer]
        sh1, sc1, g1, sh2, sc2, g2 = jnp.split(mod, N_MOD, axis=-1)
        csh1, csc1, cg1, csh2, csc2, cg2 = jnp.split(mod_c, N_MOD, axis=-1)

        h = rmsnorm(x, norm1[layer]) * (1.0 + sc1[:, None]) + sh1[:, None]
        hc = rmsnorm(xc, norm1[layer]) * (1.0 + csc1) + csh1
        q, k, v, u, gate_b, gate_c, ga, gc = jnp.split(h @ w_in[layer], list(SPLIT_POINTS), axis=-1)
        q = rope_2d(q.reshape(b, s, N_HEADS, HEAD_DIM), rows, cols)
        k = rope_2d(k.reshape(b, s, N_KV_HEADS, HEAD_DIM), rows, cols)
        v = v.reshape(b, s, N_KV_HEADS, HEAD_DIM)
        if ctx_out:
            qc, kc, vc, uc, gate_bc, gate_cc, gac, gcc = jnp.split(hc @ w_in[layer], list(SPLIT_POINTS), axis=-1)
        else:
            kc, vc = jnp.split(hc @ w_in[layer][:, Q_W:Q_W + 2 * KV_W], 2, axis=-1)
        kc = kc.reshape(b, n_ctx, N_KV_HEADS, HEAD_DIM)
        vc = vc.reshape(b, n_ctx, N_KV_HEADS, HEAD_DIM)

        y_attn = latent_attention(q, k, v, kc, vc, sink[layer])
        y_conv = short_conv(u, gate_b, gate_c, conv_w[layer])
        x = x + g1[:, None] * merge_branches(y_attn, y_conv, ga, gc, w_o_attn[layer], w_o_conv[layer], w_out[layer])
        if ctx_out:
            yc_attn = context_attention(qc.reshape(b, n_ctx, N_HEADS, HEAD_DIM), kc, vc, sink[layer])
            yc_conv = short_conv(uc, gate_bc, gate_cc, conv_w[layer])
            xc = xc + cg1 * merge_branches(yc_attn, yc_conv, gac, gcc, w_o_attn[layer], w_o_conv[layer], w_out[layer])

        h = rmsnorm(x, norm2[layer]) * (1.0 + sc2[:, None]) + sh2[:, None]
        if layer % 2 == 0:
            i = layer // 2
            x = x + g2[:, None] * swiglu(h, ffn_w1[i], ffn_w3[i], ffn_w2[i])
            if ctx_out:
                hc = rmsnorm(xc, norm2[layer]) * (1.0 + csc2) + csh2
                xc = xc + cg2 * swiglu(hc, ffn_w1[i], ffn_w3[i], ffn_w2[i])
        else:
            i = layer // 2
            x = x + g2[:, None] * moe_swiglu(h, router[i], moe_w1[i], moe_w3[i], moe_w2[i])
            if ctx_out:
                hc = rmsnorm(xc, norm2[layer]) * (1.0 + csc2) + csh2
                xc = xc + cg2 * moe_swiglu(hc, router[i], moe_w1[i], moe_w3[i], moe_w2[i])
    return rmsnorm(x, norm_f)
```

```python
import types
import numpy as np
from contextlib import ExitStack
import concourse.bass as bass
import concourse.mybir as mybir
from concourse.bass_utils import run_bass_kernel_spmd

F32 = mybir.dt.float32
BF16 = mybir.dt.bfloat16
AF = mybir.ActivationFunctionType
ALU = mybir.AluOpType
ENGS = ["tensor", "vector", "scalar", "gpsimd", "sync"]

D = 2048
NJ = 16
SEQ = 2048
CTX = 256
NH = 16
NKV = 4
HD = 128
PW = 13312
DFF = 5632
NE = 8
DFE = 7168
EPS = 1e-6
OFF_K, OFF_V, OFF_U, OFF_GB, OFF_GC, OFF_GA, OFF_GCV = 2048, 2560, 3072, 5120, 7168, 9216, 11264
XS_T = 1536
SCALE = HD ** -0.5


def _freeze(fn):
    if fn is None or fn.__closure__ is None:
        return fn
    cells = []
    for c in fn.__closure__:
        try:
            cells.append(types.CellType(c.cell_contents))
        except ValueError:
            cells.append(c)
    return types.FunctionType(fn.__code__, fn.__globals__, fn.__name__, fn.__defaults__, tuple(cells))


class Prog:
    def __init__(self, nc):
        self.nc = nc
        self.ops = {e: [] for e in ENGS}
        self.cnt = {e: 0 for e in ENGS}
        self.waited = {}
        self.semkeys = list(ENGS)

    def new_sem(self, key):
        assert key not in self.cnt
        self.cnt[key] = 0
        self.semkeys.append(key)
        return key

    def _waits(self, eng, deps):
        best = {}
        for d in deps:
            if d is None:
                continue
            k, v = d
            if v > best.get(k, 0):
                best[k] = v
        waits = []
        for k, v in best.items():
            if self.waited.get((eng, k), 0) >= v:
                continue
            self.waited[(eng, k)] = v
            waits.append((k, v))
        return waits

    def op(self, eng, fn, deps=(), mark=True):
        waits = self._waits(eng, deps)
        tok = None
        inc = None
        if mark:
            self.cnt[eng] += 1
            tok = (eng, self.cnt[eng])
            inc = (eng, 1)
        self.ops[eng].append((_freeze(fn), waits, inc))
        return tok

    def dma(self, eng, fn, semkey, deps=()):
        waits = self._waits(eng, deps)
        self.cnt[semkey] += 16
        tok = (semkey, self.cnt[semkey])
        self.ops[eng].append((_freeze(fn), waits, (semkey, 16)))
        return tok

    def wait_only(self, eng, deps):
        waits = self._waits(eng, deps)
        if waits:
            self.ops[eng].append((None, waits, None))

    def build(self, st):
        nc = self.nc
        sems = {}
        for k in self.semkeys:
            sems[k] = st.enter_context(nc.semaphore("s_" + str(k)))
        block = st.enter_context(nc.Block())

        def replay(engname):
            def f(eng):
                for fn, waits, inc in self.ops[engname]:
                    for k, v in waits:
                        eng.wait_ge(sems[k], v)
                    if fn is None:
                        continue
                    ins = fn(eng)
                    if inc is not None:
                        ins.then_inc(sems[inc[0]], inc[1])
            return f

        block.tensor(replay("tensor"))
        block.vector(replay("vector"))
        block.scalar(replay("scalar"))
        block.gpsimd(replay("gpsimd"))
        block.sync(replay("sync"))


class Rot:
    def __init__(self, bufs):
        self.bufs = bufs
        self.free = [[] for _ in bufs]
        self.i = -1

    def next(self):
        self.i = (self.i + 1) % len(self.bufs)
        deps = self.free[self.i]
        self.free[self.i] = []
        return self.i, self.bufs[self.i], deps

    def release(self, i, toks):
        self.free[i] = list(self.free[i]) + [t for t in toks if t is not None]


class K:
    pass


def build_program(n_layers=2, dump=None, small=False):
    nc = bass.Bass("TRN2", target_bir_lowering=False)
    P = Prog(nc)
    k = K()
    k.nc, k.P = nc, P

    def din(name, shape, dt=F32):
        return nc.dram_tensor(name, list(shape), dt, kind="ExternalInput").ap()

    xin = din("xin", [XS_T, D])
    cvec = din("cvec", [128, NJ, 2])
    wmod = din("w_mod", [2, D, 6 * D])
    bmodT = din("bmodT", [128, 2, 96])
    n1T = din("n1T", [128, 2, NJ])
    n2T = din("n2T", [128, 2, NJ])
    nfT = din("nfT", [128, NJ])
    w_in = din("w_in", [2, D, PW])
    w_oa = din("w_o_attn", [2, D, D])
    w_oc = din("w_o_conv", [2, D, D])
    w_o = din("w_out", [2, D, D])
    sinkB = din("sinkB", [128, 2, NH])
    convT = din("convT", [128, 2, 3, NJ])
    fw1 = fw3 = fw2 = mw1 = mw3 = mw2 = None
    if small is not True:
        fw1 = din("ffn_w1", [1, D, DFF])
        fw3 = din("ffn_w3", [1, D, DFF])
        fw2 = din("ffn_w2", [1, DFF, D])
    if not small:
        mw1 = din("moe_w1", [1, NE, D, DFE])
        mw3 = din("moe_w3", [1, NE, D, DFE])
        mw2 = din("moe_w2", [1, NE, DFE, D])
    routerT = din("routerT", [128, NJ, NE])
    cosT = din("cosT", [128, 1280])
    sinT = din("sinT", [128, 1280])
    cmat = din("cmat", [128, 4, 128])
    out = nc.dram_tensor("out", [1024, D], F32, kind="ExternalOutput").ap()
    dbg = None
    if dump is not None:
        dbg = nc.dram_tensor("dbg", [128, dump[1]], F32, kind="ExternalOutput").ap()
    XS = nc.dram_tensor("xs_scr", [D, XS_T], F32).ap()
    MS = nc.dram_tensor("ms_scr", [D, 1408], BF16).ap()
    XSv = XS.rearrange("(j p) t -> p j t", p=128)
    MSv = MS.rearrange("(j p) t -> p j t", p=128)

    st = ExitStack()
    with st:
        RAWB = 211000
        raw = st.enter_context(nc.sbuf_tensor("raw", [128, RAWB // 2], BF16))
        cur = [0]

        def carve(nbytes):
            o = cur[0]
            cur[0] += (nbytes + 63) // 64 * 64
            assert cur[0] <= RAWB, cur[0]
            return o

        def view(off, dt, n, pat=None, **kw):
            nb = n * (4 if dt == F32 else 2)
            v = raw[:, off // 2: off // 2 + nb // 2]
            if dt == F32:
                v = v.bitcast(F32)
            if pat:
                v = v.rearrange(pat, **kw)
            return v

        o_c = carve(15 * 1024)
        cc = [o_c]

        def cview(dt, n, pat=None, **kw):
            nb = n * (4 if dt == F32 else 2)
            o = cc[0]
            cc[0] += (nb + 63) // 64 * 64
            assert cc[0] <= o_c + 15 * 1024, cc[0] - o_c
            return view(o, dt, n, pat, **kw)

        ident_f = cview(F32, 128)
        cm_bf = cview(BF16, 512, "p (a b) -> p a b", a=4)
        ident_b, perm_b, mask1_b, mask2_b = cm_bf[:, 0, :], cm_bf[:, 1, :], cm_bf[:, 2, :], cm_bf[:, 3, :]
        ones_b = cview(BF16, 128)
        ones_f = cview(F32, 128)
        zeros_f = cview(F32, 128)
        cos_b = cview(BF16, 1280)
        sin_b = cview(BF16, 1280)
        modT = cview(F32, 2 * 96 * 2, "p (l m c) -> p l m c", l=2, m=96)
        bmod_s = cview(F32, 2 * 96, "p (l m) -> p l m", l=2)
        n1_s = cview(F32, 32, "p (l j) -> p l j", l=2)
        n2_s = cview(F32, 32, "p (l j) -> p l j", l=2)
        nf_s = cview(F32, 16)
        sink_s = cview(F32, 32, "p (l h) -> p l h", l=2)
        conv_s = cview(F32, 96, "p (l w j) -> p l w j", l=2, w=3)
        cv_s = cview(F32, 32, "p (j c) -> p j c", c=2)
        cs_b = cview(BF16, 32, "p (j c) -> p j c", c=2)
        A1 = cview(F32, 64, "p (l j c) -> p l j c", l=2, c=2)
        A2 = cview(F32, 64, "p (l j c) -> p l j c", l=2, c=2)
        eps_t = cview(F32, 1)
        rt_s = cview(F32, NJ * NE, "p (j e) -> p j e", e=NE)
        rtA = cview(F32, NJ * NE, "p (j e) -> p j e", e=NE)
        bias_sb = cview(F32, NE)
        sinkfull = cview(F32, 512)
        rT_all = cview(F32, 8)

        NWS = 3
        o_w = [carve(16384) for _ in range(NWS)]
        o_h = carve(48 * 1024)
        BIGB = 94208
        o_big = carve(BIGB)
        o_tmp = o_big + BIGB - 10240
        tc_ = [o_tmp]

        def tview(dt, n, pat=None, **kw):
            nb = n * (4 if dt == F32 else 2)
            o = tc_[0]
            tc_[0] += (nb + 63) // 64 * 64
            assert tc_[0] <= o_big + BIGB, (tc_[0],)
            return view(o, dt, n, pat, **kw)

        sq_bufs = Rot([tview(BF16, 512) for _ in range(2)])
        tmpf_bufs = Rot([tview(F32, 512) for _ in range(2)])
        rstd_t = tview(F32, 512)
        rt_t = tview(F32, 512)

        wslots = [view(o, BF16, 8192) for o in o_w]
        banks = [st.enter_context(nc.psum_tensor(f"ps{i}", [128, 512], F32)) for i in range(8)]
        PS = Rot([b[:, :] for b in banks])

        for i in range(NWS):
            P.new_sem(f"w{i}")
        for nm in ["c0", "c1", "xt0", "xt1", "st0", "st1", "xo0", "xo1", "xn0", "xn1", "mi0", "mi1", "mo0", "mo1",
                   "big", "fin0", "fin1", "fin2", "fin3", "dbg"]:
            P.new_sem(nm)

        class WS:
            def __init__(s):
                s.req = []
                s.issued = 0
                s.tok = {}
                s.rel = {}
                s.nget = 0

            def plan(s, ap, kind):
                s.req.append((ap, kind))

            def _issue(s, q):
                ap, kind = s.req[q]
                sl = q % NWS
                deps = s.rel.get(q - NWS, [])
                if kind == "col":
                    dst = wslots[sl].rearrange("p (k n) -> p k n", k=16)
                    src = ap.rearrange("(k p) n -> p k n", p=128)
                else:
                    dst = wslots[sl].rearrange("p (c n) -> p c n", c=4)
                    src = ap.rearrange("(c p) n -> p c n", p=128)
                s.tok[q] = P.dma("gpsimd", (lambda e, dst=dst, src=src: e.dma_start(out=dst, in_=src)), f"w{sl}", deps=deps)

            def get(s):
                r = s.nget
                s.nget += 1
                while s.issued < len(s.req) and s.issued <= r + NWS - 1 and (s.issued < NWS or (s.issued - NWS) in s.rel):
                    s._issue(s.issued)
                    s.issued += 1
                assert r in s.tok, (r, s.issued)
                kind = s.req[r][1]
                sl = r % NWS
                if kind == "col":
                    t = wslots[sl].rearrange("p (k n) -> p k n", k=16)
                else:
                    t = wslots[sl].rearrange("p (c n) -> p c n", c=4)
                return r, t, s.tok[r]

            def release(s, r, toks):
                s.rel[r] = [t for t in toks if t is not None]
                while s.issued < len(s.req) and (s.issued - NWS) in s.rel and s.issued <= s.nget + NWS - 1:
                    s._issue(s.issued)
                    s.issued += 1

        ws = WS()

        def plan_layer(l):
            for cg in range(24):
                ws.plan(wmod[l][:, cg * 512:(cg + 1) * 512], "col")

        for l in range(n_layers):
            plan_layer(l)

        def plan_mixer(l):
            ws.plan(w_in[l][:, OFF_K:OFF_K + 512], "col")
            ws.plan(w_in[l][:, OFF_V:OFF_V + 512], "col")
            for g in range(4):
                ws.plan(w_in[l][:, g * 512:(g + 1) * 512], "col")
            for jg in range(4):
                ws.plan(w_in[l][:, OFF_GA + jg * 512: OFF_GA + (jg + 1) * 512], "col")
                ws.plan(w_oa[l][:, jg * 512:(jg + 1) * 512], "col")
            for jg in range(4):
                ws.plan(w_in[l][:, OFF_U + jg * 512: OFF_U + (jg + 1) * 512], "col")
                ws.plan(w_in[l][:, OFF_GC + jg * 512: OFF_GC + (jg + 1) * 512], "col")
                ws.plan(w_in[l][:, OFF_GB + jg * 512: OFF_GB + (jg + 1) * 512], "col")
            for jg in range(4):
                ws.plan(w_in[l][:, OFF_GCV + jg * 512: OFF_GCV + (jg + 1) * 512], "col")
                ws.plan(w_oc[l][:, jg * 512:(jg + 1) * 512], "col")
            for jg in range(4):
                ws.plan(w_o[l][:, jg * 512:(jg + 1) * 512], "col")

        def plan_ffn(w1, w3, w2, nfg):
            for fg in range(nfg):
                ws.plan(w1[:, fg * 512:(fg + 1) * 512], "col")
                ws.plan(w3[:, fg * 512:(fg + 1) * 512], "col")
                ws.plan(w2[fg * 512:(fg + 1) * 512, :], "row")

        stages = dump[0] if dump else "all"
        if stages in ("all", "l0", "attn0", "mix0", "k0", "q0", "mix1"):
            plan_mixer(0)
        if stages in ("all", "l0", "mix1"):
            plan_ffn(fw1[0], fw3[0], fw2[0], 11)
            plan_ffn(fw1[0], fw3[0], fw2[0], 11)
        if stages in ("all", "mix1") and n_layers == 2:
            plan_mixer(1)
        if stages in ("all",) and n_layers == 2:
            for e in range(NE):
                plan_ffn(mw1[0, e], mw3[0, e], mw2[0, e], 14)

        tc0 = []
        def cload(dst, src, eng="sync", key="c0"):
            t = P.dma(eng, (lambda e, dst=dst, src=src: e.dma_start(out=dst, in_=src)), key)
            tc0.append(t)
            return t
        cload(ident_f, cmat[:, 0, :])
        cload(cm_bf, cmat, eng="gpsimd", key="c1")
        cload(cos_b, cosT, eng="gpsimd", key="c1")
        cload(sin_b, sinT, eng="gpsimd", key="c1")
        cload(bmod_s, bmodT)
        cload(n1_s, n1T)
        cload(n2_s, n2T)
        cload(nf_s, nfT)
        cload(sink_s, sinkB)
        cload(conv_s, convT)
        cload(cv_s, cvec)
        cload(rt_s, routerT)
        t_ms = [P.op("vector", lambda e: e.memset(ones_b, 1.0)),
                P.op("vector", lambda e: e.memset(ones_f, 1.0)),
                P.op("vector", lambda e: e.memset(zeros_f, 0.0)),
                P.op("vector", lambda e: e.memset(eps_t, EPS))]
        CONST = tc0 + t_ms

        t_cs = P.op("scalar", lambda e: e.activation(out=cs_b, in_=cv_s, func=AF.Silu), deps=CONST)

        t_mod = {}
        for l in range(n_layers):
            bi, pb, pdeps = PS.next()
            last = None
            for cg in range(24):
                r, wt, wtok = ws.get()
                for jj in range(4):
                    m = cg * 4 + jj
                    for kc in range(NJ):
                        last = P.op("tensor", (lambda e, pb=pb, wt=wt, jj=jj, kc=kc, m=m: e.matmul(
                            pb[:, 2 * m:2 * m + 2], lhsT=wt[:, kc, jj * 128:(jj + 1) * 128], rhs=cs_b[:, kc, :],
                            start=(kc == 0), stop=(kc == NJ - 1))),
                            deps=[wtok, t_cs] + pdeps + CONST, mark=(kc == NJ - 1 and jj == 3))
                ws.release(r, [last])
            tm = P.op("vector", (lambda e, pb=pb, l=l: e.tensor_tensor(
                out=modT[:, l], in0=pb[:, 0:192].rearrange("p (m c) -> p m c", c=2),
                in1=bmod_s[:, l].unsqueeze(2).to_broadcast([128, 96, 2]), op=ALU.add)), deps=[last] + CONST)
            PS.release(bi, [tm])
            ta = P.op("vector", (lambda e, l=l: e.scalar_tensor_tensor(
                out=A1[:, l], in0=modT[:, l, 16:32, :], scalar=1.0,
                in1=n1_s[:, l].unsqueeze(2).to_broadcast([128, NJ, 2]), op0=ALU.add, op1=ALU.mult)), deps=[tm])
            tb = P.op("vector", (lambda e, l=l: e.scalar_tensor_tensor(
                out=A2[:, l], in0=modT[:, l, 64:80, :], scalar=1.0,
                in1=n2_s[:, l].unsqueeze(2).to_broadcast([128, NJ, 2]), op0=ALU.add, op1=ALU.mult)), deps=[ta])
            t_mod[l] = [tm, ta, tb]
        MODT = [t for l in t_mod for t in t_mod[l]]

        def modcol(l, s, j, c):
            return modT[:, l, s * 16 + j, c:c + 1]

        big_f = view(o_big, F32, 8192)
        xt_tok = [big_f[:, 0:2048], big_f[:, 2048:4096]]
        xs_stg = [big_f[:, 4096:6144].rearrange("p (j t) -> p j t", j=NJ), big_f[:, 6144:8192].rearrange("p (j t) -> p j t", j=NJ)]
        xt_rot = Rot(xt_tok)
        stg_rot = Rot(xs_stg)
        t_xs = []
        for tb in range(XS_T // 128):
            xi, xb, xdeps = xt_rot.next()
            tl = P.dma("sync", (lambda e, xb=xb, tb=tb: e.dma_start(out=xb, in_=xin[tb * 128:(tb + 1) * 128, :])), f"xt{xi}", deps=xdeps)
            si, sb, sdeps = stg_rot.next()
            evs = []
            lastmm = None
            for q4 in range(4):
                bi, pb, pdeps = PS.next()
                for jj in range(4):
                    j = q4 * 4 + jj
                    lastmm = P.op("tensor", (lambda e, pb=pb, xb=xb, jj=jj, j=j: e.transpose(
                        pb[:, jj * 128:(jj + 1) * 128], xb[:, j * 128:(j + 1) * 128], ident_f)),
                        deps=[tl] + pdeps + CONST, mark=(jj == 3))
                eng = "scalar" if q4 % 2 == 0 else "vector"
                if eng == "scalar":
                    ev = P.op("scalar", (lambda e, pb=pb, sb=sb, q4=q4: e.activation(
                        out=sb[:, q4 * 4:(q4 + 1) * 4, :], in_=pb.rearrange("p (a b) -> p a b", a=4), func=AF.Copy)),
                        deps=[lastmm] + sdeps)
                else:
                    ev = P.op("vector", (lambda e, pb=pb, sb=sb, q4=q4: e.tensor_copy(
                        out=sb[:, q4 * 4:(q4 + 1) * 4, :], in_=pb.rearrange("p (a b) -> p a b", a=4))),
                        deps=[lastmm] + sdeps)
                PS.release(bi, [ev])
                evs.append(ev)
            xt_rot.release(xi, [lastmm])
            td = P.dma("sync", (lambda e, sb=sb, tb=tb: e.dma_start(out=XSv[:, :, tb * 128:(tb + 1) * 128], in_=sb)), f"st{si}", deps=evs)
            stg_rot.release(si, [td])
            t_xs.append(td)
        XS_READY = list(t_xs)

        h_all = view(o_h, BF16, 24576)

        def norm_tile(l, which, xsrc, n, cidx, hdst, deps_x, extra_deps, want_rstd=False):
            A = A1 if which == 1 else A2
            bs = 0 if which == 1 else 3
            bi, pb, pdeps = PS.next()
            last = None
            for j in range(NJ):
                qi, qb, qdeps = sq_bufs.next()
                tsq = P.op("scalar", (lambda e, qb=qb, j=j: e.activation(out=qb[:, :n], in_=xsrc[:, j, :], func=AF.Square)),
                           deps=deps_x + qdeps + CONST)
                last = P.op("tensor", (lambda e, pb=pb, qb=qb, j=j: e.matmul(pb[:, :n], lhsT=ones_b, rhs=qb[:, :n], start=(j == 0), stop=(j == NJ - 1))),
                            deps=[tsq] + (pdeps if j == 0 else []), mark=True)
                sq_bufs.release(qi, [last])
            t1 = P.op("scalar", (lambda e, pb=pb: e.activation(out=rt_t[:, :n], in_=pb[:, :n], func=AF.Sqrt, bias=eps_t, scale=1.0 / D)),
                      deps=[last] + extra_deps)
            PS.release(bi, [t1])
            t2 = P.op("vector", (lambda e: e.reciprocal(out=rstd_t[:, :n], in_=rt_t[:, :n])), deps=[t1])
            toks = []
            for j in range(NJ):
                ti, tbuf, tdeps = tmpf_bufs.next()
                ta = P.op("vector", (lambda e, tbuf=tbuf, j=j: e.scalar_tensor_tensor(
                    out=tbuf[:, :n], in0=xsrc[:, j, :], scalar=A[:, l, j, cidx:cidx + 1], in1=rstd_t[:, :n], op0=ALU.mult, op1=ALU.mult)),
                    deps=[t2] + tdeps + MODT)
                tb_ = P.op("scalar", (lambda e, tbuf=tbuf, j=j: e.activation(
                    out=hdst[:, j, :], in_=tbuf[:, :n], func=AF.Identity, bias=modcol(l, bs, j, cidx), scale=1.0)),
                    deps=[ta] + extra_deps + MODT)
                tmpf_bufs.release(ti, [tb_])
                toks.append(tb_)
            return toks, t2

        def mm_group(pb, n, lhs_list, rhs_list, deps, pdeps):
            last = None
            nk = len(lhs_list)
            for i in range(nk):
                last = P.op("tensor", (lambda e, i=i: e.matmul(pb[:, :n], lhsT=lhs_list[i], rhs=rhs_list[i], start=(i == 0), stop=(i == nk - 1))),
                            deps=(list(deps) + list(pdeps)) if i == 0 else [], mark=(i == nk - 1))
            return last

        def dump_and_finish(src_ap_f32_or_bf16, ncols, deps, is_bf16):
            eng = "gpsimd" if is_bf16 else "sync"
            t = P.dma(eng, (lambda e: e.dma_start(out=dbg[:, :ncols], in_=src_ap_f32_or_bf16)), "dbg", deps=deps)
            P.wait_only(eng, [t])

        def mixer(l):
            TFl = 1152 if l == 0 else 1024
            ctx_full = (l == 0)
            TF = TFl + (CTX if ctx_full else 0)
            TH = TFl + CTX + 128
            o_ctx, o_ext = TFl, TFl + CTX
            if l == 0:
                tiles = [(0, 0, 512, 0, 0), (512, 512, 512, 0, 512), (1024, 1024, 128, 0, 1024),
                         (1152, 1152, 256, 1, None), (1408, 1408, 128, 0, 1152)]
            else:
                tiles = [(0, 0, 512, 0, 0), (512, 512, 512, 0, 512), (1024, 1152, 256, 1, None), (1280, 1024, 128, 0, 1024)]
            full_tiles = [t for t in tiles if t[0] + t[2] <= TF]
            h = h_all[:, :NJ * TH].rearrange("p (j t) -> p j t", j=NJ)
            o_y = o_big
            y = view(o_y, BF16, NJ * 1408)[:, :NJ * TF].rearrange("p (j t) -> p j t", j=NJ)
            o_kv = o_big + 45056
            kT = view(o_kv, BF16, 4 * 1536)[:, :4 * TH].rearrange("p (g t) -> p g t", g=4)
            vt = view(o_kv + 12288, BF16, 12 * 512).rearrange("p (b n) -> p b n", n=512)
            o_scr = o_big + 45056 + 24576
            NBQ = TFl // 128

            xt = [view(o_big, F32, 8192).rearrange("p (j t) -> p j t", j=NJ),
                  view(o_big + 32768, F32, 8192).rearrange("p (j t) -> p j t", j=NJ)]
            xrot = Rot(xt)
            h_tok = {}
            for (doff, xcol, n, isc, pos) in tiles:
                xi, xb, xdeps = xrot.next()
                xb = xb[:, :, :n] if n == 512 else view(o_big + xi * 32768, F32, NJ * n).rearrange("p (j t) -> p j t", j=NJ)
                tl = P.dma("sync", (lambda e, xb=xb, xcol=xcol, n=n: e.dma_start(out=xb, in_=XSv[:, :, xcol:xcol + n])),
                           f"xt{xi}", deps=xdeps + XS_READY + k.prev_phase)
                toks, _ = norm_tile(l, 1, xb, n, isc, h[:, :, doff:doff + n], [tl], k.prev_phase)
                xrot.release(xi, toks)
                h_tok[doff] = toks
            H_READY = [t for v in h_tok.values() for t in v]
            if dump and dump[0] == "h0":
                dump_and_finish(h_all[:, :NJ * TH], NJ * TH, H_READY, True)
                return None

            qraw_r = Rot([view(o_scr + i * 1024, BF16, 512) for i in range(2)])
            t1_r = Rot([view(o_scr + 2048 + i * 2048, F32, 512) for i in range(2)])
            t2_r = Rot([view(o_scr + 6144 + i * 2048, F32, 512) for i in range(2)])

            def rope_evac(pb, bi, n, pos, dst, mmtok):
                import os
                mode = os.environ.get("ROPE_DBG", "")
                if mode == "copy":
                    ev = P.op("scalar", (lambda e: e.activation(out=dst, in_=pb[:, :n], func=AF.Copy)), deps=[mmtok])
                    PS.release(bi, [ev])
                    return ev
                if mode == "nomm":
                    i1, b1, d1 = t1_r.next()
                    tx = P.op("vector", (lambda e: e.tensor_tensor(out=b1[:, :n], in0=pb[:, :n], in1=cos_b[:, pos:pos + n], op=ALU.mult)), deps=[mmtok] + d1 + CONST)
                    PS.release(bi, [tx])
                    tz = P.op("vector", (lambda e: e.tensor_tensor(out=dst, in0=b1[:, :n], in1=b1[:, :n], op=ALU.add)), deps=[tx])
                    t1_r.release(i1, [tz])
                    return tz
                qi, qb, qd = qraw_r.next()
                ta = P.op("scalar", (lambda e: e.activation(out=qb[:, :n], in_=pb[:, :n], func=AF.Copy)), deps=[mmtok] + qd)
                b2, pb2, pd2 = PS.next()
                lw = ones_b if mode == "ones" else perm_b
                if mode == "nope":
                    tm = ta
                    pb2 = pb
                else:
                    tm = P.op("tensor", (lambda e: e.matmul(pb2[:, :n], lhsT=lw, rhs=qb[:, :n], start=True, stop=True)), deps=[ta] + pd2 + CONST)
                qraw_r.release(qi, [tm])
                i1, b1, d1 = t1_r.next()
                tx = P.op("vector", (lambda e: e.tensor_tensor(out=b1[:, :n], in0=pb[:, :n], in1=cos_b[:, pos:pos + n], op=ALU.mult)), deps=[mmtok, ta] + d1 + CONST)
                PS.release(bi, [ta, tx])
                i2, bb2, d2 = t2_r.next()
                ty = P.op("vector", (lambda e: e.tensor_tensor(out=bb2[:, :n], in0=pb2[:, :n], in1=sin_b[:, pos:pos + n], op=ALU.mult)), deps=[tm] + d2)
                PS.release(b2, [ty])
                tz = P.op("vector", (lambda e: e.tensor_tensor(out=dst, in0=b1[:, :n], in1=bb2[:, :n], op=ALU.add)), deps=[tx, ty])
                t1_r.release(i1, [tz])
                t2_r.release(i2, [tz])
                return tz

            r, wt, wtok = ws.get()
            k_tok = []
            lastmm = None
            for g in range(4):
                for (doff, xcol, n, isc, pos) in tiles:
                    bi, pb, pd = PS.next()
                    mm = mm_group(pb, n, [wt[:, kc, g * 128:(g + 1) * 128] for kc in range(NJ)], [h[:, kc, doff:doff + n] for kc in range(NJ)],
                                  [wtok] + H_READY, pd)
                    lastmm = mm
                    if isc:
                        ev = P.op("scalar", (lambda e, pb=pb, g=g, doff=doff, n=n: e.activation(out=kT[:, g, doff:doff + n], in_=pb[:, :n], func=AF.Copy)), deps=[mm])
                        PS.release(bi, [ev])
                    else:
                        ev = rope_evac(pb, bi, n, pos, kT[:, g, doff:doff + n], mm)
                    k_tok.append(ev)
            ws.release(r, [lastmm])
            if dump and dump[0] == "k0":
                dump_and_finish(view(o_kv, BF16, 4 * 1536), 4 * 1536, k_tok, True)
                return None
            r, wt, wtok = ws.get()
            v_tok = []
            for tb in range(TH // 128):
                bi, pb, pd = PS.next()
                mm = mm_group(pb, 512, [h[:, kc, tb * 128:(tb + 1) * 128] for kc in range(NJ)], [wt[:, kc, :] for kc in range(NJ)], [wtok] + H_READY, pd)
                lastmm = mm
                if tb % 2 == 0:
                    ev = P.op("scalar", (lambda e, pb=pb, tb=tb: e.activation(out=vt[:, tb, :], in_=pb, func=AF.Copy)), deps=[mm])
                else:
                    ev = P.op("vector", (lambda e, pb=pb, tb=tb: e.tensor_copy(out=vt[:, tb, :], in_=pb)), deps=[mm])
                PS.release(bi, [ev])
                v_tok.append(ev)
            ws.release(r, [lastmm])
            q_tok = []
            for g4 in range(4):
                r, wt, wtok = ws.get()
                for jj in range(4):
                    hd = g4 * 4 + jj
                    for (doff, xcol, n, isc, pos) in full_tiles:
                        bi, pb, pd = PS.next()
                        mm = mm_group(pb, n, [wt[:, kc, jj * 128:(jj + 1) * 128] for kc in range(NJ)], [h[:, kc, doff:doff + n] for kc in range(NJ)],
                                      [wtok] + H_READY, pd)
                        lastmm = mm
                        if isc:
                            ev = P.op("scalar", (lambda e, pb=pb, hd=hd, doff=doff, n=n: e.activation(out=y[:, hd, doff:doff + n], in_=pb[:, :n], func=AF.Copy)), deps=[mm])
                            PS.release(bi, [ev])
                        else:
                            ev = rope_evac(pb, bi, n, pos, y[:, hd, doff:doff + n], mm)
                        q_tok.append(ev)
                ws.release(r, [lastmm])
            QKV = k_tok + v_tok + q_tok
            if dump and dump[0] == "q0":
                dump_and_finish(view(o_y, BF16, NJ * 1408)[:, :NJ * TF], NJ * TF, QKV, True)
                return None

            et_r = Rot([view(o_scr + 10240 + i * 5120, BF16, 2560).rearrange("p (c n) -> p c n", c=5) for i in range(2)])
            ds_r = Rot([view(o_scr + 20480, F32, 512)])
            rd_r = Rot([view(o_scr + 22528, F32, 512)])
            att_tok = []
            qblocks = [(n, n * 128, False) for n in range(NBQ)]
            if ctx_full:
                qblocks += [(None, o_ctx, True), (None, o_ctx + 128, True)]
            prev_sf = []
            for g in range(4):
                tsf = None
                for hi in range(4):
                    hd = g * 4 + hi
                    tsf = P.op("scalar", (lambda e, hi=hi, hd=hd: e.activation(out=sinkfull[:, hi * 128:(hi + 1) * 128], in_=zeros_f, func=AF.Exp,
                                                                         bias=sink_s[:, l, hd:hd + 1], scale=1.0)), deps=CONST + prev_sf)
                prev_sf = []
                for (nb, qoff, qctx) in qblocks:
                    chunks = []
                    if not qctx:
                        if nb - 1 >= 0:
                            chunks.append(((nb - 1) * 128, mask1_b))
                        chunks.append((nb * 128, None))
                        chunks.append(((nb + 1) * 128 if nb + 1 < NBQ else o_ext, mask2_b))
                    chunks.append((o_ctx, None))
                    chunks.append((o_ctx + 128, None))
                    ei, et, ed = et_r.next()
                    bn, pnum, pdn = PS.next()
                    bd, pden, pdd = PS.next()
                    etoks = []
                    for ci, (koff, msk) in enumerate(chunks):
                        bs_, ps_, pds = PS.next()
                        mm = P.op("tensor", (lambda e, ps_=ps_, koff=koff, qoff=qoff: e.matmul(
                            ps_, lhsT=kT[:, g, koff:koff + 128], rhs=y[:, g * 4:(g + 1) * 4, qoff:qoff + 128], start=True, stop=True)),
                            deps=QKV + pds + att_tok[-2:], mark=True)
                        te = P.op("scalar", (lambda e, ps_=ps_, et=et, ci=ci: e.activation(out=et[:, ci, :], in_=ps_, func=AF.Exp, scale=SCALE)), deps=[mm] + ed)
                        PS.release(bs_, [te])
                        if msk is not None:
                            te = P.op("vector", (lambda e, et=et, ci=ci, msk=msk: e.tensor_tensor(
                                out=et[:, ci, :].rearrange("p (a b) -> p a b", a=4), in0=et[:, ci, :].rearrange("p (a b) -> p a b", a=4),
                                in1=msk.unsqueeze(1).to_broadcast([128, 4, 128]), op=ALU.mult)), deps=[te] + CONST)
                        etoks.append(te)
                    nc_ = len(chunks)
                    lastn = None
                    for ci, (koff, msk) in enumerate(chunks):
                        lastn = P.op("tensor", (lambda e, ci=ci, koff=koff, et=et, pnum=pnum: e.matmul(
                            pnum, lhsT=vt[:, koff // 128, g * 128:(g + 1) * 128], rhs=et[:, ci, :], start=(ci == 0), stop=(ci == nc_ - 1))),
                            deps=[etoks[ci]] + (pdn if ci == 0 else []), mark=(ci == nc_ - 1))
                    lastd = None
                    for ci in range(nc_):
                        lastd = P.op("tensor", (lambda e, ci=ci, et=et, pden=pden: e.matmul(
                            pden, lhsT=ones_b, rhs=et[:, ci, :], start=(ci == 0), stop=(ci == nc_ - 1))),
                            deps=(pdd if ci == 0 else []), mark=(ci == nc_ - 1))
                    et_r.release(ei, [lastd])
                    di, dsb, dd = ds_r.next()
                    ta = P.op("vector", (lambda e, dsb=dsb, pden=pden: e.tensor_tensor(out=dsb, in0=pden, in1=sinkfull, op=ALU.add)), deps=[lastd, tsf] + dd)
                    PS.release(bd, [ta])
                    ri, rdb, rdd = rd_r.next()
                    tr = P.op("vector", (lambda e, dsb=dsb, rdb=rdb: e.reciprocal(out=rdb, in_=dsb)), deps=[ta] + rdd)
                    ds_r.release(di, [tr])
                    ty = P.op("vector", (lambda e, rdb=rdb, pnum=pnum, qoff=qoff: e.tensor_tensor(
                        out=y[:, g * 4:(g + 1) * 4, qoff:qoff + 128], in0=pnum.rearrange("p (a b) -> p a b", a=4),
                        in1=rdb.rearrange("p (a b) -> p a b", a=4), op=ALU.mult)), deps=[tr, lastn])
                    PS.release(bn, [ty])
                    rd_r.release(ri, [ty])
                    att_tok.append(ty)
                    prev_sf = [ta]
            ATT = att_tok
            if dump and dump[0] == "attn0":
                dump_and_finish(view(o_y, BF16, NJ * 1408)[:, :NJ * TF], NJ * TF, ATT, True)
                return None

            ms_r = Rot([view(o_scr, BF16, 4 * 1408)[:, :4 * TF].rearrange("p (a t) -> p a t", a=4)])
            min_r = Rot([view(o_scr + 11264 + i * 1024, BF16, 512) for i in range(2)])
            x1_r = Rot([rstd_t])
            a_done = []
            ms_writes = []
            for jg in range(4):
                r1, wg, wgt = ws.get()
                r2, wa, wat = ws.get()
                mi, msb, msd = ms_r.next()
                evs = []
                lastmm = None
                for jj in range(4):
                    for (doff, xcol, n, isc, pos) in full_tiles:
                        b1, p1, pd1 = PS.next()
                        m1 = mm_group(p1, n, [wg[:, kc, jj * 128:(jj + 1) * 128] for kc in range(NJ)], [h[:, kc, doff:doff + n] for kc in range(NJ)], [wgt] + H_READY, pd1)
                        b2, p2, pd2 = PS.next()
                        m2 = mm_group(p2, n, [wa[:, kc, jj * 128:(jj + 1) * 128] for kc in range(NJ)], [y[:, kc, doff:doff + n] for kc in range(NJ)], [wat] + ATT, pd2)
                        lastmm = m2
                        ti, tb_, td = tmpf_bufs.next()
                        ts = P.op("scalar", (lambda e, p1=p1, tb_=tb_, n=n: e.activation(out=tb_[:, :n], in_=p1[:, :n], func=AF.Sigmoid)), deps=[m1] + td)
                        PS.release(b1, [ts])
                        tv = P.op("vector", (lambda e, p2=p2, tb_=tb_, n=n, msb=msb, jj=jj, doff=doff: e.tensor_tensor(
                            out=msb[:, jj, doff:doff + n], in0=p2[:, :n], in1=tb_[:, :n], op=ALU.mult)), deps=[ts, m2] + msd)
                        PS.release(b2, [tv])
                        tmpf_bufs.release(ti, [tv])
                        evs.append(tv)
                ws.release(r1, [lastmm])
                ws.release(r2, [lastmm])
                a_done.append(lastmm)
                tw = P.dma("sync", (lambda e, msb=msb, jg=jg: e.dma_start(out=MSv[:, jg * 4:(jg + 1) * 4, :TF], in_=msb)), "mo0", deps=evs)
                ms_r.release(mi, [tw])
                ms_writes.append(tw)

            zL = view(o_scr, F32, 1160)
            zC = view(o_scr + 4640, F32, 264)
            a1 = view(o_scr + 5760, F32, 1408)
            tz0 = [P.op("vector", lambda e: e.memset(zL[:, 0:1], 0.0), deps=ms_writes + a_done),
                   P.op("vector", lambda e: e.memset(zC[:, 0:1], 0.0)),
                   P.op("vector", lambda e: e.memset(zC[:, 257:258], 0.0))]
            conv_done = []
            prev_chunk = list(tz0)
            for jg in range(4):
                r1, wu, wut = ws.get()
                r2, wc, wct = ws.get()
                r3, wb, wbt = ws.get()
                lastmm = None
                for jj in range(4):
                    j = jg * 4 + jj
                    ztoks = []
                    for (doff, xcol, n, isc, pos) in tiles:
                        if isc and not ctx_full:
                            continue
                        is_ext = (doff == o_ext)
                        nn = 1 if is_ext else n
                        b1, p1, pd1 = PS.next()
                        m1 = mm_group(p1, nn, [wu[:, kc, jj * 128:(jj + 1) * 128] for kc in range(NJ)], [h[:, kc, doff:doff + nn] for kc in range(NJ)], [wut] + H_READY, pd1)
                        b2, p2, pd2 = PS.next()
                        m2 = mm_group(p2, nn, [wc[:, kc, jj * 128:(jj + 1) * 128] for kc in range(NJ)], [h[:, kc, doff:doff + nn] for kc in range(NJ)], [wct] + H_READY, pd2)
                        lastmm = m2
                        ti, tb_, td = tmpf_bufs.next()
                        ts = P.op("scalar", (lambda e, p1=p1, tb_=tb_, nn=nn: e.activation(out=tb_[:, :nn], in_=p1[:, :nn], func=AF.Copy)), deps=[m1] + td)
                        PS.release(b1, [ts])
                        if isc:
                            zdst = zC[:, 1:257]
                        elif is_ext:
                            zdst = zL[:, 1 + TFl:2 + TFl]
                        else:
                            zdst = zL[:, 1 + doff:1 + doff + n]
                        tv = P.op("vector", (lambda e, p2=p2, tb_=tb_, nn=nn, zdst=zdst: e.tensor_tensor(out=zdst, in0=p2[:, :nn], in1=tb_[:, :nn], op=ALU.mult)),
                                  deps=[ts, m2] + prev_chunk)
                        PS.release(b2, [tv])
                        tmpf_bufs.release(ti, [tv])
                        ztoks.append(tv)
                    segs = [(zL, 0, TFl)] + ([(zC, o_ctx, CTX)] if ctx_full else [])
                    ctoks = []
                    for (zb, aoff, T_) in segs:
                        c0 = P.op("vector", (lambda e, zb=zb, aoff=aoff, T_=T_, j=j: e.tensor_scalar(
                            out=a1[:, aoff:aoff + T_], in0=zb[:, 0:T_], scalar1=conv_s[:, l, 0, j:j + 1], scalar2=None, op0=ALU.mult)), deps=ztoks + prev_chunk)
                        c1 = P.op("vector", (lambda e, zb=zb, aoff=aoff, T_=T_, j=j: e.scalar_tensor_tensor(
                            out=a1[:, aoff:aoff + T_], in0=zb[:, 1:T_ + 1], scalar=conv_s[:, l, 1, j:j + 1], in1=a1[:, aoff:aoff + T_], op0=ALU.mult, op1=ALU.add)), deps=[c0])
                        c2 = P.op("vector", (lambda e, zb=zb, aoff=aoff, T_=T_, j=j: e.scalar_tensor_tensor(
                            out=a1[:, aoff:aoff + T_], in0=zb[:, 2:T_ + 2], scalar=conv_s[:, l, 2, j:j + 1], in1=a1[:, aoff:aoff + T_], op0=ALU.mult, op1=ALU.add)), deps=[c1])
                        ctoks.append(c2)
                    ytoks = []
                    for (doff, xcol, n, isc, pos) in full_tiles:
                        b3, p3, pd3 = PS.next()
                        m3 = mm_group(p3, n, [wb[:, kc, jj * 128:(jj + 1) * 128] for kc in range(NJ)], [h[:, kc, doff:doff + n] for kc in range(NJ)], [wbt] + H_READY, pd3)
                        lastmm = m3
                        ty = P.op("vector", (lambda e, p3=p3, j=j, doff=doff, n=n: e.tensor_tensor(out=y[:, j, doff:doff + n], in0=p3[:, :n], in1=a1[:, doff:doff + n], op=ALU.mult)),
                                  deps=[m3] + ctoks + a_done)
                        PS.release(b3, [ty])
                        ytoks.append(ty)
                    prev_chunk = ytoks
                    conv_done += ytoks
                ws.release(r1, [lastmm])
                ws.release(r2, [lastmm])
                ws.release(r3, [lastmm])

            c_done = []
            ms2_writes = []
            ms_r2 = Rot([view(o_scr, BF16, 4 * 1408)[:, :4 * TF].rearrange("p (a t) -> p a t", a=4)])
            first = True
            for jg in range(4):
                r1, wg, wgt = ws.get()
                r2, wa, wat = ws.get()
                mi, msb, msd = ms_r2.next()
                if first:
                    msd = msd + conv_done
                    first = False
                evs = []
                lastmm = None
                for jj in range(4):
                    j = jg * 4 + jj
                    for (doff, xcol, n, isc, pos) in full_tiles:
                        ii, mib, mid_ = min_r.next()
                        tl = P.dma("sync", (lambda e, mib=mib, j=j, doff=doff, n=n: e.dma_start(out=mib[:, :n], in_=MSv[:, j, doff:doff + n])), f"mi{ii}", deps=mid_ + ms_writes + conv_done[-1:])
                        b1, p1, pd1 = PS.next()
                        m1 = mm_group(p1, n, [wg[:, kc, jj * 128:(jj + 1) * 128] for kc in range(NJ)], [h[:, kc, doff:doff + n] for kc in range(NJ)], [wgt] + H_READY, pd1)
                        b2, p2, pd2 = PS.next()
                        m2 = mm_group(p2, n, [wa[:, kc, jj * 128:(jj + 1) * 128] for kc in range(NJ)], [y[:, kc, doff:doff + n] for kc in range(NJ)], [wat] + conv_done, pd2)
                        lastmm = m2
                        ti, tb_, td = tmpf_bufs.next()
                        ts = P.op("scalar", (lambda e, p1=p1, tb_=tb_, n=n: e.activation(out=tb_[:, :n], in_=p1[:, :n], func=AF.Sigmoid)), deps=[m1] + td)
                        PS.release(b1, [ts])
                        xi_, xb_, xd_ = x1_r.next()
                        tv = P.op("vector", (lambda e, p2=p2, tb_=tb_, xb_=xb_, n=n: e.tensor_tensor(out=xb_[:, :n], in0=p2[:, :n], in1=tb_[:, :n], op=ALU.mult)), deps=[ts, m2] + xd_)
                        PS.release(b2, [tv])
                        tmpf_bufs.release(ti, [tv])
                        tw_ = P.op("vector", (lambda e, xb_=xb_, mib=mib, msb=msb, jj=jj, doff=doff, n=n: e.tensor_tensor(
                            out=msb[:, jj, doff:doff + n], in0=xb_[:, :n], in1=mib[:, :n], op=ALU.add)), deps=[tv, tl] + msd)
                        x1_r.release(xi_, [tw_])
                        min_r.release(ii, [tw_])
                        evs.append(tw_)
                ws.release(r1, [lastmm])
                ws.release(r2, [lastmm])
                c_done.append(lastmm)
                tw = P.dma("sync", (lambda e, msb=msb, jg=jg: e.dma_start(out=MSv[:, jg * 4:(jg + 1) * 4, :TF], in_=msb)), "mo1", deps=evs)
                ms_r2.release(mi, [tw])
                ms2_writes.append(tw)

            Mall = y
            tM = P.dma("sync", (lambda e: e.dma_start(out=Mall, in_=MSv[:, :, :TF])), "big", deps=ms2_writes + c_done)
            xo_r = Rot([view(o_scr + i * 2048, F32, 512) for i in range(2)])
            xn_r = Rot([view(o_scr + 4096 + i * 2048, F32, 512) for i in range(2)])
            o_writes = []
            for jg in range(4):
                r1, wo_, wot = ws.get()
                lastmm = None
                for jj in range(4):
                    j = jg * 4 + jj
                    for (doff, xcol, n, isc, pos) in full_tiles:
                        oi, xob, xod = xo_r.next()
                        tl = P.dma("sync", (lambda e, xob=xob, j=j, xcol=xcol, n=n: e.dma_start(out=xob[:, :n], in_=XSv[:, j, xcol:xcol + n])), f"xo{oi}", deps=xod + XS_READY + k.prev_phase + ms2_writes[-1:])
                        b1, p1, pd1 = PS.next()
                        m1 = mm_group(p1, n, [wo_[:, kc, jj * 128:(jj + 1) * 128] for kc in range(NJ)], [Mall[:, kc, doff:doff + n] for kc in range(NJ)], [wot, tM], pd1)
                        lastmm = m1
                        ni, xnb, xnd = xn_r.next()
                        tv = P.op("vector", (lambda e, p1=p1, xob=xob, xnb=xnb, n=n, j=j, isc=isc: e.scalar_tensor_tensor(
                            out=xnb[:, :n], in0=p1[:, :n], scalar=modcol(l, 2, j, isc), in1=xob[:, :n], op0=ALU.mult, op1=ALU.add)), deps=[m1, tl] + xnd + MODT)
                        PS.release(b1, [tv])
                        xo_r.release(oi, [tv])
                        tw = P.dma("sync", (lambda e, xnb=xnb, j=j, xcol=xcol, n=n: e.dma_start(out=XSv[:, j, xcol:xcol + n], in_=xnb[:, :n])), f"xn{ni}", deps=[tv])
                        xn_r.release(ni, [tw])
                        o_writes.append(tw)
                ws.release(r1, [lastmm])
            return o_writes + [lastmm]

        def barrier(toks):
            for eng in ["tensor", "vector", "scalar", "sync"]:
                P.wait_only(eng, toks)

        def ffn(l, ptiles, w_list, moe, final):
            Tp = sum(t[1] for t in ptiles)
            xr = view(o_big, F32, NJ * 1024)[:, :NJ * Tp].rearrange("p (j t) -> p j t", j=NJ)
            h2 = h_all[:, :NJ * Tp].rearrange("p (j t) -> p j t", j=NJ)
            ag_r = Rot([view(o_big + 65536 + i * 8192, BF16, 4 * 1024)[:, :4 * Tp].rearrange("p (a t) -> p a t", a=4) for i in range(2)])
            combB = h_all[:, 16384:16384 + NE * 1024].rearrange("p (e t) -> p e t", e=NE)
            offs = []
            o = 0
            for (xcol, n, isc) in ptiles:
                offs.append(o)
                o += n
            xl = []
            for ti_, (xcol, n, isc) in enumerate(ptiles):
                tl = P.dma("sync", (lambda e, xcol=xcol, n=n, o_=offs[ti_]: e.dma_start(out=xr[:, :, o_:o_ + n], in_=XSv[:, :, xcol:xcol + n])), f"xt{ti_}", deps=k.prev_phase)
                xl.append(tl)
            h2_tok = []
            rt_prev = []
            for ti_, (xcol, n, isc) in enumerate(ptiles):
                o_ = offs[ti_]
                toks, t2 = norm_tile(l, 2, xr[:, :, o_:o_ + n], n, isc, h2[:, :, o_:o_ + n], [xl[ti_]], k.prev_phase + rt_prev)
                rt_prev = []
                h2_tok += toks
                if moe:
                    for tb in range(n // 128):
                        bi, pb, pd = PS.next()
                        tt = P.op("tensor", (lambda e, pb=pb, tb=tb: e.transpose(pb[:, 0:128], rstd_t[:, tb * 128:(tb + 1) * 128], ident_f)), deps=[t2] + pd + CONST)
                        gtb = (o_ + tb * 128) // 128
                        tc_2 = P.op("vector", (lambda e, pb=pb, gtb=gtb: e.tensor_copy(out=rT_all[:, gtb:gtb + 1], in_=pb[:, 0:1])), deps=[tt])
                        PS.release(bi, [tc_2])
                        h2_tok.append(tc_2)
                        rt_prev.append(tc_2)
            H2 = h2_tok
            import os
            mdbg = os.environ.get("MOE_DBG", "")
            if moe and mdbg == "noroute":
                tcb = P.op("vector", (lambda e: e.memset(combB, 0.5)), deps=H2)
                H2 = H2 + [tcb]
            if moe and mdbg != "noroute":
                NTB = Tp // 128
                t_ra = P.op("vector", (lambda e: e.tensor_tensor(out=rtA, in0=rt_s, in1=A2[:, l, :, 0:1].to_broadcast([128, NJ, NE]), op=ALU.mult)), deps=CONST + MODT)
                bmr = Rot([tmpf_bufs.bufs[0][:, 0:128], tmpf_bufs.bufs[1][:, 0:128]])
                bi, pbias, pd = PS.next()
                lastb = None
                for j in range(NJ):
                    mi_, bm, bmd = bmr.next()
                    tbm = P.op("vector", (lambda e, bm=bm, j=j: e.tensor_scalar(out=bm, in0=ones_f, scalar1=modcol(l, 3, j, 0), scalar2=None, op0=ALU.mult)), deps=bmd + H2 + MODT)
                    lastb = P.op("tensor", (lambda e, bm=bm, j=j: e.matmul(pbias[:, 0:NE], lhsT=bm, rhs=rt_s[:, j, :], start=(j == 0), stop=(j == NJ - 1))), deps=[tbm] + (pd if j == 0 else []))
                    bmr.release(mi_, [lastb])
                tbs = P.op("vector", (lambda e: e.tensor_copy(out=bias_sb, in_=pbias[:, 0:NE])), deps=[lastb])
                PS.release(bi, [tbs])
                bi, plg, pd = PS.next()
                lastl = None
                for tb in range(NTB):
                    for j in range(NJ):
                        lastl = P.op("tensor", (lambda e, tb=tb, j=j: e.matmul(plg[:, tb * NE:(tb + 1) * NE], lhsT=xr[:, j, tb * 128:(tb + 1) * 128], rhs=rtA[:, j, :], start=(j == 0), stop=(j == NJ - 1))),
                                     deps=[t_ra] + xl + (pd if (tb == 0 and j == 0) else []))
                S_ = NTB * NE
                rsc = rt_t
                lg = rsc[:, 0:S_].rearrange("p (b e) -> p b e", e=NE)
                eq1 = rsc[:, 64:64 + S_].rearrange("p (b e) -> p b e", e=NE)
                lg2 = rsc[:, 128:128 + S_].rearrange("p (b e) -> p b e", e=NE)
                eq2 = rsc[:, 192:192 + S_].rearrange("p (b e) -> p b e", e=NE)
                cmb = rsc[:, 256:256 + S_].rearrange("p (b e) -> p b e", e=NE)
                m1_ = rsc[:, 320:320 + NTB]
                m2_ = rsc[:, 328:328 + NTB]
                dd_ = rsc[:, 336:336 + NTB]
                e2_ = rsc[:, 344:344 + NTB]
                p1_ = rsc[:, 352:352 + NTB]
                p2_ = rsc[:, 360:360 + NTB]
                tq = None
                for tb in range(NTB):
                    tq = P.op("vector", (lambda e, tb=tb: e.scalar_tensor_tensor(out=lg[:, tb, :], in0=plg[:, tb * NE:(tb + 1) * NE], scalar=rT_all[:, tb:tb + 1], in1=bias_sb, op0=ALU.mult, op1=ALU.add)),
                              deps=[lastl, tbs] + H2)
                PS.release(bi, [tq])
                bc = lambda v: v.unsqueeze(2).to_broadcast([128, NTB, NE])
                tq = P.op("vector", (lambda e: e.tensor_reduce(out=m1_, in_=lg, axis=mybir.AxisListType.X, op=ALU.max)), deps=[tq])
                tq = P.op("vector", (lambda e: e.tensor_tensor(out=eq1, in0=lg, in1=bc(m1_), op=ALU.is_equal)), deps=[tq])
                tq = P.op("vector", (lambda e: e.scalar_tensor_tensor(out=lg2, in0=eq1, scalar=-1e30, in1=lg, op0=ALU.mult, op1=ALU.add)), deps=[tq])
                tq = P.op("vector", (lambda e: e.tensor_reduce(out=m2_, in_=lg2, axis=mybir.AxisListType.X, op=ALU.max)), deps=[tq])
                tq = P.op("vector", (lambda e: e.tensor_tensor(out=eq2, in0=lg2, in1=bc(m2_), op=ALU.is_equal)), deps=[tq])
                tq = P.op("vector", (lambda e: e.tensor_tensor(out=dd_, in0=m2_, in1=m1_, op=ALU.subtract)), deps=[tq])
                tq = P.op("scalar", (lambda e: e.activation(out=e2_, in_=dd_, func=AF.Exp)), deps=[tq])
                tq = P.op("vector", (lambda e: e.tensor_scalar(out=p2_, in0=e2_, scalar1=1.0, scalar2=None, op0=ALU.add)), deps=[tq])
                tq = P.op("vector", (lambda e: e.reciprocal(out=p1_, in_=p2_)), deps=[tq])
                tq = P.op("vector", (lambda e: e.tensor_tensor(out=p2_, in0=e2_, in1=p1_, op=ALU.mult)), deps=[tq])
                tq = P.op("vector", (lambda e: e.tensor_tensor(out=eq1, in0=eq1, in1=bc(p1_), op=ALU.mult)), deps=[tq])
                tq = P.op("vector", (lambda e: e.tensor_tensor(out=eq2, in0=eq2, in1=bc(p2_), op=ALU.mult)), deps=[tq])
                tq = P.op("vector", (lambda e: e.tensor_tensor(out=cmb, in0=eq1, in1=eq2, op=ALU.add)), deps=[tq])
                dgv = view(o_tmp + 2048, BF16, 1024).rearrange("p (e t) -> p e t", e=NE)
                cb_tok = []
                tprev = [tq]
                for tb in range(NTB):
                    td_ = P.op("vector", (lambda e, tb=tb: e.tensor_tensor(out=dgv, in0=ident_f.unsqueeze(1).to_broadcast([128, NE, 128]),
                                                                          in1=cmb[:, tb, :].unsqueeze(2).to_broadcast([128, NE, 128]), op=ALU.mult)), deps=tprev)
                    mms = []
                    for hf in range(2):
                        bi, pb, pd = PS.next()
                        tm = P.op("tensor", (lambda e, pb=pb, hf=hf: e.matmul(pb, lhsT=ones_b, rhs=dgv[:, hf * 4:(hf + 1) * 4, :], start=True, stop=True)), deps=[td_] + pd)
                        te = P.op("vector", (lambda e, pb=pb, hf=hf, tb=tb: e.tensor_copy(out=combB[:, hf * 4:(hf + 1) * 4, tb * 128:(tb + 1) * 128], in_=pb.rearrange("p (a b) -> p a b", a=4))), deps=[tm])
                        PS.release(bi, [te])
                        mms.append(tm)
                        cb_tok.append(te)
                    tprev = mms
                if mdbg == "r2":
                    cb_tok = [P.op("vector", (lambda e: e.memset(combB, 0.5)), deps=cb_tok)]
                H2 = H2 + cb_tok

            tile_list = [(offs[i], ptiles[i][1], ptiles[i][2]) for i in range(len(ptiles))]
            x_last = {}
            for ei, nfg in enumerate(w_list):
                for fg in range(nfg):
                    r1, w1t, w1k = ws.get()
                    r2, w3t, w3k = ws.get()
                    r3, w2t, w2k = ws.get()
                    gi, ag, agd = ag_r.next()
                    atoks = []
                    lastmm = None
                    for ff in range(4):
                        for (o_, n, isc) in tile_list:
                            b1, p1, pd1 = PS.next()
                            m1 = mm_group(p1, n, [w1t[:, kc, ff * 128:(ff + 1) * 128] for kc in range(NJ)], [h2[:, kc, o_:o_ + n] for kc in range(NJ)], [w1k] + H2, pd1)
                            b3, p3, pd3 = PS.next()
                            m3 = mm_group(p3, n, [w3t[:, kc, ff * 128:(ff + 1) * 128] for kc in range(NJ)], [h2[:, kc, o_:o_ + n] for kc in range(NJ)], [w3k] + H2, pd3)
                            lastmm = m3
                            qi, qb, qd = sq_bufs.next()
                            ts = P.op("scalar", (lambda e, p1=p1, qb=qb, n=n: e.activation(out=qb[:, :n], in_=p1[:, :n], func=AF.Silu)), deps=[m1] + qd)
                            PS.release(b1, [ts])
                            tv = P.op("vector", (lambda e, p3=p3, qb=qb, ag=ag, ff=ff, o_=o_, n=n: e.tensor_tensor(out=ag[:, ff, o_:o_ + n], in0=p3[:, :n], in1=qb[:, :n], op=ALU.mult)), deps=[ts, m3] + agd)
                            PS.release(b3, [tv])
                            sq_bufs.release(qi, [tv])
                            if moe:
                                tv = P.op("vector", (lambda e, ag=ag, ff=ff, o_=o_, n=n, ei=ei: e.tensor_tensor(out=ag[:, ff, o_:o_ + n], in0=ag[:, ff, o_:o_ + n], in1=combB[:, ei, o_:o_ + n], op=ALU.mult)), deps=[tv])
                            atoks.append(tv)
                    ws.release(r1, [lastmm])
                    ws.release(r2, [lastmm])
                    lastd = None
                    for i in range(NJ):
                        for (o_, n, isc) in tile_list:
                            bo, po, pdo = PS.next()
                            lastd = mm_group(po, n, [w2t[:, ff, i * 128:(i + 1) * 128] for ff in range(4)], [ag[:, ff, o_:o_ + n] for ff in range(4)], [w2k] + atoks, pdo)
                            tx = P.op("vector", (lambda e, po=po, i=i, o_=o_, n=n, isc=isc: e.scalar_tensor_tensor(
                                out=xr[:, i, o_:o_ + n], in0=po[:, :n], scalar=modcol(l, 5, i, isc), in1=xr[:, i, o_:o_ + n], op0=ALU.mult, op1=ALU.add)),
                                deps=[lastd] + ([x_last[(i, o_)]] if (i, o_) in x_last else xl + H2) + MODT)
                            PS.release(bo, [tx])
                            x_last[(i, o_)] = tx
                    ws.release(r3, [lastd])
                    ag_r.release(gi, [lastd])
            XDONE = list(x_last.values())
            if not final:
                outs = []
                for ti_, (xcol, n, isc) in enumerate(ptiles):
                    o_ = offs[ti_]
                    tw = P.dma("sync", (lambda e, xcol=xcol, n=n, o_=o_: e.dma_start(out=XSv[:, :, xcol:xcol + n], in_=xr[:, :, o_:o_ + n])), "big", deps=XDONE)
                    outs.append(tw)
                return outs
            ost = [view(o_big + 65536 + i * 8192, F32, 2048) for i in range(2)]
            ost_r = Rot(ost)
            fouts = []
            for (o_, n, isc) in tile_list:
                bi, pb, pdeps = PS.next()
                last = None
                for j in range(NJ):
                    qi, qb, qdeps = sq_bufs.next()
                    tsq = P.op("scalar", (lambda e, qb=qb, j=j, o_=o_, n=n: e.activation(out=qb[:, :n], in_=xr[:, j, o_:o_ + n], func=AF.Square)), deps=XDONE + qdeps)
                    last = P.op("tensor", (lambda e, pb=pb, qb=qb, j=j, n=n: e.matmul(pb[:, :n], lhsT=ones_b, rhs=qb[:, :n], start=(j == 0), stop=(j == NJ - 1))), deps=[tsq] + (pdeps if j == 0 else []))
                    sq_bufs.release(qi, [last])
                t1 = P.op("scalar", (lambda e, pb=pb, n=n: e.activation(out=rt_t[:, :n], in_=pb[:, :n], func=AF.Sqrt, bias=eps_t, scale=1.0 / D)), deps=[last] + fouts[-1:])
                PS.release(bi, [t1])
                t2 = P.op("vector", (lambda e, n=n: e.reciprocal(out=rstd_t[:, :n], in_=rt_t[:, :n])), deps=[t1])
                for tb in range(n // 128):
                    oi, ob, od = ost_r.next()
                    evs = []
                    for q4 in range(4):
                        bi, pb, pd = PS.next()
                        lastt = None
                        for jj in range(4):
                            j = q4 * 4 + jj
                            ti, tbuf, tdeps = tmpf_bufs.next()
                            ta = P.op("vector", (lambda e, tbuf=tbuf, j=j, o_=o_, tb=tb: e.scalar_tensor_tensor(
                                out=tbuf[:, :128], in0=xr[:, j, o_ + tb * 128:o_ + (tb + 1) * 128], scalar=nf_s[:, j:j + 1], in1=rstd_t[:, tb * 128:(tb + 1) * 128], op0=ALU.mult, op1=ALU.mult)),
                                deps=[t2] + tdeps)
                            lastt = P.op("tensor", (lambda e, pb=pb, tbuf=tbuf, jj=jj: e.transpose(pb[:, jj * 128:(jj + 1) * 128], tbuf[:, :128], ident_f)), deps=[ta] + (pd if jj == 0 else []))
                            tmpf_bufs.release(ti, [lastt])
                        if q4 % 2 == 0:
                            ev = P.op("scalar", (lambda e, pb=pb, ob=ob, q4=q4: e.activation(out=ob[:, q4 * 512:(q4 + 1) * 512], in_=pb, func=AF.Copy)), deps=[lastt] + od)
                        else:
                            ev = P.op("vector", (lambda e, pb=pb, ob=ob, q4=q4: e.tensor_copy(out=ob[:, q4 * 512:(q4 + 1) * 512], in_=pb)), deps=[lastt] + od)
                        PS.release(bi, [ev])
                        evs.append(ev)
                    r0 = o_ + tb * 128
                    tw = P.dma("sync", (lambda e, ob=ob, r0=r0: e.dma_start(out=out[r0:r0 + 128, :], in_=ob)), f"fin{oi}", deps=evs)
                    ost_r.release(oi, [tw])
                    fouts.append(tw)
            return fouts

        k.prev_phase = []
        res = mixer(0)
        if res is None:
            P.build(st)
            return nc
        barrier(res)
        k.prev_phase = res
        if dump and dump[0] == "mix0":
            t = P.dma("sync", (lambda e: e.dma_start(out=dbg.rearrange("p (j t) -> p j t", j=NJ), in_=XSv)), "dbg", deps=res)
            P.wait_only("sync", [t])
            P.build(st)
            return nc
        r1_ = ffn(0, [(0, 512, 0), (512, 192, 0)], [11], False, False)
        barrier(r1_)
        k.prev_phase = r1_
        r2_ = ffn(0, [(704, 448, 0), (1152, 256, 1)], [11], False, False)
        barrier(r2_)
        k.prev_phase = r2_
        if dump and dump[0] == "l0":
            t = P.dma("sync", (lambda e: e.dma_start(out=dbg.rearrange("p (j t) -> p j t", j=NJ), in_=XSv)), "dbg", deps=r2_)
            P.wait_only("sync", [t])
            P.build(st)
            return nc
        res = mixer(1)
        barrier(res)
        k.prev_phase = res
        if dump and dump[0] == "mix1":
            t = P.dma("sync", (lambda e: e.dma_start(out=dbg.rearrange("p (j t) -> p j t", j=NJ), in_=XSv)), "dbg", deps=res)
            P.wait_only("sync", [t])
            P.build(st)
            return nc
        fo = ffn(1, [(0, 512, 0), (512, 512, 0)], [14] * NE, True, True)
        P.wait_only("sync", fo)
        P.build(st)
    return nc


def _consts(mirror):
    half = 32
    inv = np.power(10000.0, -np.arange(half, dtype=np.float32) / half).astype(np.float32)
    loc = np.arange(1280)
    pos = (SEQ - 1 - loc) if mirror else loc
    rows = (pos // 64).astype(np.float32)
    cols = (pos % 64).astype(np.float32)
    cosT = np.zeros((128, 1280), np.float32)
    sinT = np.zeros((128, 1280), np.float32)
    for p in range(128):
        pp = rows if p < 64 else cols
        ang = pp * inv[p % 32]
        cosT[p] = np.cos(ang)
        sgn = -1.0 if (p % 64) < 32 else 1.0
        sinT[p] = sgn * np.sin(ang)
    ident = np.eye(128, dtype=np.float32)
    perm = np.zeros((128, 128), np.float32)
    for p in range(128):
        q = p + 32 if (p % 64) < 32 else p - 32
        perm[q, p] = 1.0
    jj, ii = np.meshgrid(np.arange(128), np.arange(128), indexing="ij")
    mask1 = (ii <= jj).astype(np.float32)
    mask2 = (jj <= ii).astype(np.float32)
    cmat = np.stack([ident, perm, mask1, mask2], axis=1)
    return cosT, sinT, np.ascontiguousarray(cmat)


def make_in_maps(inp, cores):
    f = lambda a: np.ascontiguousarray(np.asarray(a, dtype=np.float32))
    x, c, ctx, c_ctx = f(inp["x"]), f(inp["c"]), f(inp["ctx"]), f(inp["c_ctx"])
    shared = {
        "w_mod": f(inp["w_mod"]), "w_in": f(inp["w_in"]), "w_o_attn": f(inp["w_o_attn"]), "w_o_conv": f(inp["w_o_conv"]),
        "w_out": f(inp["w_out"]), "ffn_w1": f(inp["ffn_w1"]), "ffn_w3": f(inp["ffn_w3"]), "ffn_w2": f(inp["ffn_w2"]),
        "moe_w1": f(inp["moe_w1"]), "moe_w3": f(inp["moe_w3"]), "moe_w2": f(inp["moe_w2"]),
    }
    tr = lambda v: np.ascontiguousarray(v.reshape(-1, 128).T)
    bmodT = np.ascontiguousarray(np.stack([tr(f(inp["b_mod"])[l]) for l in range(2)], axis=1))
    n1T = np.ascontiguousarray(np.stack([tr(f(inp["norm1"])[l]) for l in range(2)], axis=1))
    n2T = np.ascontiguousarray(np.stack([tr(f(inp["norm2"])[l]) for l in range(2)], axis=1))
    nfT = tr(f(inp["norm_f"]))
    sinkB = np.ascontiguousarray(np.broadcast_to(f(inp["sink"])[None], (128, 2, NH)))
    cw = f(inp["conv_w"])
    routerT = np.ascontiguousarray(f(inp["router"])[0].reshape(NJ, 128, NE).transpose(1, 0, 2))
    maps = []
    for cid in cores:
        b, hf = cid // 2, cid % 2
        mirror = hf == 1
        xs = x[b]
        loc = xs[::-1][:1280] if mirror else xs[:1280]
        xin = np.ascontiguousarray(np.concatenate([loc[:1152], ctx[b], loc[1152:1280]], axis=0))
        cvec = np.ascontiguousarray(np.stack([tr(c[b]), tr(c_ctx)], axis=2))
        cwl = cw[:, ::-1, :] if mirror else cw
        convT = np.ascontiguousarray(np.stack([np.stack([tr(cwl[l, w]) for w in range(3)], axis=1) for l in range(2)], axis=1))
        cosT, sinT, cmat = _consts(mirror)
        m = dict(shared)
        m.update({"xin": xin, "cvec": cvec, "bmodT": bmodT, "n1T": n1T, "n2T": n2T, "nfT": nfT, "sinkB": sinkB,
                  "convT": convT, "routerT": routerT, "cosT": cosT, "sinT": sinT, "cmat": cmat})
        maps.append(m)
    return maps


def kernel(**inputs):
    nc = build_program()
    maps = make_in_maps(inputs, list(range(8)))
    res = run_bass_kernel_spmd(nc, maps, core_ids=list(range(8)))
    outp = np.zeros((4, SEQ, D), np.float32)
    for cid in range(8):
        b, hf = cid // 2, cid % 2
        o = res.results[cid]["out"]
        if hf == 0:
            outp[b, :1024] = o
        else:
            outp[b, 1024:] = o[::-1]
    return outp
```

```python
import types
import numpy as np
from contextlib import ExitStack
import concourse.bass as bass
import concourse.mybir as mybir
from concourse.bass_utils import run_bass_kernel_spmd

F32 = mybir.dt.float32
BF16 = mybir.dt.bfloat16
AF = mybir.ActivationFunctionType
ALU = mybir.AluOpType
ENGS = ["tensor", "vector", "scalar", "gpsimd", "sync"]

D = 2048
NJ = 16
SEQ = 2048
CTX = 256
NH = 16
NKV = 4
HD = 128
PW = 13312
DFF = 5632
NE = 8
DFE = 7168
EPS = 1e-6
OFF_K, OFF_V, OFF_U, OFF_GB, OFF_GC, OFF_GA, OFF_GCV = 2048, 2560, 3072, 5120, 7168, 9216, 11264
XS_T = 1536
SCALE = HD ** -0.5


def _freeze(fn):
    if fn is None or fn.__closure__ is None:
        return fn
    cells = []
    for c in fn.__closure__:
        try:
            cells.append(types.CellType(c.cell_contents))
        except ValueError:
            cells.append(c)
    return types.FunctionType(fn.__code__, fn.__globals__, fn.__name__, fn.__defaults__, tuple(cells))


class Prog:
    def __init__(self, nc):
        self.nc = nc
        self.ops = {e: [] for e in ENGS}
        self.cnt = {e: 0 for e in ENGS}
        self.waited = {}
        self.semkeys = list(ENGS)

    def new_sem(self, key):
        assert key not in self.cnt
        self.cnt[key] = 0
        self.semkeys.append(key)
        return key

    def _waits(self, eng, deps):
        best = {}
        for d in deps:
            if d is None:
                continue
            k, v = d
            if v > best.get(k, 0):
                best[k] = v
        waits = []
        for k, v in best.items():
            if self.waited.get((eng, k), 0) >= v:
                continue
            self.waited[(eng, k)] = v
            waits.append((k, v))
        return waits

    def op(self, eng, fn, deps=(), mark=True):
        waits = self._waits(eng, deps)
        tok = None
        inc = None
        if mark:
            self.cnt[eng] += 1
            tok = (eng, self.cnt[eng])
            inc = (eng, 1)
        self.ops[eng].append((_freeze(fn), waits, inc))
        return tok

    def dma(self, eng, fn, semkey, deps=()):
        waits = self._waits(eng, deps)
        self.cnt[semkey] += 16
        tok = (semkey, self.cnt[semkey])
        self.ops[eng].append((_freeze(fn), waits, (semkey, 16)))
        return tok

    def wait_only(self, eng, deps):
        waits = self._waits(eng, deps)
        if waits:
            self.ops[eng].append((None, waits, None))

    def build(self, st):
        nc = self.nc
        sems = {}
        for k in self.semkeys:
            sems[k] = st.enter_context(nc.semaphore("s_" + str(k)))
        block = st.enter_context(nc.Block())

        def replay(engname):
            def f(eng):
                for fn, waits, inc in self.ops[engname]:
                    for k, v in waits:
                        eng.wait_ge(sems[k], v)
                    if fn is None:
                        continue
                    ins = fn(eng)
                    if inc is not None:
                        ins.then_inc(sems[inc[0]], inc[1])
            return f

        block.tensor(replay("tensor"))
        block.vector(replay("vector"))
        block.scalar(replay("scalar"))
        block.gpsimd(replay("gpsimd"))
        block.sync(replay("sync"))


class Rot:
    def __init__(self, bufs):
        self.bufs = bufs
        self.free = [[] for _ in bufs]
        self.i = -1

    def next(self):
        self.i = (self.i + 1) % len(self.bufs)
        deps = self.free[self.i]
        self.free[self.i] = []
        return self.i, self.bufs[self.i], deps

    def release(self, i, toks):
        self.free[i] = list(self.free[i]) + [t for t in toks if t is not None]


class K:
    pass


def build_program(n_layers=2, dump=None, small=False):
    nc = bass.Bass("TRN2", target_bir_lowering=False)
    P = Prog(nc)
    k = K()
    k.nc, k.P = nc, P

    def din(name, shape, dt=F32):
        return nc.dram_tensor(name, list(shape), dt, kind="ExternalInput").ap()

    xin = din("xin", [XS_T, D])
    cvec = din("cvec", [128, NJ, 2])
    wmod = din("w_mod", [2, D, 6 * D])
    bmodT = din("bmodT", [128, 2, 96])
    n1T = din("n1T", [128, 2, NJ])
    n2T = din("n2T", [128, 2, NJ])
    nfT = din("nfT", [128, NJ])
    w_in = din("w_in", [2, D, PW])
    w_oa = din("w_o_attn", [2, D, D])
    w_oc = din("w_o_conv", [2, D, D])
    w_o = din("w_out", [2, D, D])
    sinkB = din("sinkB", [128, 2, NH])
    convT = din("convT", [128, 2, 3, NJ])
    fw1 = fw3 = fw2 = mw1 = mw3 = mw2 = None
    if small is not True:
        fw1 = din("ffn_w1", [1, D, DFF])
        fw3 = din("ffn_w3", [1, D, DFF])
        fw2 = din("ffn_w2", [1, DFF, D])
    if not small:
        mw1 = din("moe_w1", [1, NE, D, DFE])
        mw3 = din("moe_w3", [1, NE, D, DFE])
        mw2 = din("moe_w2", [1, NE, DFE, D])
    routerT = din("routerT", [128, NJ, NE])
    cosT = din("cosT", [128, 1280])
    sinT = din("sinT", [128, 1280])
    cmat = din("cmat", [128, 4, 128])
    out = nc.dram_tensor("out", [1024, D], F32, kind="ExternalOutput").ap()
    dbg = None
    if dump is not None:
        dbg = nc.dram_tensor("dbg", [128, dump[1]], F32, kind="ExternalOutput").ap()
    XS = nc.dram_tensor("xs_scr", [D, XS_T], F32).ap()
    MS = nc.dram_tensor("ms_scr", [D, 1408], BF16).ap()
    XSv = XS.rearrange("(j p) t -> p j t", p=128)
    MSv = MS.rearrange("(j p) t -> p j t", p=128)

    st = ExitStack()
    with st:
        RAWB = 211000
        raw = st.enter_context(nc.sbuf_tensor("raw", [128, RAWB // 2], BF16))
        cur = [0]

        def carve(nbytes):
            o = cur[0]
            cur[0] += (nbytes + 63) // 64 * 64
            assert cur[0] <= RAWB, cur[0]
            return o

        def view(off, dt, n, pat=None, **kw):
            nb = n * (4 if dt == F32 else 2)
            v = raw[:, off // 2: off // 2 + nb // 2]
            if dt == F32:
                v = v.bitcast(F32)
            if pat:
                v = v.rearrange(pat, **kw)
            return v

        o_c = carve(15 * 1024)
        cc = [o_c]

        def cview(dt, n, pat=None, **kw):
            nb = n * (4 if dt == F32 else 2)
            o = cc[0]
            cc[0] += (nb + 63) // 64 * 64
            assert cc[0] <= o_c + 15 * 1024, cc[0] - o_c
            return view(o, dt, n, pat, **kw)

        ident_f = cview(F32, 128)
        cm_bf = cview(BF16, 512, "p (a b) -> p a b", a=4)
        ident_b, perm_b, mask1_b, mask2_b = cm_bf[:, 0, :], cm_bf[:, 1, :], cm_bf[:, 2, :], cm_bf[:, 3, :]
        ones_b = cview(BF16, 128)
        ones_f = cview(F32, 128)
        zeros_f = cview(F32, 128)
        cos_b = cview(BF16, 1280)
        sin_b = cview(BF16, 1280)
        modT = cview(F32, 2 * 96 * 2, "p (l m c) -> p l m c", l=2, m=96)
        bmod_s = cview(F32, 2 * 96, "p (l m) -> p l m", l=2)
        n1_s = cview(F32, 32, "p (l j) -> p l j", l=2)
        n2_s = cview(F32, 32, "p (l j) -> p l j", l=2)
        nf_s = cview(F32, 16)
        sink_s = cview(F32, 32, "p (l h) -> p l h", l=2)
        conv_s = cview(F32, 96, "p (l w j) -> p l w j", l=2, w=3)
        cv_s = cview(F32, 32, "p (j c) -> p j c", c=2)
        cs_b = cview(BF16, 32, "p (j c) -> p j c", c=2)
        A1 = cview(F32, 64, "p (l j c) -> p l j c", l=2, c=2)
        A2 = cview(F32, 64, "p (l j c) -> p l j c", l=2, c=2)
        eps_t = cview(F32, 1)
        rt_s = cview(F32, NJ * NE, "p (j e) -> p j e", e=NE)
        rtA = cview(F32, NJ * NE, "p (j e) -> p j e", e=NE)
        bias_sb = cview(F32, NE)
        sinkfull = cview(F32, 512)
        rT_all = cview(F32, 8)

        NWS = 3
        o_w = [carve(16384) for _ in range(NWS)]
        o_h = carve(48 * 1024)
        BIGB = 94208
        o_big = carve(BIGB)
        o_tmp = o_big + BIGB - 10240
        tc_ = [o_tmp]

        def tview(dt, n, pat=None, **kw):
            nb = n * (4 if dt == F32 else 2)
            o = tc_[0]
            tc_[0] += (nb + 63) // 64 * 64
            assert tc_[0] <= o_big + BIGB, (tc_[0],)
            return view(o, dt, n, pat, **kw)

        sq_bufs = Rot([tview(BF16, 512) for _ in range(2)])
        tmpf_bufs = Rot([tview(F32, 512) for _ in range(2)])
        rstd_t = tview(F32, 512)
        rt_t = tview(F32, 512)

        wslots = [view(o, BF16, 8192) for o in o_w]
        banks = [st.enter_context(nc.psum_tensor(f"ps{i}", [128, 512], F32)) for i in range(8)]
        PS = Rot([b[:, :] for b in banks[:7]])
        mod_bank = banks[7][:, :]

        for i in range(NWS):
            P.new_sem(f"w{i}")
        for nm in ["c0", "c1", "xt0", "xt1", "st0", "st1", "xo0", "xo1", "xn0", "xn1", "mi0", "mi1", "mo0", "mo1",
                   "big", "fin0", "fin1", "fin2", "fin3", "dbg"]:
            P.new_sem(nm)

        class WS:
            def __init__(s):
                s.req = []
                s.issued = 0
                s.tok = {}
                s.rel = {}
                s.nget = 0

            def plan(s, ap, kind):
                s.req.append((ap, kind))

            def _issue(s, q):
                ap, kind = s.req[q]
                sl = q % NWS
                deps = s.rel.get(q - NWS, [])
                if kind == "col":
                    dst = wslots[sl].rearrange("p (k n) -> p k n", k=16)
                    src = ap.rearrange("(k p) n -> p k n", p=128)
                else:
                    dst = wslots[sl].rearrange("p (c n) -> p c n", c=4)
                    src = ap.rearrange("(c p) n -> p c n", p=128)
                s.tok[q] = P.dma("gpsimd", (lambda e, dst=dst, src=src: e.dma_start(out=dst, in_=src)), f"w{sl}", deps=deps)

            def get(s):
                r = s.nget
                s.nget += 1
                while s.issued < len(s.req) and s.issued <= r + NWS - 1 and (s.issued < NWS or (s.issued - NWS) in s.rel):
                    s._issue(s.issued)
                    s.issued += 1
                assert r in s.tok, (r, s.issued)
                kind = s.req[r][1]
                sl = r % NWS
                if kind == "col":
                    t = wslots[sl].rearrange("p (k n) -> p k n", k=16)
                else:
                    t = wslots[sl].rearrange("p (c n) -> p c n", c=4)
                return r, t, s.tok[r]

            def release(s, r, toks):
                s.rel[r] = [t for t in toks if t is not None]
                while s.issued < len(s.req) and (s.issued - NWS) in s.rel and s.issued <= s.nget + NWS - 1:
                    s._issue(s.issued)
                    s.issued += 1

        ws = WS()

        def plan_mod(l, cgs):
            for cg in cgs:
                ws.plan(wmod[l][:, cg * 512:(cg + 1) * 512], "col")

        plan_mod(0, range(0, 8))

        def plan_mixer(l):
            ws.plan(w_in[l][:, OFF_K:OFF_K + 512], "col")
            ws.plan(w_in[l][:, OFF_V:OFF_V + 512], "col")
            for g in range(4):
                ws.plan(w_in[l][:, g * 512:(g + 1) * 512], "col")
            if l == 0:
                plan_mod(0, range(8, 24))
                plan_mod(1, range(0, 24))
            for jg in range(4):
                ws.plan(w_in[l][:, OFF_GA + jg * 512: OFF_GA + (jg + 1) * 512], "col")
                ws.plan(w_oa[l][:, jg * 512:(jg + 1) * 512], "col")
            for jg in range(4):
                ws.plan(w_in[l][:, OFF_U + jg * 512: OFF_U + (jg + 1) * 512], "col")
                ws.plan(w_in[l][:, OFF_GC + jg * 512: OFF_GC + (jg + 1) * 512], "col")
                ws.plan(w_in[l][:, OFF_GB + jg * 512: OFF_GB + (jg + 1) * 512], "col")
            for jg in range(4):
                ws.plan(w_in[l][:, OFF_GCV + jg * 512: OFF_GCV + (jg + 1) * 512], "col")
                ws.plan(w_oc[l][:, jg * 512:(jg + 1) * 512], "col")
            for jg in range(4):
                ws.plan(w_o[l][:, jg * 512:(jg + 1) * 512], "col")

        def plan_ffn(w1, w3, w2, nfg):
            for fg in range(nfg):
                ws.plan(w1[:, fg * 512:(fg + 1) * 512], "col")
                ws.plan(w3[:, fg * 512:(fg + 1) * 512], "col")
                ws.plan(w2[fg * 512:(fg + 1) * 512, :], "row")

        stages = dump[0] if dump else "all"
        if stages in ("all", "l0", "attn0", "mix0", "k0", "q0", "mix1"):
            plan_mixer(0)
        if stages in ("all", "l0", "mix1"):
            plan_ffn(fw1[0], fw3[0], fw2[0], 11)
            plan_ffn(fw1[0], fw3[0], fw2[0], 11)
        if stages in ("all", "mix1") and n_layers == 2:
            plan_mixer(1)
        if stages in ("all",) and n_layers == 2:
            for e in range(NE):
                plan_ffn(mw1[0, e], mw3[0, e], mw2[0, e], 14)

        tc0 = []
        def cload(dst, src, eng="sync", key="c0"):
            t = P.dma(eng, (lambda e, dst=dst, src=src: e.dma_start(out=dst, in_=src)), key)
            tc0.append(t)
            return t
        cload(ident_f, cmat[:, 0, :])
        cload(cm_bf, cmat, eng="gpsimd", key="c1")
        cload(cos_b, cosT, eng="gpsimd", key="c1")
        cload(sin_b, sinT, eng="gpsimd", key="c1")
        cload(bmod_s, bmodT)
        cload(n1_s, n1T)
        cload(n2_s, n2T)
        cload(nf_s, nfT)
        cload(sink_s, sinkB)
        cload(conv_s, convT)
        cload(cv_s, cvec)
        cload(rt_s, routerT)
        t_ms = [P.op("vector", lambda e: e.memset(ones_b, 1.0)),
                P.op("vector", lambda e: e.memset(ones_f, 1.0)),
                P.op("vector", lambda e: e.memset(zeros_f, 0.0)),
                P.op("vector", lambda e: e.memset(eps_t, EPS))]
        CONST = tc0 + t_ms

        t_cs = P.op("scalar", lambda e: e.activation(out=cs_b, in_=cv_s, func=AF.Silu), deps=CONST)

        MODT = []
        mod_state = {"last": None, "fin": []}

        def mod_task(l, cg):
            r, wt, wtok = ws.get()
            last = None
            for jj in range(4):
                m = cg * 4 + jj
                for kc in range(NJ):
                    last = P.op("tensor", (lambda e, wt=wt, jj=jj, kc=kc, m=m: e.matmul(
                        mod_bank[:, 2 * m:2 * m + 2], lhsT=wt[:, kc, jj * 128:(jj + 1) * 128], rhs=cs_b[:, kc, :],
                        start=(kc == 0), stop=(kc == NJ - 1))),
                        deps=[wtok, t_cs] + mod_state["fin"] + CONST, mark=(kc == NJ - 1 and jj == 3))
            ws.release(r, [last])
            mod_state["last"] = last

        def mod_fin(l, part):
            m0, m1 = (0, 32) if part == 0 else (32, 96)
            tm = P.op("vector", (lambda e: e.tensor_tensor(
                out=modT[:, l, m0:m1], in0=mod_bank[:, 2 * m0:2 * m1].rearrange("p (m c) -> p m c", c=2),
                in1=bmod_s[:, l, m0:m1].unsqueeze(2).to_broadcast([128, m1 - m0, 2]), op=ALU.add)), deps=[mod_state["last"]] + CONST)
            if part == 0:
                ta = P.op("vector", (lambda e: e.scalar_tensor_tensor(
                    out=A1[:, l], in0=modT[:, l, 16:32, :], scalar=1.0,
                    in1=n1_s[:, l].unsqueeze(2).to_broadcast([128, NJ, 2]), op0=ALU.add, op1=ALU.mult)), deps=[tm])
            else:
                ta = P.op("vector", (lambda e: e.scalar_tensor_tensor(
                    out=A2[:, l], in0=modT[:, l, 64:80, :], scalar=1.0,
                    in1=n2_s[:, l].unsqueeze(2).to_broadcast([128, NJ, 2]), op0=ALU.add, op1=ALU.mult)), deps=[tm])
            MODT.extend([tm, ta])
            mod_state["fin"] = mod_state["fin"] + [tm]

        for cg in range(8):
            mod_task(0, cg)
        mod_fin(0, 0)
        mod_queue = []
        for cg in range(8, 24):
            mod_queue.append((lambda cg=cg: mod_task(0, cg)))
        mod_queue.append(lambda: mod_fin(0, 1))
        for cg in range(24):
            mod_queue.append((lambda cg=cg: mod_task(1, cg)))
            if cg == 7:
                mod_queue.append(lambda: mod_fin(1, 0))
        mod_queue.append(lambda: mod_fin(1, 1))

        def modcol(l, s, j, c):
            return modT[:, l, s * 16 + j, c:c + 1]

        big_f = view(o_big, F32, 8192)
        xt_tok = [big_f[:, 0:2048], big_f[:, 2048:4096]]
        xs_stg = [big_f[:, 4096:6144].rearrange("p (j t) -> p j t", j=NJ), big_f[:, 6144:8192].rearrange("p (j t) -> p j t", j=NJ)]
        xt_rot = Rot(xt_tok)
        stg_rot = Rot(xs_stg)
        t_xs = []
        for tb in range(XS_T // 128):
            xi, xb, xdeps = xt_rot.next()
            tl = P.dma("sync", (lambda e, xb=xb, tb=tb: e.dma_start(out=xb, in_=xin[tb * 128:(tb + 1) * 128, :])), f"xt{xi}", deps=xdeps)
            si, sb, sdeps = stg_rot.next()
            evs = []
            lastmm = None
            for q4 in range(4):
                bi, pb, pdeps = PS.next()
                for jj in range(4):
                    j = q4 * 4 + jj
                    lastmm = P.op("tensor", (lambda e, pb=pb, xb=xb, jj=jj, j=j: e.transpose(
                        pb[:, jj * 128:(jj + 1) * 128], xb[:, j * 128:(j + 1) * 128], ident_f)),
                        deps=[tl] + pdeps + CONST, mark=(jj == 3))
                eng = "scalar" if q4 % 2 == 0 else "vector"
                if eng == "scalar":
                    ev = P.op("scalar", (lambda e, pb=pb, sb=sb, q4=q4: e.activation(
                        out=sb[:, q4 * 4:(q4 + 1) * 4, :], in_=pb.rearrange("p (a b) -> p a b", a=4), func=AF.Copy)),
                        deps=[lastmm] + sdeps)
                else:
                    ev = P.op("vector", (lambda e, pb=pb, sb=sb, q4=q4: e.tensor_copy(
                        out=sb[:, q4 * 4:(q4 + 1) * 4, :], in_=pb.rearrange("p (a b) -> p a b", a=4))),
                        deps=[lastmm] + sdeps)
                PS.release(bi, [ev])
                evs.append(ev)
            xt_rot.release(xi, [lastmm])
            td = P.dma("sync", (lambda e, sb=sb, tb=tb: e.dma_start(out=XSv[:, :, tb * 128:(tb + 1) * 128], in_=sb)), f"st{si}", deps=evs)
            stg_rot.release(si, [td])
            t_xs.append(td)
        XS_READY = list(t_xs)

        h_all = view(o_h, BF16, 24576)

        def norm_tile(l, which, xsrc, n, cidx, hdst, deps_x, extra_deps, want_rstd=False):
            A = A1 if which == 1 else A2
            bs = 0 if which == 1 else 3
            bi, pb, pdeps = PS.next()
            last = None
            for j in range(NJ):
                qi, qb, qdeps = sq_bufs.next()
                tsq = P.op("scalar", (lambda e, qb=qb, j=j: e.activation(out=qb[:, :n], in_=xsrc[:, j, :], func=AF.Square)),
                           deps=deps_x + qdeps + CONST)
                last = P.op("tensor", (lambda e, pb=pb, qb=qb, j=j: e.matmul(pb[:, :n], lhsT=ones_b, rhs=qb[:, :n], start=(j == 0), stop=(j == NJ - 1))),
                            deps=[tsq] + (pdeps if j == 0 else []), mark=True)
                sq_bufs.release(qi, [last])
            t1 = P.op("scalar", (lambda e, pb=pb: e.activation(out=rt_t[:, :n], in_=pb[:, :n], func=AF.Sqrt, bias=eps_t, scale=1.0 / D)),
                      deps=[last] + extra_deps)
            PS.release(bi, [t1])
            t2 = P.op("vector", (lambda e: e.reciprocal(out=rstd_t[:, :n], in_=rt_t[:, :n])), deps=[t1])
            toks = []
            for j in range(NJ):
                ti, tbuf, tdeps = tmpf_bufs.next()
                ta = P.op("vector", (lambda e, tbuf=tbuf, j=j: e.scalar_tensor_tensor(
                    out=tbuf[:, :n], in0=xsrc[:, j, :], scalar=A[:, l, j, cidx:cidx + 1], in1=rstd_t[:, :n], op0=ALU.mult, op1=ALU.mult)),
                    deps=[t2] + tdeps + MODT)
                tb_ = P.op("scalar", (lambda e, tbuf=tbuf, j=j: e.activation(
                    out=hdst[:, j, :], in_=tbuf[:, :n], func=AF.Identity, bias=modcol(l, bs, j, cidx), scale=1.0)),
                    deps=[ta] + extra_deps + MODT)
                tmpf_bufs.release(ti, [tb_])
                toks.append(tb_)
            return toks, t2

        def mm_group(pb, n, lhs_list, rhs_list, deps, pdeps):
            last = None
            nk = len(lhs_list)
            for i in range(nk):
                last = P.op("tensor", (lambda e, i=i: e.matmul(pb[:, :n], lhsT=lhs_list[i], rhs=rhs_list[i], start=(i == 0), stop=(i == nk - 1))),
                            deps=(list(deps) + list(pdeps)) if i == 0 else [], mark=(i == nk - 1))
            return last

        def dump_and_finish(src_ap_f32_or_bf16, ncols, deps, is_bf16):
            eng = "gpsimd" if is_bf16 else "sync"
            t = P.dma(eng, (lambda e: e.dma_start(out=dbg[:, :ncols], in_=src_ap_f32_or_bf16)), "dbg", deps=deps)
            P.wait_only(eng, [t])

        def mixer(l):
            TFl = 1152 if l == 0 else 1024
            ctx_full = (l == 0)
            TF = TFl + (CTX if ctx_full else 0)
            TH = TFl + CTX + 128
            o_ctx, o_ext = TFl, TFl + CTX
            if l == 0:
                tiles = [(0, 0, 512, 0, 0), (512, 512, 512, 0, 512), (1024, 1024, 128, 0, 1024),
                         (1152, 1152, 256, 1, None), (1408, 1408, 128, 0, 1152)]
            else:
                tiles = [(0, 0, 512, 0, 0), (512, 512, 512, 0, 512), (1024, 1152, 256, 1, None), (1280, 1024, 128, 0, 1024)]
            full_tiles = [t for t in tiles if t[0] + t[2] <= TF]
            h = h_all[:, :NJ * TH].rearrange("p (j t) -> p j t", j=NJ)
            o_y = o_big
            y = view(o_y, BF16, NJ * 1408)[:, :NJ * TF].rearrange("p (j t) -> p j t", j=NJ)
            o_kv = o_big + 45056
            kT = view(o_kv, BF16, 4 * 1536)[:, :4 * TH].rearrange("p (g t) -> p g t", g=4)
            vt = view(o_kv + 12288, BF16, 12 * 512).rearrange("p (b n) -> p b n", n=512)
            o_scr = o_big + 45056 + 24576
            NBQ = TFl // 128

            xt = [view(o_big, F32, 8192).rearrange("p (j t) -> p j t", j=NJ),
                  view(o_big + 32768, F32, 8192).rearrange("p (j t) -> p j t", j=NJ)]
            xrot = Rot(xt)
            h_tok = {}
            for (doff, xcol, n, isc, pos) in tiles:
                xi, xb, xdeps = xrot.next()
                xb = xb[:, :, :n] if n == 512 else view(o_big + xi * 32768, F32, NJ * n).rearrange("p (j t) -> p j t", j=NJ)
                tl = P.dma("sync", (lambda e, xb=xb, xcol=xcol, n=n: e.dma_start(out=xb, in_=XSv[:, :, xcol:xcol + n])),
                           f"xt{xi}", deps=xdeps + XS_READY + k.prev_phase)
                toks, _ = norm_tile(l, 1, xb, n, isc, h[:, :, doff:doff + n], [tl], k.prev_phase)
                xrot.release(xi, toks)
                h_tok[doff] = toks
            H_READY = [t for v in h_tok.values() for t in v]
            if dump and dump[0] == "h0":
                dump_and_finish(h_all[:, :NJ * TH], NJ * TH, H_READY, True)
                return None

            qraw_r = Rot([view(o_scr + i * 1024, BF16, 512) for i in range(2)])
            t1_r = Rot([view(o_scr + 2048 + i * 2048, F32, 512) for i in range(2)])
            t2_r = Rot([view(o_scr + 6144 + i * 2048, F32, 512) for i in range(2)])

            def rope_evac(pb, bi, n, pos, dst, mmtok):
                import os
                mode = os.environ.get("ROPE_DBG", "")
                if mode == "copy":
                    ev = P.op("scalar", (lambda e: e.activation(out=dst, in_=pb[:, :n], func=AF.Copy)), deps=[mmtok])
                    PS.release(bi, [ev])
                    return ev
                if mode == "nomm":
                    i1, b1, d1 = t1_r.next()
                    tx = P.op("vector", (lambda e: e.tensor_tensor(out=b1[:, :n], in0=pb[:, :n], in1=cos_b[:, pos:pos + n], op=ALU.mult)), deps=[mmtok] + d1 + CONST)
                    PS.release(bi, [tx])
                    tz = P.op("vector", (lambda e: e.tensor_tensor(out=dst, in0=b1[:, :n], in1=b1[:, :n], op=ALU.add)), deps=[tx])
                    t1_r.release(i1, [tz])
                    return tz
                qi, qb, qd = qraw_r.next()
                ta = P.op("scalar", (lambda e: e.activation(out=qb[:, :n], in_=pb[:, :n], func=AF.Copy)), deps=[mmtok] + qd)
                b2, pb2, pd2 = PS.next()
                lw = ones_b if mode == "ones" else perm_b
                if mode == "nope":
                    tm = ta
                    pb2 = pb
                else:
                    tm = P.op("tensor", (lambda e: e.matmul(pb2[:, :n], lhsT=lw, rhs=qb[:, :n], start=True, stop=True)), deps=[ta] + pd2 + CONST)
                qraw_r.release(qi, [tm])
                i1, b1, d1 = t1_r.next()
                tx = P.op("vector", (lambda e: e.tensor_tensor(out=b1[:, :n], in0=pb[:, :n], in1=cos_b[:, pos:pos + n], op=ALU.mult)), deps=[mmtok, ta] + d1 + CONST)
                PS.release(bi, [ta, tx])
                i2, bb2, d2 = t2_r.next()
                ty = P.op("vector", (lambda e: e.tensor_tensor(out=bb2[:, :n], in0=pb2[:, :n], in1=sin_b[:, pos:pos + n], op=ALU.mult)), deps=[tm] + d2)
                PS.release(b2, [ty])
                tz = P.op("vector", (lambda e: e.tensor_tensor(out=dst, in0=b1[:, :n], in1=bb2[:, :n], op=ALU.add)), deps=[tx, ty])
                t1_r.release(i1, [tz])
                t2_r.release(i2, [tz])
                return tz

            r, wt, wtok = ws.get()
            k_tok = []
            lastmm = None
            for g in range(4):
                for (doff, xcol, n, isc, pos) in tiles:
                    bi, pb, pd = PS.next()
                    mm = mm_group(pb, n, [wt[:, kc, g * 128:(g + 1) * 128] for kc in range(NJ)], [h[:, kc, doff:doff + n] for kc in range(NJ)],
                                  [wtok] + H_READY, pd)
                    lastmm = mm
                    if isc:
                        ev = P.op("scalar", (lambda e, pb=pb, g=g, doff=doff, n=n: e.activation(out=kT[:, g, doff:doff + n], in_=pb[:, :n], func=AF.Copy)), deps=[mm])
                        PS.release(bi, [ev])
                    else:
                        ev = rope_evac(pb, bi, n, pos, kT[:, g, doff:doff + n], mm)
                    k_tok.append(ev)
            ws.release(r, [lastmm])
            if dump and dump[0] == "k0":
                dump_and_finish(view(o_kv, BF16, 4 * 1536), 4 * 1536, k_tok, True)
                return None
            r, wt, wtok = ws.get()
            v_tok = []
            for tb in range(TH // 128):
                bi, pb, pd = PS.next()
                mm = mm_group(pb, 512, [h[:, kc, tb * 128:(tb + 1) * 128] for kc in range(NJ)], [wt[:, kc, :] for kc in range(NJ)], [wtok] + H_READY, pd)
                lastmm = mm
                if tb % 2 == 0:
                    ev = P.op("scalar", (lambda e, pb=pb, tb=tb: e.activation(out=vt[:, tb, :], in_=pb, func=AF.Copy)), deps=[mm])
                else:
                    ev = P.op("vector", (lambda e, pb=pb, tb=tb: e.tensor_copy(out=vt[:, tb, :], in_=pb)), deps=[mm])
                PS.release(bi, [ev])
                v_tok.append(ev)
            ws.release(r, [lastmm])
            q_tok = []
            for g4 in range(4):
                r, wt, wtok = ws.get()
                for jj in range(4):
                    hd = g4 * 4 + jj
                    for (doff, xcol, n, isc, pos) in full_tiles:
                        bi, pb, pd = PS.next()
                        mm = mm_group(pb, n, [wt[:, kc, jj * 128:(jj + 1) * 128] for kc in range(NJ)], [h[:, kc, doff:doff + n] for kc in range(NJ)],
                                      [wtok] + H_READY, pd)
                        lastmm = mm
                        if isc:
                            ev = P.op("scalar", (lambda e, pb=pb, hd=hd, doff=doff, n=n: e.activation(out=y[:, hd, doff:doff + n], in_=pb[:, :n], func=AF.Copy)), deps=[mm])
                            PS.release(bi, [ev])
                        else:
                            ev = rope_evac(pb, bi, n, pos, y[:, hd, doff:doff + n], mm)
                        q_tok.append(ev)
                ws.release(r, [lastmm])
            QKV = k_tok + v_tok + q_tok
            if dump and dump[0] == "q0":
                dump_and_finish(view(o_y, BF16, NJ * 1408)[:, :NJ * TF], NJ * TF, QKV, True)
                return None

            et_r = Rot([view(o_scr + 10240 + i * 5120, BF16, 2560).rearrange("p (c n) -> p c n", c=5) for i in range(2)])
            ds_r = Rot([view(o_scr + 20480, F32, 512)])
            rd_r = Rot([view(o_scr + 22528, F32, 512)])
            att_tok = []
            qblocks = [(n, n * 128, False) for n in range(NBQ)]
            if ctx_full:
                qblocks += [(None, o_ctx, True), (None, o_ctx + 128, True)]
            prev_sf = []
            for g in range(4):
                tsf = None
                for hi in range(4):
                    hd = g * 4 + hi
                    tsf = P.op("scalar", (lambda e, hi=hi, hd=hd: e.activation(out=sinkfull[:, hi * 128:(hi + 1) * 128], in_=zeros_f, func=AF.Exp,
                                                                         bias=sink_s[:, l, hd:hd + 1], scale=1.0)), deps=CONST + prev_sf)
                prev_sf = []
                for (nb, qoff, qctx) in qblocks:
                    if mod_queue:
                        mod_queue.pop(0)()
                    chunks = []
                    if not qctx:
                        if nb - 1 >= 0:
                            chunks.append(((nb - 1) * 128, mask1_b))
                        chunks.append((nb * 128, None))
                        chunks.append(((nb + 1) * 128 if nb + 1 < NBQ else o_ext, mask2_b))
                    chunks.append((o_ctx, None))
                    chunks.append((o_ctx + 128, None))
                    ei, et, ed = et_r.next()
                    bn, pnum, pdn = PS.next()
                    bd, pden, pdd = PS.next()
                    etoks = []
                    for ci, (koff, msk) in enumerate(chunks):
                        bs_, ps_, pds = PS.next()
                        mm = P.op("tensor", (lambda e, ps_=ps_, koff=koff, qoff=qoff: e.matmul(
                            ps_, lhsT=kT[:, g, koff:koff + 128], rhs=y[:, g * 4:(g + 1) * 4, qoff:qoff + 128], start=True, stop=True)),
                            deps=QKV + pds + att_tok[-2:], mark=True)
                        te = P.op("scalar", (lambda e, ps_=ps_, et=et, ci=ci: e.activation(out=et[:, ci, :], in_=ps_, func=AF.Exp, scale=SCALE)), deps=[mm] + ed)
                        PS.release(bs_, [te])
                        if msk is not None:
                            te = P.op("vector", (lambda e, et=et, ci=ci, msk=msk: e.tensor_tensor(
                                out=et[:, ci, :].rearrange("p (a b) -> p a b", a=4), in0=et[:, ci, :].rearrange("p (a b) -> p a b", a=4),
                                in1=msk.unsqueeze(1).to_broadcast([128, 4, 128]), op=ALU.mult)), deps=[te] + CONST)
                        etoks.append(te)
                    nc_ = len(chunks)
                    lastn = None
                    for ci, (koff, msk) in enumerate(chunks):
                        lastn = P.op("tensor", (lambda e, ci=ci, koff=koff, et=et, pnum=pnum: e.matmul(
                            pnum, lhsT=vt[:, koff // 128, g * 128:(g + 1) * 128], rhs=et[:, ci, :], start=(ci == 0), stop=(ci == nc_ - 1))),
                            deps=[etoks[ci]] + (pdn if ci == 0 else []), mark=(ci == nc_ - 1))
                    lastd = None
                    for ci in range(nc_):
                        lastd = P.op("tensor", (lambda e, ci=ci, et=et, pden=pden: e.matmul(
                            pden, lhsT=ones_b, rhs=et[:, ci, :], start=(ci == 0), stop=(ci == nc_ - 1))),
                            deps=(pdd if ci == 0 else []), mark=(ci == nc_ - 1))
                    et_r.release(ei, [lastd])
                    di, dsb, dd = ds_r.next()
                    ta = P.op("vector", (lambda e, dsb=dsb, pden=pden: e.tensor_tensor(out=dsb, in0=pden, in1=sinkfull, op=ALU.add)), deps=[lastd, tsf] + dd)
                    PS.release(bd, [ta])
                    ri, rdb, rdd = rd_r.next()
                    tr = P.op("vector", (lambda e, dsb=dsb, rdb=rdb: e.reciprocal(out=rdb, in_=dsb)), deps=[ta] + rdd)
                    ds_r.release(di, [tr])
                    ty = P.op("vector", (lambda e, rdb=rdb, pnum=pnum, qoff=qoff: e.tensor_tensor(
                        out=y[:, g * 4:(g + 1) * 4, qoff:qoff + 128], in0=pnum.rearrange("p (a b) -> p a b", a=4),
                        in1=rdb.rearrange("p (a b) -> p a b", a=4), op=ALU.mult)), deps=[tr, lastn])
                    PS.release(bn, [ty])
                    rd_r.release(ri, [ty])
                    att_tok.append(ty)
                    prev_sf = [ta]
            while mod_queue:
                mod_queue.pop(0)()
            ATT = att_tok
            if dump and dump[0] == "attn0":
                dump_and_finish(view(o_y, BF16, NJ * 1408)[:, :NJ * TF], NJ * TF, ATT, True)
                return None

            ms_r = Rot([view(o_scr, BF16, 4 * 1408)[:, :4 * TF].rearrange("p (a t) -> p a t", a=4)])
            min_r = Rot([view(o_scr + 11264 + i * 1024, BF16, 512) for i in range(2)])
            x1_r = Rot([rstd_t])
            a_done = []
            ms_writes = []
            for jg in range(4):
                r1, wg, wgt = ws.get()
                r2, wa, wat = ws.get()
                mi, msb, msd = ms_r.next()
                evs = []
                lastmm = None
                for jj in range(4):
                    for (doff, xcol, n, isc, pos) in full_tiles:
                        b1, p1, pd1 = PS.next()
                        m1 = mm_group(p1, n, [wg[:, kc, jj * 128:(jj + 1) * 128] for kc in range(NJ)], [h[:, kc, doff:doff + n] for kc in range(NJ)], [wgt] + H_READY, pd1)
                        b2, p2, pd2 = PS.next()
                        m2 = mm_group(p2, n, [wa[:, kc, jj * 128:(jj + 1) * 128] for kc in range(NJ)], [y[:, kc, doff:doff + n] for kc in range(NJ)], [wat] + ATT, pd2)
                        lastmm = m2
                        ti, tb_, td = tmpf_bufs.next()
                        ts = P.op("scalar", (lambda e, p1=p1, tb_=tb_, n=n: e.activation(out=tb_[:, :n], in_=p1[:, :n], func=AF.Sigmoid)), deps=[m1] + td)
                        PS.release(b1, [ts])
                        tv = P.op("vector", (lambda e, p2=p2, tb_=tb_, n=n, msb=msb, jj=jj, doff=doff: e.tensor_tensor(
                            out=msb[:, jj, doff:doff + n], in0=p2[:, :n], in1=tb_[:, :n], op=ALU.mult)), deps=[ts, m2] + msd)
                        PS.release(b2, [tv])
                        tmpf_bufs.release(ti, [tv])
                        evs.append(tv)
                ws.release(r1, [lastmm])
                ws.release(r2, [lastmm])
                a_done.append(lastmm)
                tw = P.dma("sync", (lambda e, msb=msb, jg=jg: e.dma_start(out=MSv[:, jg * 4:(jg + 1) * 4, :TF], in_=msb)), "mo0", deps=evs)
                ms_r.release(mi, [tw])
                ms_writes.append(tw)

            zL = view(o_scr, F32, 1160)
            zC = view(o_scr + 4640, F32, 264)
            a1 = view(o_scr + 5760, F32, 1408)
            tz0 = [P.op("vector", lambda e: e.memset(zL[:, 0:1], 0.0), deps=ms_writes + a_done),
                   P.op("vector", lambda e: e.memset(zC[:, 0:1], 0.0)),
                   P.op("vector", lambda e: e.memset(zC[:, 257:258], 0.0))]
            conv_done = []
            prev_chunk = list(tz0)
            for jg in range(4):
                r1, wu, wut = ws.get()
                r2, wc, wct = ws.get()
                r3, wb, wbt = ws.get()
                lastmm = None
                for jj in range(4):
                    j = jg * 4 + jj
                    ztoks = []
                    for (doff, xcol, n, isc, pos) in tiles:
                        if isc and not ctx_full:
                            continue
                        is_ext = (doff == o_ext)
                        nn = 1 if is_ext else n
                        b1, p1, pd1 = PS.next()
                        m1 = mm_group(p1, nn, [wu[:, kc, jj * 128:(jj + 1) * 128] for kc in range(NJ)], [h[:, kc, doff:doff + nn] for kc in range(NJ)], [wut] + H_READY, pd1)
                        b2, p2, pd2 = PS.next()
                        m2 = mm_group(p2, nn, [wc[:, kc, jj * 128:(jj + 1) * 128] for kc in range(NJ)], [h[:, kc, doff:doff + nn] for kc in range(NJ)], [wct] + H_READY, pd2)
                        lastmm = m2
                        ti, tb_, td = tmpf_bufs.next()
                        ts = P.op("scalar", (lambda e, p1=p1, tb_=tb_, nn=nn: e.activation(out=tb_[:, :nn], in_=p1[:, :nn], func=AF.Copy)), deps=[m1] + td)
                        PS.release(b1, [ts])
                        if isc:
                            zdst = zC[:, 1:257]
                        elif is_ext:
                            zdst = zL[:, 1 + TFl:2 + TFl]
                        else:
                            zdst = zL[:, 1 + doff:1 + doff + n]
                        tv = P.op("vector", (lambda e, p2=p2, tb_=tb_, nn=nn, zdst=zdst: e.tensor_tensor(out=zdst, in0=p2[:, :nn], in1=tb_[:, :nn], op=ALU.mult)),
                                  deps=[ts, m2] + prev_chunk)
                        PS.release(b2, [tv])
                        tmpf_bufs.release(ti, [tv])
                        ztoks.append(tv)
                    segs = [(zL, 0, TFl)] + ([(zC, o_ctx, CTX)] if ctx_full else [])
                    ctoks = []
                    for (zb, aoff, T_) in segs:
                        c0 = P.op("vector", (lambda e, zb=zb, aoff=aoff, T_=T_, j=j: e.tensor_scalar(
                            out=a1[:, aoff:aoff + T_], in0=zb[:, 0:T_], scalar1=conv_s[:, l, 0, j:j + 1], scalar2=None, op0=ALU.mult)), deps=ztoks + prev_chunk)
                        c1 = P.op("vector", (lambda e, zb=zb, aoff=aoff, T_=T_, j=j: e.scalar_tensor_tensor(
                            out=a1[:, aoff:aoff + T_], in0=zb[:, 1:T_ + 1], scalar=conv_s[:, l, 1, j:j + 1], in1=a1[:, aoff:aoff + T_], op0=ALU.mult, op1=ALU.add)), deps=[c0])
                        c2 = P.op("vector", (lambda e, zb=zb, aoff=aoff, T_=T_, j=j: e.scalar_tensor_tensor(
                            out=a1[:, aoff:aoff + T_], in0=zb[:, 2:T_ + 2], scalar=conv_s[:, l, 2, j:j + 1], in1=a1[:, aoff:aoff + T_], op0=ALU.mult, op1=ALU.add)), deps=[c1])
                        ctoks.append(c2)
                    ytoks = []
                    for (doff, xcol, n, isc, pos) in full_tiles:
                        b3, p3, pd3 = PS.next()
                        m3 = mm_group(p3, n, [wb[:, kc, jj * 128:(jj + 1) * 128] for kc in range(NJ)], [h[:, kc, doff:doff + n] for kc in range(NJ)], [wbt] + H_READY, pd3)
                        lastmm = m3
                        ty = P.op("vector", (lambda e, p3=p3, j=j, doff=doff, n=n: e.tensor_tensor(out=y[:, j, doff:doff + n], in0=p3[:, :n], in1=a1[:, doff:doff + n], op=ALU.mult)),
                                  deps=[m3] + ctoks + a_done)
                        PS.release(b3, [ty])
                        ytoks.append(ty)
                    prev_chunk = ytoks
                    conv_done += ytoks
                ws.release(r1, [lastmm])
                ws.release(r2, [lastmm])
                ws.release(r3, [lastmm])

            c_done = []
            ms2_writes = []
            ms_r2 = Rot([view(o_scr, BF16, 4 * 1408)[:, :4 * TF].rearrange("p (a t) -> p a t", a=4)])
            first = True
            for jg in range(4):
                r1, wg, wgt = ws.get()
                r2, wa, wat = ws.get()
                mi, msb, msd = ms_r2.next()
                if first:
                    msd = msd + conv_done
                    first = False
                evs = []
                lastmm = None
                for jj in range(4):
                    j = jg * 4 + jj
                    for (doff, xcol, n, isc, pos) in full_tiles:
                        ii, mib, mid_ = min_r.next()
                        tl = P.dma("sync", (lambda e, mib=mib, j=j, doff=doff, n=n: e.dma_start(out=mib[:, :n], in_=MSv[:, j, doff:doff + n])), f"mi{ii}", deps=mid_ + ms_writes + conv_done[-1:])
                        b1, p1, pd1 = PS.next()
                        m1 = mm_group(p1, n, [wg[:, kc, jj * 128:(jj + 1) * 128] for kc in range(NJ)], [h[:, kc, doff:doff + n] for kc in range(NJ)], [wgt] + H_READY, pd1)
                        b2, p2, pd2 = PS.next()
                        m2 = mm_group(p2, n, [wa[:, kc, jj * 128:(jj + 1) * 128] for kc in range(NJ)], [y[:, kc, doff:doff + n] for kc in range(NJ)], [wat] + conv_done, pd2)
                        lastmm = m2
                        ti, tb_, td = tmpf_bufs.next()
                        ts = P.op("scalar", (lambda e, p1=p1, tb_=tb_, n=n: e.activation(out=tb_[:, :n], in_=p1[:, :n], func=AF.Sigmoid)), deps=[m1] + td)
                        PS.release(b1, [ts])
                        xi_, xb_, xd_ = x1_r.next()
                        tv = P.op("vector", (lambda e, p2=p2, tb_=tb_, xb_=xb_, n=n: e.tensor_tensor(out=xb_[:, :n], in0=p2[:, :n], in1=tb_[:, :n], op=ALU.mult)), deps=[ts, m2] + xd_)
                        PS.release(b2, [tv])
                        tmpf_bufs.release(ti, [tv])
                        tw_ = P.op("vector", (lambda e, xb_=xb_, mib=mib, msb=msb, jj=jj, doff=doff, n=n: e.tensor_tensor(
                            out=msb[:, jj, doff:doff + n], in0=xb_[:, :n], in1=mib[:, :n], op=ALU.add)), deps=[tv, tl] + msd)
                        x1_r.release(xi_, [tw_])
                        min_r.release(ii, [tw_])
                        evs.append(tw_)
                ws.release(r1, [lastmm])
                ws.release(r2, [lastmm])
                c_done.append(lastmm)
                tw = P.dma("sync", (lambda e, msb=msb, jg=jg: e.dma_start(out=MSv[:, jg * 4:(jg + 1) * 4, :TF], in_=msb)), "mo1", deps=evs)
                ms_r2.release(mi, [tw])
                ms2_writes.append(tw)

            Mall = y
            tM = P.dma("sync", (lambda e: e.dma_start(out=Mall, in_=MSv[:, :, :TF])), "big", deps=ms2_writes + c_done)
            xo_r = Rot([view(o_scr + i * 2048, F32, 512) for i in range(2)])
            xn_r = Rot([view(o_scr + 4096 + i * 2048, F32, 512) for i in range(2)])
            o_writes = []
            for jg in range(4):
                r1, wo_, wot = ws.get()
                lastmm = None
                for jj in range(4):
                    j = jg * 4 + jj
                    for (doff, xcol, n, isc, pos) in full_tiles:
                        oi, xob, xod = xo_r.next()
                        tl = P.dma("sync", (lambda e, xob=xob, j=j, xcol=xcol, n=n: e.dma_start(out=xob[:, :n], in_=XSv[:, j, xcol:xcol + n])), f"xo{oi}", deps=xod + XS_READY + k.prev_phase + ms2_writes[-1:])
                        b1, p1, pd1 = PS.next()
                        m1 = mm_group(p1, n, [wo_[:, kc, jj * 128:(jj + 1) * 128] for kc in range(NJ)], [Mall[:, kc, doff:doff + n] for kc in range(NJ)], [wot, tM], pd1)
                        lastmm = m1
                        ni, xnb, xnd = xn_r.next()
                        tv = P.op("vector", (lambda e, p1=p1, xob=xob, xnb=xnb, n=n, j=j, isc=isc: e.scalar_tensor_tensor(
                            out=xnb[:, :n], in0=p1[:, :n], scalar=modcol(l, 2, j, isc), in1=xob[:, :n], op0=ALU.mult, op1=ALU.add)), deps=[m1, tl] + xnd + MODT)
                        PS.release(b1, [tv])
                        xo_r.release(oi, [tv])
                        tw = P.dma("sync", (lambda e, xnb=xnb, j=j, xcol=xcol, n=n: e.dma_start(out=XSv[:, j, xcol:xcol + n], in_=xnb[:, :n])), f"xn{ni}", deps=[tv])
                        xn_r.release(ni, [tw])
                        o_writes.append(tw)
                ws.release(r1, [lastmm])
            return o_writes + [lastmm]

        def barrier(toks):
            for eng in ["tensor", "vector", "scalar", "sync"]:
                P.wait_only(eng, toks)

        def ffn(l, ptiles, w_list, moe, final):
            Tp = sum(t[1] for t in ptiles)
            xr = view(o_big, F32, NJ * 1024)[:, :NJ * Tp].rearrange("p (j t) -> p j t", j=NJ)
            h2 = h_all[:, :NJ * Tp].rearrange("p (j t) -> p j t", j=NJ)
            ag_r = Rot([view(o_big + 65536 + i * 8192, BF16, 4 * 1024)[:, :4 * Tp].rearrange("p (a t) -> p a t", a=4) for i in range(2)])
            combB = h_all[:, 16384:16384 + NE * 1024].rearrange("p (e t) -> p e t", e=NE)
            offs = []
            o = 0
            for (xcol, n, isc) in ptiles:
                offs.append(o)
                o += n
            xl = []
            for ti_, (xcol, n, isc) in enumerate(ptiles):
                tl = P.dma("sync", (lambda e, xcol=xcol, n=n, o_=offs[ti_]: e.dma_start(out=xr[:, :, o_:o_ + n], in_=XSv[:, :, xcol:xcol + n])), f"xt{ti_}", deps=k.prev_phase)
                xl.append(tl)
            h2_tok = []
            rt_prev = []
            for ti_, (xcol, n, isc) in enumerate(ptiles):
                o_ = offs[ti_]
                toks, t2 = norm_tile(l, 2, xr[:, :, o_:o_ + n], n, isc, h2[:, :, o_:o_ + n], [xl[ti_]], k.prev_phase + rt_prev)
                rt_prev = []
                h2_tok += toks
                if moe:
                    for tb in range(n // 128):
                        bi, pb, pd = PS.next()
                        tt = P.op("tensor", (lambda e, pb=pb, tb=tb: e.transpose(pb[:, 0:128], rstd_t[:, tb * 128:(tb + 1) * 128], ident_f)), deps=[t2] + pd + CONST)
                        gtb = (o_ + tb * 128) // 128
                        tc_2 = P.op("vector", (lambda e, pb=pb, gtb=gtb: e.tensor_copy(out=rT_all[:, gtb:gtb + 1], in_=pb[:, 0:1])), deps=[tt])
                        PS.release(bi, [tc_2])
                        h2_tok.append(tc_2)
                        rt_prev.append(tc_2)
            H2 = h2_tok
            import os
            mdbg = os.environ.get("MOE_DBG", "")
            if moe and mdbg == "noroute":
                tcb = P.op("vector", (lambda e: e.memset(combB, 0.5)), deps=H2)
                H2 = H2 + [tcb]
            if moe and mdbg != "noroute":
                NTB = Tp // 128
                t_ra = P.op("vector", (lambda e: e.tensor_tensor(out=rtA, in0=rt_s, in1=A2[:, l, :, 0:1].to_broadcast([128, NJ, NE]), op=ALU.mult)), deps=CONST + MODT)
                bmr = Rot([tmpf_bufs.bufs[0][:, 0:128], tmpf_bufs.bufs[1][:, 0:128]])
                bi, pbias, pd = PS.next()
                lastb = None
                for j in range(NJ):
                    mi_, bm, bmd = bmr.next()
                    tbm = P.op("vector", (lambda e, bm=bm, j=j: e.tensor_scalar(out=bm, in0=ones_f, scalar1=modcol(l, 3, j, 0), scalar2=None, op0=ALU.mult)), deps=bmd + H2 + MODT)
                    lastb = P.op("tensor", (lambda e, bm=bm, j=j: e.matmul(pbias[:, 0:NE], lhsT=bm, rhs=rt_s[:, j, :], start=(j == 0), stop=(j == NJ - 1))), deps=[tbm] + (pd if j == 0 else []))
                    bmr.release(mi_, [lastb])
                tbs = P.op("vector", (lambda e: e.tensor_copy(out=bias_sb, in_=pbias[:, 0:NE])), deps=[lastb])
                PS.release(bi, [tbs])
                bi, plg, pd = PS.next()
                lastl = None
                for tb in range(NTB):
                    for j in range(NJ):
                        lastl = P.op("tensor", (lambda e, tb=tb, j=j: e.matmul(plg[:, tb * NE:(tb + 1) * NE], lhsT=xr[:, j, tb * 128:(tb + 1) * 128], rhs=rtA[:, j, :], start=(j == 0), stop=(j == NJ - 1))),
                                     deps=[t_ra] + xl + (pd if (tb == 0 and j == 0) else []))
                S_ = NTB * NE
                rsc = rt_t
                lg = rsc[:, 0:S_].rearrange("p (b e) -> p b e", e=NE)
                eq1 = rsc[:, 64:64 + S_].rearrange("p (b e) -> p b e", e=NE)
                lg2 = rsc[:, 128:128 + S_].rearrange("p (b e) -> p b e", e=NE)
                eq2 = rsc[:, 192:192 + S_].rearrange("p (b e) -> p b e", e=NE)
                cmb = rsc[:, 256:256 + S_].rearrange("p (b e) -> p b e", e=NE)
                m1_ = rsc[:, 320:320 + NTB]
                m2_ = rsc[:, 328:328 + NTB]
                dd_ = rsc[:, 336:336 + NTB]
                e2_ = rsc[:, 344:344 + NTB]
                p1_ = rsc[:, 352:352 + NTB]
                p2_ = rsc[:, 360:360 + NTB]
                tq = None
                for tb in range(NTB):
                    tq = P.op("vector", (lambda e, tb=tb: e.scalar_tensor_tensor(out=lg[:, tb, :], in0=plg[:, tb * NE:(tb + 1) * NE], scalar=rT_all[:, tb:tb + 1], in1=bias_sb, op0=ALU.mult, op1=ALU.add)),
                              deps=[lastl, tbs] + H2)
                PS.release(bi, [tq])
                bc = lambda v: v.unsqueeze(2).to_broadcast([128, NTB, NE])
                tq = P.op("vector", (lambda e: e.tensor_reduce(out=m1_, in_=lg, axis=mybir.AxisListType.X, op=ALU.max)), deps=[tq])
                tq = P.op("vector", (lambda e: e.tensor_tensor(out=eq1, in0=lg, in1=bc(m1_), op=ALU.is_equal)), deps=[tq])
                tq = P.op("vector", (lambda e: e.scalar_tensor_tensor(out=lg2, in0=eq1, scalar=-1e30, in1=lg, op0=ALU.mult, op1=ALU.add)), deps=[tq])
                tq = P.op("vector", (lambda e: e.tensor_reduce(out=m2_, in_=lg2, axis=mybir.AxisListType.X, op=ALU.max)), deps=[tq])
                tq = P.op("vector", (lambda e: e.tensor_tensor(out=eq2, in0=lg2, in1=bc(m2_), op=ALU.is_equal)), deps=[tq])
                tq = P.op("vector", (lambda e: e.tensor_tensor(out=dd_, in0=m2_, in1=m1_, op=ALU.subtract)), deps=[tq])
                tq = P.op("scalar", (lambda e: e.activation(out=e2_, in_=dd_, func=AF.Exp)), deps=[tq])
                tq = P.op("vector", (lambda e: e.tensor_scalar(out=p2_, in0=e2_, scalar1=1.0, scalar2=None, op0=ALU.add)), deps=[tq])
                tq = P.op("vector", (lambda e: e.reciprocal(out=p1_, in_=p2_)), deps=[tq])
                tq = P.op("vector", (lambda e: e.tensor_tensor(out=p2_, in0=e2_, in1=p1_, op=ALU.mult)), deps=[tq])
                tq = P.op("vector", (lambda e: e.tensor_tensor(out=eq1, in0=eq1, in1=bc(p1_), op=ALU.mult)), deps=[tq])
                tq = P.op("vector", (lambda e: e.tensor_tensor(out=eq2, in0=eq2, in1=bc(p2_), op=ALU.mult)), deps=[tq])
                tq = P.op("vector", (lambda e: e.tensor_tensor(out=cmb, in0=eq1, in1=eq2, op=ALU.add)), deps=[tq])
                dgv = view(o_tmp + 2048, BF16, 1024).rearrange("p (e t) -> p e t", e=NE)
                cb_tok = []
                tprev = [tq]
                for tb in range(NTB):
                    td_ = P.op("vector", (lambda e, tb=tb: e.tensor_tensor(out=dgv, in0=ident_f.unsqueeze(1).to_broadcast([128, NE, 128]),
                                                                          in1=cmb[:, tb, :].unsqueeze(2).to_broadcast([128, NE, 128]), op=ALU.mult)), deps=tprev)
                    mms = []
                    for hf in range(2):
                        bi, pb, pd = PS.next()
                        tm = P.op("tensor", (lambda e, pb=pb, hf=hf: e.matmul(pb, lhsT=ones_b, rhs=dgv[:, hf * 4:(hf + 1) * 4, :], start=True, stop=True)), deps=[td_] + pd)
                        te = P.op("vector", (lambda e, pb=pb, hf=hf, tb=tb: e.tensor_copy(out=combB[:, hf * 4:(hf + 1) * 4, tb * 128:(tb + 1) * 128], in_=pb.rearrange("p (a b) -> p a b", a=4))), deps=[tm])
                        PS.release(bi, [te])
                        mms.append(tm)
                        cb_tok.append(te)
                    tprev = mms
                if mdbg == "r2":
                    cb_tok = [P.op("vector", (lambda e: e.memset(combB, 0.5)), deps=cb_tok)]
                H2 = H2 + cb_tok

            tile_list = [(offs[i], ptiles[i][1], ptiles[i][2]) for i in range(len(ptiles))]
            x_last = {}
            for ei, nfg in enumerate(w_list):
                for fg in range(nfg):
                    r1, w1t, w1k = ws.get()
                    r2, w3t, w3k = ws.get()
                    r3, w2t, w2k = ws.get()
                    gi, ag, agd = ag_r.next()
                    atoks = []
                    lastmm = None
                    for ff in range(4):
                        for (o_, n, isc) in tile_list:
                            b1, p1, pd1 = PS.next()
                            m1 = mm_group(p1, n, [w1t[:, kc, ff * 128:(ff + 1) * 128] for kc in range(NJ)], [h2[:, kc, o_:o_ + n] for kc in range(NJ)], [w1k] + H2, pd1)
                            b3, p3, pd3 = PS.next()
                            m3 = mm_group(p3, n, [w3t[:, kc, ff * 128:(ff + 1) * 128] for kc in range(NJ)], [h2[:, kc, o_:o_ + n] for kc in range(NJ)], [w3k] + H2, pd3)
                            lastmm = m3
                            qi, qb, qd = sq_bufs.next()
                            ts = P.op("scalar", (lambda e, p1=p1, qb=qb, n=n: e.activation(out=qb[:, :n], in_=p1[:, :n], func=AF.Silu)), deps=[m1] + qd)
                            PS.release(b1, [ts])
                            tv = P.op("vector", (lambda e, p3=p3, qb=qb, ag=ag, ff=ff, o_=o_, n=n: e.tensor_tensor(out=ag[:, ff, o_:o_ + n], in0=p3[:, :n], in1=qb[:, :n], op=ALU.mult)), deps=[ts, m3] + agd)
                            PS.release(b3, [tv])
                            sq_bufs.release(qi, [tv])
                            if moe:
                                tv = P.op("vector", (lambda e, ag=ag, ff=ff, o_=o_, n=n, ei=ei: e.tensor_tensor(out=ag[:, ff, o_:o_ + n], in0=ag[:, ff, o_:o_ + n], in1=combB[:, ei, o_:o_ + n], op=ALU.mult)), deps=[tv])
                            atoks.append(tv)
                    ws.release(r1, [lastmm])
                    ws.release(r2, [lastmm])
                    lastd = None
                    for i in range(NJ):
                        for (o_, n, isc) in tile_list:
                            bo, po, pdo = PS.next()
                            lastd = mm_group(po, n, [w2t[:, ff, i * 128:(i + 1) * 128] for ff in range(4)], [ag[:, ff, o_:o_ + n] for ff in range(4)], [w2k] + atoks, pdo)
                            tx = P.op("vector", (lambda e, po=po, i=i, o_=o_, n=n, isc=isc: e.scalar_tensor_tensor(
                                out=xr[:, i, o_:o_ + n], in0=po[:, :n], scalar=modcol(l, 5, i, isc), in1=xr[:, i, o_:o_ + n], op0=ALU.mult, op1=ALU.add)),
                                deps=[lastd] + ([x_last[(i, o_)]] if (i, o_) in x_last else xl + H2) + MODT)
                            PS.release(bo, [tx])
                            x_last[(i, o_)] = tx
                    ws.release(r3, [lastd])
                    ag_r.release(gi, [lastd])
            XDONE = list(x_last.values())
            if not final:
                outs = []
                for ti_, (xcol, n, isc) in enumerate(ptiles):
                    o_ = offs[ti_]
                    tw = P.dma("sync", (lambda e, xcol=xcol, n=n, o_=o_: e.dma_start(out=XSv[:, :, xcol:xcol + n], in_=xr[:, :, o_:o_ + n])), "big", deps=XDONE)
                    outs.append(tw)
                return outs
            ost = [view(o_big + 65536 + i * 8192, F32, 2048) for i in range(2)]
            ost_r = Rot(ost)
            fouts = []
            for (o_, n, isc) in tile_list:
                bi, pb, pdeps = PS.next()
                last = None
                for j in range(NJ):
                    qi, qb, qdeps = sq_bufs.next()
                    tsq = P.op("scalar", (lambda e, qb=qb, j=j, o_=o_, n=n: e.activation(out=qb[:, :n], in_=xr[:, j, o_:o_ + n], func=AF.Square)), deps=XDONE + qdeps)
                    last = P.op("tensor", (lambda e, pb=pb, qb=qb, j=j, n=n: e.matmul(pb[:, :n], lhsT=ones_b, rhs=qb[:, :n], start=(j == 0), stop=(j == NJ - 1))), deps=[tsq] + (pdeps if j == 0 else []))
                    sq_bufs.release(qi, [last])
                t1 = P.op("scalar", (lambda e, pb=pb, n=n: e.activation(out=rt_t[:, :n], in_=pb[:, :n], func=AF.Sqrt, bias=eps_t, scale=1.0 / D)), deps=[last] + fouts[-1:])
                PS.release(bi, [t1])
                t2 = P.op("vector", (lambda e, n=n: e.reciprocal(out=rstd_t[:, :n], in_=rt_t[:, :n])), deps=[t1])
                for tb in range(n // 128):
                    oi, ob, od = ost_r.next()
                    evs = []
                    for q4 in range(4):
                        bi, pb, pd = PS.next()
                        lastt = None
                        for jj in range(4):
                            j = q4 * 4 + jj
                            ti, tbuf, tdeps = tmpf_bufs.next()
                            ta = P.op("vector", (lambda e, tbuf=tbuf, j=j, o_=o_, tb=tb: e.scalar_tensor_tensor(
                                out=tbuf[:, :128], in0=xr[:, j, o_ + tb * 128:o_ + (tb + 1) * 128], scalar=nf_s[:, j:j + 1], in1=rstd_t[:, tb * 128:(tb + 1) * 128], op0=ALU.mult, op1=ALU.mult)),
                                deps=[t2] + tdeps)
                            lastt = P.op("tensor", (lambda e, pb=pb, tbuf=tbuf, jj=jj: e.transpose(pb[:, jj * 128:(jj + 1) * 128], tbuf[:, :128], ident_f)), deps=[ta] + (pd if jj == 0 else []))
                            tmpf_bufs.release(ti, [lastt])
                        if q4 % 2 == 0:
                            ev = P.op("scalar", (lambda e, pb=pb, ob=ob, q4=q4: e.activation(out=ob[:, q4 * 512:(q4 + 1) * 512], in_=pb, func=AF.Copy)), deps=[lastt] + od)
                        else:
                            ev = P.op("vector", (lambda e, pb=pb, ob=ob, q4=q4: e.tensor_copy(out=ob[:, q4 * 512:(q4 + 1) * 512], in_=pb)), deps=[lastt] + od)
                        PS.release(bi, [ev])
                        evs.append(ev)
                    r0 = o_ + tb * 128
                    tw = P.dma("sync", (lambda e, ob=ob, r0=r0: e.dma_start(out=out[r0:r0 + 128, :], in_=ob)), f"fin{oi}", deps=evs)
                    ost_r.release(oi, [tw])
                    fouts.append(tw)
            return fouts

        k.prev_phase = []
        res = mixer(0)
        if res is None:
            P.build(st)
            return nc
        barrier(res)
        k.prev_phase = res
        if dump and dump[0] == "mix0":
            t = P.dma("sync", (lambda e: e.dma_start(out=dbg.rearrange("p (j t) -> p j t", j=NJ), in_=XSv)), "dbg", deps=res)
            P.wait_only("sync", [t])
            P.build(st)
            return nc
        r1_ = ffn(0, [(0, 512, 0), (512, 192, 0)], [11], False, False)
        barrier(r1_)
        k.prev_phase = r1_
        r2_ = ffn(0, [(704, 448, 0), (1152, 256, 1)], [11], False, False)
        barrier(r2_)
        k.prev_phase = r2_
        if dump and dump[0] == "l0":
            t = P.dma("sync", (lambda e: e.dma_start(out=dbg.rearrange("p (j t) -> p j t", j=NJ), in_=XSv)), "dbg", deps=r2_)
            P.wait_only("sync", [t])
            P.build(st)
            return nc
        res = mixer(1)
        barrier(res)
        k.prev_phase = res
        if dump and dump[0] == "mix1":
            t = P.dma("sync", (lambda e: e.dma_start(out=dbg.rearrange("p (j t) -> p j t", j=NJ), in_=XSv)), "dbg", deps=res)
            P.wait_only("sync", [t])
            P.build(st)
            return nc
        fo = ffn(1, [(0, 512, 0), (512, 512, 0)], [14] * NE, True, True)
        P.wait_only("sync", fo)
        P.build(st)
    return nc


def _consts(mirror):
    half = 32
    inv = np.power(10000.0, -np.arange(half, dtype=np.float32) / half).astype(np.float32)
    loc = np.arange(1280)
    pos = (SEQ - 1 - loc) if mirror else loc
    rows = (pos // 64).astype(np.float32)
    cols = (pos % 64).astype(np.float32)
    cosT = np.zeros((128, 1280), np.float32)
    sinT = np.zeros((128, 1280), np.float32)
    for p in range(128):
        pp = rows if p < 64 else cols
        ang = pp * inv[p % 32]
        cosT[p] = np.cos(ang)
        sgn = -1.0 if (p % 64) < 32 else 1.0
        sinT[p] = sgn * np.sin(ang)
    ident = np.eye(128, dtype=np.float32)
    perm = np.zeros((128, 128), np.float32)
    for p in range(128):
        q = p + 32 if (p % 64) < 32 else p - 32
        perm[q, p] = 1.0
    jj, ii = np.meshgrid(np.arange(128), np.arange(128), indexing="ij")
    mask1 = (ii <= jj).astype(np.float32)
    mask2 = (jj <= ii).astype(np.float32)
    cmat = np.stack([ident, perm, mask1, mask2], axis=1)
    return cosT, sinT, np.ascontiguousarray(cmat)


def make_in_maps(inp, cores):
    f = lambda a: np.ascontiguousarray(np.asarray(a, dtype=np.float32))
    x, c, ctx, c_ctx = f(inp["x"]), f(inp["c"]), f(inp["ctx"]), f(inp["c_ctx"])
    shared = {
        "w_mod": f(inp["w_mod"]), "w_in": f(inp["w_in"]), "w_o_attn": f(inp["w_o_attn"]), "w_o_conv": f(inp["w_o_conv"]),
        "w_out": f(inp["w_out"]), "ffn_w1": f(inp["ffn_w1"]), "ffn_w3": f(inp["ffn_w3"]), "ffn_w2": f(inp["ffn_w2"]),
        "moe_w1": f(inp["moe_w1"]), "moe_w3": f(inp["moe_w3"]), "moe_w2": f(inp["moe_w2"]),
    }
    tr = lambda v: np.ascontiguousarray(v.reshape(-1, 128).T)
    bmodT = np.ascontiguousarray(np.stack([tr(f(inp["b_mod"])[l]) for l in range(2)], axis=1))
    n1T = np.ascontiguousarray(np.stack([tr(f(inp["norm1"])[l]) for l in range(2)], axis=1))
    n2T = np.ascontiguousarray(np.stack([tr(f(inp["norm2"])[l]) for l in range(2)], axis=1))
    nfT = tr(f(inp["norm_f"]))
    sinkB = np.ascontiguousarray(np.broadcast_to(f(inp["sink"])[None], (128, 2, NH)))
    cw = f(inp["conv_w"])
    routerT = np.ascontiguousarray(f(inp["router"])[0].reshape(NJ, 128, NE).transpose(1, 0, 2))
    maps = []
    for cid in cores:
        b, hf = cid // 2, cid % 2
        mirror = hf == 1
        xs = x[b]
        loc = xs[::-1][:1280] if mirror else xs[:1280]
        xin = np.ascontiguousarray(np.concatenate([loc[:1152], ctx[b], loc[1152:1280]], axis=0))
        cvec = np.ascontiguousarray(np.stack([tr(c[b]), tr(c_ctx)], axis=2))
        cwl = cw[:, ::-1, :] if mirror else cw
        convT = np.ascontiguousarray(np.stack([np.stack([tr(cwl[l, w]) for w in range(3)], axis=1) for l in range(2)], axis=1))
        cosT, sinT, cmat = _consts(mirror)
        m = dict(shared)
        m.update({"xin": xin, "cvec": cvec, "bmodT": bmodT, "n1T": n1T, "n2T": n2T, "nfT": nfT, "sinkB": sinkB,
                  "convT": convT, "routerT": routerT, "cosT": cosT, "sinT": sinT, "cmat": cmat})
        maps.append(m)
    return maps


def kernel(**inputs):
    nc = build_program()
    maps = make_in_maps(inputs, list(range(8)))
    res = run_bass_kernel_spmd(nc, maps, core_ids=list(range(8)))
    outp = np.zeros((4, SEQ, D), np.float32)
    for cid in range(8):
        b, hf = cid // 2, cid % 2
        o = res.results[cid]["out"]
        if hf == 0:
            outp[b, :1024] = o
        else:
            outp[b, 1024:] = o[::-1]
    return outp
```

```python
import types
import numpy as np
from contextlib import ExitStack
import concourse.bass as bass
import concourse.mybir as mybir
from concourse.bass_utils import run_bass_kernel_spmd

F32 = mybir.dt.float32
BF16 = mybir.dt.bfloat16
AF = mybir.ActivationFunctionType
ALU = mybir.AluOpType
ENGS = ["tensor", "vector", "scalar", "gpsimd", "sync"]

D = 2048
NJ = 16
SEQ = 2048
CTX = 256
NH = 16
NKV = 4
HD = 128
PW = 13312
DFF = 5632
NE = 8
DFE = 7168
EPS = 1e-6
OFF_K, OFF_V, OFF_U, OFF_GB, OFF_GC, OFF_GA, OFF_GCV = 2048, 2560, 3072, 5120, 7168, 9216, 11264
XS_T = 1536
SCALE = HD ** -0.5


def _freeze(fn):
    if fn is None or fn.__closure__ is None:
        return fn
    cells = []
    for c in fn.__closure__:
        try:
            cells.append(types.CellType(c.cell_contents))
        except ValueError:
            cells.append(c)
    return types.FunctionType(fn.__code__, fn.__globals__, fn.__name__, fn.__defaults__, tuple(cells))


class Prog:
    def __init__(self, nc):
        self.nc = nc
        self.ops = {e: [] for e in ENGS}
        self.cnt = {e: 0 for e in ENGS}
        self.waited = {}
        self.semkeys = list(ENGS)

    def new_sem(self, key):
        assert key not in self.cnt
        self.cnt[key] = 0
        self.semkeys.append(key)
        return key

    def _waits(self, eng, deps):
        best = {}
        for d in deps:
            if d is None:
                continue
            k, v = d
            if v > best.get(k, 0):
                best[k] = v
        waits = []
        for k, v in best.items():
            if self.waited.get((eng, k), 0) >= v:
                continue
            self.waited[(eng, k)] = v
            waits.append((k, v))
        return waits

    def op(self, eng, fn, deps=(), mark=True):
        waits = self._waits(eng, deps)
        tok = None
        inc = None
        if mark:
            self.cnt[eng] += 1
            tok = (eng, self.cnt[eng])
            inc = (eng, 1)
        self.ops[eng].append((_freeze(fn), waits, inc))
        return tok

    def dma(self, eng, fn, semkey, deps=()):
        waits = self._waits(eng, deps)
        self.cnt[semkey] += 16
        tok = (semkey, self.cnt[semkey])
        self.ops[eng].append((_freeze(fn), waits, (semkey, 16)))
        return tok

    def wait_only(self, eng, deps):
        waits = self._waits(eng, deps)
        if waits:
            self.ops[eng].append((None, waits, None))

    def build(self, st):
        nc = self.nc
        sems = {}
        for k in self.semkeys:
            sems[k] = st.enter_context(nc.semaphore("s_" + str(k)))
        block = st.enter_context(nc.Block())

        def replay(engname):
            def f(eng):
                for fn, waits, inc in self.ops[engname]:
                    for k, v in waits:
                        eng.wait_ge(sems[k], v)
                    if fn is None:
                        continue
                    ins = fn(eng)
                    if inc is not None:
                        ins.then_inc(sems[inc[0]], inc[1])
            return f

        block.tensor(replay("tensor"))
        block.vector(replay("vector"))
        block.scalar(replay("scalar"))
        block.gpsimd(replay("gpsimd"))
        block.sync(replay("sync"))


class Rot:
    def __init__(self, bufs):
        self.bufs = bufs
        self.free = [[] for _ in bufs]
        self.i = -1

    def next(self):
        self.i = (self.i + 1) % len(self.bufs)
        deps = self.free[self.i]
        self.free[self.i] = []
        return self.i, self.bufs[self.i], deps

    def release(self, i, toks):
        self.free[i] = list(self.free[i]) + [t for t in toks if t is not None]


class K:
    pass


def build_program(n_layers=2, dump=None, small=False):
    nc = bass.Bass("TRN2", target_bir_lowering=False)
    P = Prog(nc)
    k = K()
    k.nc, k.P = nc, P

    def din(name, shape, dt=F32):
        return nc.dram_tensor(name, list(shape), dt, kind="ExternalInput").ap()

    xin = din("xin", [XS_T, D])
    cvec = din("cvec", [128, NJ, 2])
    wmod = din("w_mod", [2, D, 6 * D])
    bmodT = din("bmodT", [128, 2, 96])
    n1T = din("n1T", [128, 2, NJ])
    n2T = din("n2T", [128, 2, NJ])
    nfT = din("nfT", [128, NJ])
    w_in = din("w_in", [2, D, PW])
    w_oa = din("w_o_attn", [2, D, D])
    w_oc = din("w_o_conv", [2, D, D])
    w_o = din("w_out", [2, D, D])
    sinkB = din("sinkB", [128, 2, NH])
    convT = din("convT", [128, 2, 3, NJ])
    fw1 = fw3 = fw2 = mw1 = mw3 = mw2 = None
    if small is not True:
        fw1 = din("ffn_w1", [1, D, DFF])
        fw3 = din("ffn_w3", [1, D, DFF])
        fw2 = din("ffn_w2", [1, DFF, D])
    if not small:
        mw1 = din("moe_w1", [1, NE, D, DFE])
        mw3 = din("moe_w3", [1, NE, D, DFE])
        mw2 = din("moe_w2", [1, NE, DFE, D])
    routerT = din("routerT", [128, NJ, NE])
    cosT = din("cosT", [128, 1280])
    sinT = din("sinT", [128, 1280])
    cmat = din("cmat", [128, 4, 128])
    out = nc.dram_tensor("out", [1024, D], F32, kind="ExternalOutput").ap()
    dbg = None
    if dump is not None:
        dbg = nc.dram_tensor("dbg", [128, dump[1]], F32, kind="ExternalOutput").ap()
    XS = nc.dram_tensor("xs_scr", [D, XS_T], F32).ap()
    MS = nc.dram_tensor("ms_scr", [D, 1408], BF16).ap()
    XSv = XS.rearrange("(j p) t -> p j t", p=128)
    MSv = MS.rearrange("(j p) t -> p j t", p=128)

    st = ExitStack()
    with st:
        RAWB = 211000
        raw = st.enter_context(nc.sbuf_tensor("raw", [128, RAWB // 2], BF16))
        cur = [0]

        def carve(nbytes):
            o = cur[0]
            cur[0] += (nbytes + 63) // 64 * 64
            assert cur[0] <= RAWB, cur[0]
            return o

        def view(off, dt, n, pat=None, **kw):
            nb = n * (4 if dt == F32 else 2)
            v = raw[:, off // 2: off // 2 + nb // 2]
            if dt == F32:
                v = v.bitcast(F32)
            if pat:
                v = v.rearrange(pat, **kw)
            return v

        o_c = carve(15 * 1024)
        cc = [o_c]

        def cview(dt, n, pat=None, **kw):
            nb = n * (4 if dt == F32 else 2)
            o = cc[0]
            cc[0] += (nb + 63) // 64 * 64
            assert cc[0] <= o_c + 15 * 1024, cc[0] - o_c
            return view(o, dt, n, pat, **kw)

        ident_f = cview(F32, 128)
        cm_bf = cview(BF16, 512, "p (a b) -> p a b", a=4)
        ident_b, perm_b, mask1_b, mask2_b = cm_bf[:, 0, :], cm_bf[:, 1, :], cm_bf[:, 2, :], cm_bf[:, 3, :]
        ones_b = cview(BF16, 128)
        ones_f = cview(F32, 128)
        zeros_f = cview(F32, 128)
        cos_b = cview(BF16, 1280)
        sin_b = cview(BF16, 1280)
        modT = cview(F32, 2 * 96 * 2, "p (l m c) -> p l m c", l=2, m=96)
        bmod_s = cview(F32, 2 * 96, "p (l m) -> p l m", l=2)
        n1_s = cview(F32, 32, "p (l j) -> p l j", l=2)
        n2_s = cview(F32, 32, "p (l j) -> p l j", l=2)
        nf_s = cview(F32, 16)
        sink_s = cview(F32, 32, "p (l h) -> p l h", l=2)
        conv_s = cview(F32, 96, "p (l w j) -> p l w j", l=2, w=3)
        cv_s = cview(F32, 32, "p (j c) -> p j c", c=2)
        cs_b = cview(BF16, 32, "p (j c) -> p j c", c=2)
        A1 = cview(F32, 64, "p (l j c) -> p l j c", l=2, c=2)
        A2 = cview(F32, 64, "p (l j c) -> p l j c", l=2, c=2)
        eps_t = cview(F32, 1)
        rt_s = cview(F32, NJ * NE, "p (j e) -> p j e", e=NE)
        rtA = cview(F32, NJ * NE, "p (j e) -> p j e", e=NE)
        bias_sb = cview(F32, NE)
        sinkfull = cview(F32, 512)
        rT_all = cview(F32, 8)

        NWS = 3
        o_w = [carve(16384) for _ in range(NWS)]
        o_h = carve(48 * 1024)
        BIGB = 94208
        o_big = carve(BIGB)
        o_tmp = o_big + BIGB - 10240
        tc_ = [o_tmp]

        def tview(dt, n, pat=None, **kw):
            nb = n * (4 if dt == F32 else 2)
            o = tc_[0]
            tc_[0] += (nb + 63) // 64 * 64
            assert tc_[0] <= o_big + BIGB, (tc_[0],)
            return view(o, dt, n, pat, **kw)

        sq_bufs = Rot([tview(BF16, 512) for _ in range(2)])
        tmpf_bufs = Rot([tview(F32, 512) for _ in range(2)])
        rstd_t = tview(F32, 512)
        rt_t = tview(F32, 512)

        wslots = [view(o, BF16, 8192) for o in o_w]
        banks = [st.enter_context(nc.psum_tensor(f"ps{i}", [128, 512], F32)) for i in range(8)]
        PS = Rot([b[:, :] for b in banks[:7]])
        mod_bank = banks[7][:, :]

        for i in range(NWS):
            P.new_sem(f"w{i}")
        for nm in ["c0", "c1", "xt0", "xt1", "st0", "st1", "xo0", "xo1", "xn0", "xn1", "mi0", "mi1", "mo0", "mo1",
                   "big", "fin0", "fin1", "fin2", "fin3", "dbg"]:
            P.new_sem(nm)

        class WS:
            def __init__(s):
                s.req = []
                s.issued = 0
                s.tok = {}
                s.rel = {}
                s.nget = 0

            def plan(s, ap, kind):
                s.req.append((ap, kind))

            def _issue(s, q):
                ap, kind = s.req[q]
                sl = q % NWS
                deps = s.rel.get(q - NWS, [])
                if kind == "col":
                    dst = wslots[sl].rearrange("p (k n) -> p k n", k=16)
                    src = ap.rearrange("(k p) n -> p k n", p=128)
                else:
                    dst = wslots[sl].rearrange("p (c n) -> p c n", c=4)
                    src = ap.rearrange("(c p) n -> p c n", p=128)
                s.tok[q] = P.dma("gpsimd", (lambda e, dst=dst, src=src: e.dma_start(out=dst, in_=src)), f"w{sl}", deps=deps)

            def get(s):
                r = s.nget
                s.nget += 1
                while s.issued < len(s.req) and s.issued <= r + NWS - 1 and (s.issued < NWS or (s.issued - NWS) in s.rel):
                    s._issue(s.issued)
                    s.issued += 1
                assert r in s.tok, (r, s.issued)
                kind = s.req[r][1]
                sl = r % NWS
                if kind == "col":
                    t = wslots[sl].rearrange("p (k n) -> p k n", k=16)
                else:
                    t = wslots[sl].rearrange("p (c n) -> p c n", c=4)
                return r, t, s.tok[r]

            def release(s, r, toks):
                s.rel[r] = [t for t in toks if t is not None]
                while s.issued < len(s.req) and (s.issued - NWS) in s.rel and s.issued <= s.nget + NWS - 1:
                    s._issue(s.issued)
                    s.issued += 1

        ws = WS()

        def plan_mod(l, cgs):
            for cg in cgs:
                ws.plan(wmod[l][:, cg * 512:(cg + 1) * 512], "col")

        plan_mod(0, range(0, 8))

        def plan_mixer(l):
            ws.plan(w_in[l][:, OFF_K:OFF_K + 512], "col")
            ws.plan(w_in[l][:, OFF_V:OFF_V + 512], "col")
            for g in range(4):
                ws.plan(w_in[l][:, g * 512:(g + 1) * 512], "col")
            if l == 0:
                plan_mod(0, range(8, 24))
                plan_mod(1, range(0, 24))
            for jg in range(4):
                ws.plan(w_in[l][:, OFF_GA + jg * 512: OFF_GA + (jg + 1) * 512], "col")
                ws.plan(w_oa[l][:, jg * 512:(jg + 1) * 512], "col")
            for jg in range(4):
                ws.plan(w_in[l][:, OFF_U + jg * 512: OFF_U + (jg + 1) * 512], "col")
                ws.plan(w_in[l][:, OFF_GC + jg * 512: OFF_GC + (jg + 1) * 512], "col")
                ws.plan(w_in[l][:, OFF_GB + jg * 512: OFF_GB + (jg + 1) * 512], "col")
            for jg in range(4):
                ws.plan(w_in[l][:, OFF_GCV + jg * 512: OFF_GCV + (jg + 1) * 512], "col")
                ws.plan(w_oc[l][:, jg * 512:(jg + 1) * 512], "col")
            for jg in range(4):
                ws.plan(w_o[l][:, jg * 512:(jg + 1) * 512], "col")

        def plan_ffn(w1, w3, w2, nfg):
            for fg in range(nfg):
                ws.plan(w1[:, fg * 512:(fg + 1) * 512], "col")
                ws.plan(w3[:, fg * 512:(fg + 1) * 512], "col")
                ws.plan(w2[fg * 512:(fg + 1) * 512, :], "row")

        stages = dump[0] if dump else "all"
        if stages in ("all", "l0", "attn0", "mix0", "k0", "q0", "mix1"):
            plan_mixer(0)
        if stages in ("all", "l0", "mix1"):
            plan_ffn(fw1[0], fw3[0], fw2[0], 11)
            plan_ffn(fw1[0], fw3[0], fw2[0], 11)
        if stages in ("all", "mix1") and n_layers == 2:
            plan_mixer(1)
        if stages in ("all",) and n_layers == 2:
            for e in range(NE):
                plan_ffn(mw1[0, e], mw3[0, e], mw2[0, e], 14)

        tc0 = []
        def cload(dst, src, eng="sync", key="c0"):
            t = P.dma(eng, (lambda e, dst=dst, src=src: e.dma_start(out=dst, in_=src)), key)
            tc0.append(t)
            return t
        cload(ident_f, cmat[:, 0, :])
        cload(cm_bf, cmat, eng="gpsimd", key="c1")
        cload(cos_b, cosT, eng="gpsimd", key="c1")
        cload(sin_b, sinT, eng="gpsimd", key="c1")
        cload(bmod_s, bmodT)
        cload(n1_s, n1T)
        cload(n2_s, n2T)
        cload(nf_s, nfT)
        cload(sink_s, sinkB)
        cload(conv_s, convT)
        cload(cv_s, cvec)
        cload(rt_s, routerT)
        t_ms = [P.op("vector", lambda e: e.memset(ones_b, 1.0)),
                P.op("vector", lambda e: e.memset(ones_f, 1.0)),
                P.op("vector", lambda e: e.memset(zeros_f, 0.0)),
                P.op("vector", lambda e: e.memset(eps_t, EPS))]
        CONST = tc0 + t_ms

        t_cs = P.op("scalar", lambda e: e.activation(out=cs_b, in_=cv_s, func=AF.Silu), deps=CONST)

        MODT = []
        mod_state = {"last": None, "fin": []}

        def mod_task(l, cg):
            r, wt, wtok = ws.get()
            last = None
            for jj in range(4):
                m = cg * 4 + jj
                for kc in range(NJ):
                    last = P.op("tensor", (lambda e, wt=wt, jj=jj, kc=kc, m=m: e.matmul(
                        mod_bank[:, 2 * m:2 * m + 2], lhsT=wt[:, kc, jj * 128:(jj + 1) * 128], rhs=cs_b[:, kc, :],
                        start=(kc == 0), stop=(kc == NJ - 1))),
                        deps=[wtok, t_cs] + mod_state["fin"] + CONST, mark=(kc == NJ - 1 and jj == 3))
            ws.release(r, [last])
            mod_state["last"] = last

        def mod_fin(l, part):
            m0, m1 = (0, 32) if part == 0 else (32, 96)
            tm = P.op("vector", (lambda e: e.tensor_tensor(
                out=modT[:, l, m0:m1], in0=mod_bank[:, 2 * m0:2 * m1].rearrange("p (m c) -> p m c", c=2),
                in1=bmod_s[:, l, m0:m1].unsqueeze(2).to_broadcast([128, m1 - m0, 2]), op=ALU.add)), deps=[mod_state["last"]] + CONST)
            if part == 0:
                ta = P.op("vector", (lambda e: e.scalar_tensor_tensor(
                    out=A1[:, l], in0=modT[:, l, 16:32, :], scalar=1.0,
                    in1=n1_s[:, l].unsqueeze(2).to_broadcast([128, NJ, 2]), op0=ALU.add, op1=ALU.mult)), deps=[tm])
            else:
                ta = P.op("vector", (lambda e: e.scalar_tensor_tensor(
                    out=A2[:, l], in0=modT[:, l, 64:80, :], scalar=1.0,
                    in1=n2_s[:, l].unsqueeze(2).to_broadcast([128, NJ, 2]), op0=ALU.add, op1=ALU.mult)), deps=[tm])
            MODT.extend([tm, ta])
            mod_state["fin"] = mod_state["fin"] + [tm]

        for cg in range(8):
            mod_task(0, cg)
        mod_fin(0, 0)
        mod_queue = []
        for cg in range(8, 24):
            mod_queue.append((lambda cg=cg: mod_task(0, cg)))
        mod_queue.append(lambda: mod_fin(0, 1))
        for cg in range(24):
            mod_queue.append((lambda cg=cg: mod_task(1, cg)))
            if cg == 7:
                mod_queue.append(lambda: mod_fin(1, 0))
        mod_queue.append(lambda: mod_fin(1, 1))

        def modcol(l, s, j, c):
            return modT[:, l, s * 16 + j, c:c + 1]

        big_f = view(o_big, F32, 8192)
        xt_tok = [big_f[:, 0:2048], big_f[:, 2048:4096]]
        xs_stg = [big_f[:, 4096:6144].rearrange("p (j t) -> p j t", j=NJ), big_f[:, 6144:8192].rearrange("p (j t) -> p j t", j=NJ)]
        xt_rot = Rot(xt_tok)
        stg_rot = Rot(xs_stg)
        t_xs = []
        for tb in range(XS_T // 128):
            xi, xb, xdeps = xt_rot.next()
            tl = P.dma("sync", (lambda e, xb=xb, tb=tb: e.dma_start(out=xb, in_=xin[tb * 128:(tb + 1) * 128, :])), f"xt{xi}", deps=xdeps)
            si, sb, sdeps = stg_rot.next()
            evs = []
            lastmm = None
            for q4 in range(4):
                bi, pb, pdeps = PS.next()
                for jj in range(4):
                    j = q4 * 4 + jj
                    lastmm = P.op("tensor", (lambda e, pb=pb, xb=xb, jj=jj, j=j: e.transpose(
                        pb[:, jj * 128:(jj + 1) * 128], xb[:, j * 128:(j + 1) * 128], ident_f)),
                        deps=[tl] + pdeps + CONST, mark=(jj == 3))
                eng = "scalar" if q4 % 2 == 0 else "vector"
                if eng == "scalar":
                    ev = P.op("scalar", (lambda e, pb=pb, sb=sb, q4=q4: e.activation(
                        out=sb[:, q4 * 4:(q4 + 1) * 4, :], in_=pb.rearrange("p (a b) -> p a b", a=4), func=AF.Copy)),
                        deps=[lastmm] + sdeps)
                else:
                    ev = P.op("vector", (lambda e, pb=pb, sb=sb, q4=q4: e.tensor_copy(
                        out=sb[:, q4 * 4:(q4 + 1) * 4, :], in_=pb.rearrange("p (a b) -> p a b", a=4))),
                        deps=[lastmm] + sdeps)
                PS.release(bi, [ev])
                evs.append(ev)
            xt_rot.release(xi, [lastmm])
            td = P.dma("sync", (lambda e, sb=sb, tb=tb: e.dma_start(out=XSv[:, :, tb * 128:(tb + 1) * 128], in_=sb)), f"st{si}", deps=evs)
            stg_rot.release(si, [td])
            t_xs.append(td)
        XS_READY = list(t_xs)

        h_all = view(o_h, BF16, 24576)

        def norm_tile(l, which, xsrc, n, cidx, hdst, deps_x, extra_deps, want_rstd=False):
            A = A1 if which == 1 else A2
            bs = 0 if which == 1 else 3
            bi, pb, pdeps = PS.next()
            last = None
            for j in range(NJ):
                qi, qb, qdeps = sq_bufs.next()
                tsq = P.op("scalar", (lambda e, qb=qb, j=j: e.activation(out=qb[:, :n], in_=xsrc[:, j, :], func=AF.Square)),
                           deps=deps_x + qdeps + CONST)
                last = P.op("tensor", (lambda e, pb=pb, qb=qb, j=j: e.matmul(pb[:, :n], lhsT=ones_b, rhs=qb[:, :n], start=(j == 0), stop=(j == NJ - 1))),
                            deps=[tsq] + (pdeps if j == 0 else []), mark=True)
                sq_bufs.release(qi, [last])
            t1 = P.op("scalar", (lambda e, pb=pb: e.activation(out=rt_t[:, :n], in_=pb[:, :n], func=AF.Sqrt, bias=eps_t, scale=1.0 / D)),
                      deps=[last] + extra_deps)
            PS.release(bi, [t1])
            t2 = P.op("vector", (lambda e: e.reciprocal(out=rstd_t[:, :n], in_=rt_t[:, :n])), deps=[t1])
            toks = []
            for j in range(NJ):
                ti, tbuf, tdeps = tmpf_bufs.next()
                ta = P.op("vector", (lambda e, tbuf=tbuf, j=j: e.scalar_tensor_tensor(
                    out=tbuf[:, :n], in0=xsrc[:, j, :], scalar=A[:, l, j, cidx:cidx + 1], in1=rstd_t[:, :n], op0=ALU.mult, op1=ALU.mult)),
                    deps=[t2] + tdeps + MODT)
                tb_ = P.op("scalar", (lambda e, tbuf=tbuf, j=j: e.activation(
                    out=hdst[:, j, :], in_=tbuf[:, :n], func=AF.Identity, bias=modcol(l, bs, j, cidx), scale=1.0)),
                    deps=[ta] + extra_deps + MODT)
                tmpf_bufs.release(ti, [tb_])
                toks.append(tb_)
            return toks, t2

        def mm_group(pb, n, lhs_list, rhs_list, deps, pdeps):
            last = None
            nk = len(lhs_list)
            for i in range(nk):
                last = P.op("tensor", (lambda e, i=i: e.matmul(pb[:, :n], lhsT=lhs_list[i], rhs=rhs_list[i], start=(i == 0), stop=(i == nk - 1))),
                            deps=(list(deps) + list(pdeps)) if i == 0 else [], mark=(i == nk - 1))
            return last

        def dump_and_finish(src_ap_f32_or_bf16, ncols, deps, is_bf16):
            eng = "gpsimd" if is_bf16 else "sync"
            t = P.dma(eng, (lambda e: e.dma_start(out=dbg[:, :ncols], in_=src_ap_f32_or_bf16)), "dbg", deps=deps)
            P.wait_only(eng, [t])

        def mixer(l):
            TFl = 1152 if l == 0 else 1024
            ctx_full = (l == 0)
            TF = TFl + (CTX if ctx_full else 0)
            TH = TFl + CTX + 128
            o_ctx, o_ext = TFl, TFl + CTX
            if l == 0:
                tiles = [(0, 0, 512, 0, 0), (512, 512, 512, 0, 512), (1024, 1024, 128, 0, 1024),
                         (1152, 1152, 256, 1, None), (1408, 1408, 128, 0, 1152)]
            else:
                tiles = [(0, 0, 512, 0, 0), (512, 512, 512, 0, 512), (1024, 1152, 256, 1, None), (1280, 1024, 128, 0, 1024)]
            full_tiles = [t for t in tiles if t[0] + t[2] <= TF]
            h = h_all[:, :NJ * TH].rearrange("p (j t) -> p j t", j=NJ)
            o_y = o_big
            y = view(o_y, BF16, NJ * 1408)[:, :NJ * TF].rearrange("p (j t) -> p j t", j=NJ)
            o_kv = o_big + 45056
            kT = view(o_kv, BF16, 4 * 1536)[:, :4 * TH].rearrange("p (g t) -> p g t", g=4)
            vt = view(o_kv + 12288, BF16, 12 * 512).rearrange("p (b n) -> p b n", n=512)
            o_scr = o_big + 45056 + 24576
            NBQ = TFl // 128

            xt = [view(o_big, F32, 8192).rearrange("p (j t) -> p j t", j=NJ),
                  view(o_big + 32768, F32, 8192).rearrange("p (j t) -> p j t", j=NJ)]
            xrot = Rot(xt)
            h_tok = {}
            for (doff, xcol, n, isc, pos) in tiles:
                xi, xb, xdeps = xrot.next()
                xb = xb[:, :, :n] if n == 512 else view(o_big + xi * 32768, F32, NJ * n).rearrange("p (j t) -> p j t", j=NJ)
                tl = P.dma("sync", (lambda e, xb=xb, xcol=xcol, n=n: e.dma_start(out=xb, in_=XSv[:, :, xcol:xcol + n])),
                           f"xt{xi}", deps=xdeps + XS_READY + k.prev_phase)
                toks, _ = norm_tile(l, 1, xb, n, isc, h[:, :, doff:doff + n], [tl], k.prev_phase)
                xrot.release(xi, toks)
                h_tok[doff] = toks
            H_READY = [t for v in h_tok.values() for t in v]
            if dump and dump[0] == "h0":
                dump_and_finish(h_all[:, :NJ * TH], NJ * TH, H_READY, True)
                return None

            qraw_r = Rot([view(o_scr + i * 1024, BF16, 512) for i in range(2)])
            t1_r = Rot([view(o_scr + 2048 + i * 2048, F32, 512) for i in range(2)])
            t2_r = Rot([view(o_scr + 6144 + i * 2048, F32, 512) for i in range(2)])

            def rope_evac(pb, bi, n, pos, dst, mmtok):
                import os
                mode = os.environ.get("ROPE_DBG", "")
                if mode == "copy":
                    ev = P.op("scalar", (lambda e: e.activation(out=dst, in_=pb[:, :n], func=AF.Copy)), deps=[mmtok])
                    PS.release(bi, [ev])
                    return ev
                if mode == "nomm":
                    i1, b1, d1 = t1_r.next()
                    tx = P.op("vector", (lambda e: e.tensor_tensor(out=b1[:, :n], in0=pb[:, :n], in1=cos_b[:, pos:pos + n], op=ALU.mult)), deps=[mmtok] + d1 + CONST)
                    PS.release(bi, [tx])
                    tz = P.op("vector", (lambda e: e.tensor_tensor(out=dst, in0=b1[:, :n], in1=b1[:, :n], op=ALU.add)), deps=[tx])
                    t1_r.release(i1, [tz])
                    return tz
                qi, qb, qd = qraw_r.next()
                ta = P.op("scalar", (lambda e: e.activation(out=qb[:, :n], in_=pb[:, :n], func=AF.Copy)), deps=[mmtok] + qd)
                b2, pb2, pd2 = PS.next()
                lw = ones_b if mode == "ones" else perm_b
                if mode == "nope":
                    tm = ta
                    pb2 = pb
                else:
                    tm = P.op("tensor", (lambda e: e.matmul(pb2[:, :n], lhsT=lw, rhs=qb[:, :n], start=True, stop=True)), deps=[ta] + pd2 + CONST)
                qraw_r.release(qi, [tm])
                i1, b1, d1 = t1_r.next()
                tx = P.op("vector", (lambda e: e.tensor_tensor(out=b1[:, :n], in0=pb[:, :n], in1=cos_b[:, pos:pos + n], op=ALU.mult)), deps=[mmtok, ta] + d1 + CONST)
                PS.release(bi, [ta, tx])
                i2, bb2, d2 = t2_r.next()
                ty = P.op("vector", (lambda e: e.tensor_tensor(out=bb2[:, :n], in0=pb2[:, :n], in1=sin_b[:, pos:pos + n], op=ALU.mult)), deps=[tm] + d2)
                PS.release(b2, [ty])
                tz = P.op("vector", (lambda e: e.tensor_tensor(out=dst, in0=b1[:, :n], in1=bb2[:, :n], op=ALU.add)), deps=[tx, ty])
                t1_r.release(i1, [tz])
                t2_r.release(i2, [tz])
                return tz

            r, wt, wtok = ws.get()
            k_tok = []
            lastmm = None
            for g in range(4):
                for (doff, xcol, n, isc, pos) in tiles:
                    bi, pb, pd = PS.next()
                    mm = mm_group(pb, n, [wt[:, kc, g * 128:(g + 1) * 128] for kc in range(NJ)], [h[:, kc, doff:doff + n] for kc in range(NJ)],
                                  [wtok] + H_READY, pd)
                    lastmm = mm
                    if isc:
                        ev = P.op("scalar", (lambda e, pb=pb, g=g, doff=doff, n=n: e.activation(out=kT[:, g, doff:doff + n], in_=pb[:, :n], func=AF.Copy)), deps=[mm])
                        PS.release(bi, [ev])
                    else:
                        ev = rope_evac(pb, bi, n, pos, kT[:, g, doff:doff + n], mm)
                    k_tok.append(ev)
            ws.release(r, [lastmm])
            if dump and dump[0] == "k0":
                dump_and_finish(view(o_kv, BF16, 4 * 1536), 4 * 1536, k_tok, True)
                return None
            r, wt, wtok = ws.get()
            v_tok = []
            for tb in range(TH // 128):
                bi, pb, pd = PS.next()
                mm = mm_group(pb, 512, [h[:, kc, tb * 128:(tb + 1) * 128] for kc in range(NJ)], [wt[:, kc, :] for kc in range(NJ)], [wtok] + H_READY, pd)
                lastmm = mm
                if tb % 2 == 0:
                    ev = P.op("scalar", (lambda e, pb=pb, tb=tb: e.activation(out=vt[:, tb, :], in_=pb, func=AF.Copy)), deps=[mm])
                else:
                    ev = P.op("vector", (lambda e, pb=pb, tb=tb: e.tensor_copy(out=vt[:, tb, :], in_=pb)), deps=[mm])
                PS.release(bi, [ev])
                v_tok.append(ev)
            ws.release(r, [lastmm])
            q_tok = []
            for g4 in range(4):
                r, wt, wtok = ws.get()
                for jj in range(4):
                    hd = g4 * 4 + jj
                    for (doff, xcol, n, isc, pos) in full_tiles:
                        bi, pb, pd = PS.next()
                        mm = mm_group(pb, n, [wt[:, kc, jj * 128:(jj + 1) * 128] for kc in range(NJ)], [h[:, kc, doff:doff + n] for kc in range(NJ)],
                                      [wtok] + H_READY, pd)
                        lastmm = mm
                        if isc:
                            ev = P.op("scalar", (lambda e, pb=pb, hd=hd, doff=doff, n=n: e.activation(out=y[:, hd, doff:doff + n], in_=pb[:, :n], func=AF.Copy)), deps=[mm])
                            PS.release(bi, [ev])
                        else:
                            ev = rope_evac(pb, bi, n, pos, y[:, hd, doff:doff + n], mm)
                        q_tok.append(ev)
                ws.release(r, [lastmm])
            QKV = k_tok + v_tok + q_tok
            if dump and dump[0] == "q0":
                dump_and_finish(view(o_y, BF16, NJ * 1408)[:, :NJ * TF], NJ * TF, QKV, True)
                return None

            et_r = Rot([view(o_scr + 10240 + i * 5120, BF16, 2560).rearrange("p (c n) -> p c n", c=5) for i in range(2)])
            ds_r = Rot([view(o_scr + 20480, F32, 512)])
            rd_r = Rot([view(o_scr + 22528, F32, 512)])
            att_tok = []
            qblocks = [(n, n * 128, False) for n in range(NBQ)]
            if ctx_full:
                qblocks += [(None, o_ctx, True), (None, o_ctx + 128, True)]
            prev_sf = []
            for g in range(4):
                tsf = None
                for hi in range(4):
                    hd = g * 4 + hi
                    tsf = P.op("scalar", (lambda e, hi=hi, hd=hd: e.activation(out=sinkfull[:, hi * 128:(hi + 1) * 128], in_=zeros_f, func=AF.Exp,
                                                                         bias=sink_s[:, l, hd:hd + 1], scale=1.0)), deps=CONST + prev_sf)
                prev_sf = []
                for (nb, qoff, qctx) in qblocks:
                    if mod_queue:
                        mod_queue.pop(0)()
                    chunks = []
                    if not qctx:
                        if nb - 1 >= 0:
                            chunks.append(((nb - 1) * 128, mask1_b))
                        chunks.append((nb * 128, None))
                        chunks.append(((nb + 1) * 128 if nb + 1 < NBQ else o_ext, mask2_b))
                    chunks.append((o_ctx, None))
                    chunks.append((o_ctx + 128, None))
                    ei, et, ed = et_r.next()
                    bn, pnum, pdn = PS.next()
                    bd, pden, pdd = PS.next()
                    etoks = []
                    for ci, (koff, msk) in enumerate(chunks):
                        bs_, ps_, pds = PS.next()
                        mm = P.op("tensor", (lambda e, ps_=ps_, koff=koff, qoff=qoff: e.matmul(
                            ps_, lhsT=kT[:, g, koff:koff + 128], rhs=y[:, g * 4:(g + 1) * 4, qoff:qoff + 128], start=True, stop=True)),
                            deps=QKV + pds, mark=True)
                        te = P.op("scalar", (lambda e, ps_=ps_, et=et, ci=ci: e.activation(out=et[:, ci, :], in_=ps_, func=AF.Exp, scale=SCALE)), deps=[mm] + ed)
                        PS.release(bs_, [te])
                        if msk is not None:
                            te = P.op("vector", (lambda e, et=et, ci=ci, msk=msk: e.tensor_tensor(
                                out=et[:, ci, :].rearrange("p (a b) -> p a b", a=4), in0=et[:, ci, :].rearrange("p (a b) -> p a b", a=4),
                                in1=msk.unsqueeze(1).to_broadcast([128, 4, 128]), op=ALU.mult)), deps=[te] + CONST)
                        etoks.append(te)
                    nc_ = len(chunks)
                    lastn = None
                    for ci, (koff, msk) in enumerate(chunks):
                        lastn = P.op("tensor", (lambda e, ci=ci, koff=koff, et=et, pnum=pnum: e.matmul(
                            pnum, lhsT=vt[:, koff // 128, g * 128:(g + 1) * 128], rhs=et[:, ci, :], start=(ci == 0), stop=(ci == nc_ - 1))),
                            deps=[etoks[ci]] + (pdn if ci == 0 else []), mark=(ci == nc_ - 1))
                    lastd = None
                    for ci in range(nc_):
                        lastd = P.op("tensor", (lambda e, ci=ci, et=et, pden=pden: e.matmul(
                            pden, lhsT=ones_b, rhs=et[:, ci, :], start=(ci == 0), stop=(ci == nc_ - 1))),
                            deps=(pdd if ci == 0 else []), mark=(ci == nc_ - 1))
                    et_r.release(ei, [lastd])
                    di, dsb, dd = ds_r.next()
                    ta = P.op("vector", (lambda e, dsb=dsb, pden=pden: e.tensor_tensor(out=dsb, in0=pden, in1=sinkfull, op=ALU.add)), deps=[lastd, tsf] + dd)
                    PS.release(bd, [ta])
                    ri, rdb, rdd = rd_r.next()
                    tr = P.op("vector", (lambda e, dsb=dsb, rdb=rdb: e.reciprocal(out=rdb, in_=dsb)), deps=[ta] + rdd)
                    ds_r.release(di, [tr])
                    ty = P.op("vector", (lambda e, rdb=rdb, pnum=pnum, qoff=qoff: e.tensor_tensor(
                        out=y[:, g * 4:(g + 1) * 4, qoff:qoff + 128], in0=pnum.rearrange("p (a b) -> p a b", a=4),
                        in1=rdb.rearrange("p (a b) -> p a b", a=4), op=ALU.mult)), deps=[tr, lastn])
                    PS.release(bn, [ty])
                    rd_r.release(ri, [ty])
                    att_tok.append(ty)
                    prev_sf = [ta]
            while mod_queue:
                mod_queue.pop(0)()
            ATT = att_tok
            if dump and dump[0] == "attn0":
                dump_and_finish(view(o_y, BF16, NJ * 1408)[:, :NJ * TF], NJ * TF, ATT, True)
                return None

            ms_r = Rot([view(o_scr, BF16, 4 * 1408)[:, :4 * TF].rearrange("p (a t) -> p a t", a=4)])
            min_r = Rot([view(o_scr + 11264 + i * 1024, BF16, 512) for i in range(2)])
            x1_r = Rot([rstd_t])
            a_done = []
            ms_writes = []
            for jg in range(4):
                r1, wg, wgt = ws.get()
                r2, wa, wat = ws.get()
                mi, msb, msd = ms_r.next()
                evs = []
                lastmm = None
                for jj in range(4):
                    for (doff, xcol, n, isc, pos) in full_tiles:
                        b1, p1, pd1 = PS.next()
                        m1 = mm_group(p1, n, [wg[:, kc, jj * 128:(jj + 1) * 128] for kc in range(NJ)], [h[:, kc, doff:doff + n] for kc in range(NJ)], [wgt] + H_READY, pd1)
                        b2, p2, pd2 = PS.next()
                        m2 = mm_group(p2, n, [wa[:, kc, jj * 128:(jj + 1) * 128] for kc in range(NJ)], [y[:, kc, doff:doff + n] for kc in range(NJ)], [wat] + ATT, pd2)
                        lastmm = m2
                        ti, tb_, td = tmpf_bufs.next()
                        ts = P.op("scalar", (lambda e, p1=p1, tb_=tb_, n=n: e.activation(out=tb_[:, :n], in_=p1[:, :n], func=AF.Sigmoid)), deps=[m1] + td)
                        PS.release(b1, [ts])
                        tv = P.op("vector", (lambda e, p2=p2, tb_=tb_, n=n, msb=msb, jj=jj, doff=doff: e.tensor_tensor(
                            out=msb[:, jj, doff:doff + n], in0=p2[:, :n], in1=tb_[:, :n], op=ALU.mult)), deps=[ts, m2] + msd)
                        PS.release(b2, [tv])
                        tmpf_bufs.release(ti, [tv])
                        evs.append(tv)
                ws.release(r1, [lastmm])
                ws.release(r2, [lastmm])
                a_done.append(lastmm)
                tw = P.dma("sync", (lambda e, msb=msb, jg=jg: e.dma_start(out=MSv[:, jg * 4:(jg + 1) * 4, :TF], in_=msb)), "mo0", deps=evs)
                ms_r.release(mi, [tw])
                ms_writes.append(tw)

            zL = view(o_scr, F32, 1160)
            zC = view(o_scr + 4640, F32, 264)
            a1 = view(o_scr + 5760, F32, 1408)
            tz0 = [P.op("vector", lambda e: e.memset(zL[:, 0:1], 0.0), deps=ms_writes + a_done),
                   P.op("vector", lambda e: e.memset(zC[:, 0:1], 0.0)),
                   P.op("vector", lambda e: e.memset(zC[:, 257:258], 0.0))]
            conv_done = []
            prev_chunk = list(tz0)
            for jg in range(4):
                r1, wu, wut = ws.get()
                r2, wc, wct = ws.get()
                r3, wb, wbt = ws.get()
                lastmm = None
                for jj in range(4):
                    j = jg * 4 + jj
                    ztoks = []
                    for (doff, xcol, n, isc, pos) in tiles:
                        if isc and not ctx_full:
                            continue
                        is_ext = (doff == o_ext)
                        nn = 1 if is_ext else n
                        b1, p1, pd1 = PS.next()
                        m1 = mm_group(p1, nn, [wu[:, kc, jj * 128:(jj + 1) * 128] for kc in range(NJ)], [h[:, kc, doff:doff + nn] for kc in range(NJ)], [wut] + H_READY, pd1)
                        b2, p2, pd2 = PS.next()
                        m2 = mm_group(p2, nn, [wc[:, kc, jj * 128:(jj + 1) * 128] for kc in range(NJ)], [h[:, kc, doff:doff + nn] for kc in range(NJ)], [wct] + H_READY, pd2)
                        lastmm = m2
                        ti, tb_, td = tmpf_bufs.next()
                        ts = P.op("scalar", (lambda e, p1=p1, tb_=tb_, nn=nn: e.activation(out=tb_[:, :nn], in_=p1[:, :nn], func=AF.Copy)), deps=[m1] + td)
                        PS.release(b1, [ts])
                        if isc:
                            zdst = zC[:, 1:257]
                        elif is_ext:
                            zdst = zL[:, 1 + TFl:2 + TFl]
                        else:
                            zdst = zL[:, 1 + doff:1 + doff + n]
                        tv = P.op("vector", (lambda e, p2=p2, tb_=tb_, nn=nn, zdst=zdst: e.tensor_tensor(out=zdst, in0=p2[:, :nn], in1=tb_[:, :nn], op=ALU.mult)),
                                  deps=[ts, m2] + prev_chunk)
                        PS.release(b2, [tv])
                        tmpf_bufs.release(ti, [tv])
                        ztoks.append(tv)
                    segs = [(zL, 0, TFl)] + ([(zC, o_ctx, CTX)] if ctx_full else [])
                    ctoks = []
                    for (zb, aoff, T_) in segs:
                        c0 = P.op("vector", (lambda e, zb=zb, aoff=aoff, T_=T_, j=j: e.tensor_scalar(
                            out=a1[:, aoff:aoff + T_], in0=zb[:, 0:T_], scalar1=conv_s[:, l, 0, j:j + 1], scalar2=None, op0=ALU.mult)), deps=ztoks + prev_chunk)
                        c1 = P.op("vector", (lambda e, zb=zb, aoff=aoff, T_=T_, j=j: e.scalar_tensor_tensor(
                            out=a1[:, aoff:aoff + T_], in0=zb[:, 1:T_ + 1], scalar=conv_s[:, l, 1, j:j + 1], in1=a1[:, aoff:aoff + T_], op0=ALU.mult, op1=ALU.add)), deps=[c0])
                        c2 = P.op("vector", (lambda e, zb=zb, aoff=aoff, T_=T_, j=j: e.scalar_tensor_tensor(
                            out=a1[:, aoff:aoff + T_], in0=zb[:, 2:T_ + 2], scalar=conv_s[:, l, 2, j:j + 1], in1=a1[:, aoff:aoff + T_], op0=ALU.mult, op1=ALU.add)), deps=[c1])
                        ctoks.append(c2)
                    ytoks = []
                    for (doff, xcol, n, isc, pos) in full_tiles:
                        b3, p3, pd3 = PS.next()
                        m3 = mm_group(p3, n, [wb[:, kc, jj * 128:(jj + 1) * 128] for kc in range(NJ)], [h[:, kc, doff:doff + n] for kc in range(NJ)], [wbt] + H_READY, pd3)
                        lastmm = m3
                        ty = P.op("vector", (lambda e, p3=p3, j=j, doff=doff, n=n: e.tensor_tensor(out=y[:, j, doff:doff + n], in0=p3[:, :n], in1=a1[:, doff:doff + n], op=ALU.mult)),
                                  deps=[m3] + ctoks + a_done)
                        PS.release(b3, [ty])
                        ytoks.append(ty)
                    prev_chunk = ytoks
                    conv_done += ytoks
                ws.release(r1, [lastmm])
                ws.release(r2, [lastmm])
                ws.release(r3, [lastmm])

            c_done = []
            ms2_writes = []
            ms_r2 = Rot([view(o_scr, BF16, 4 * 1408)[:, :4 * TF].rearrange("p (a t) -> p a t", a=4)])
            first = True
            for jg in range(4):
                r1, wg, wgt = ws.get()
                r2, wa, wat = ws.get()
                mi, msb, msd = ms_r2.next()
                if first:
                    msd = msd + conv_done
                    first = False
                evs = []
                lastmm = None
                for jj in range(4):
                    j = jg * 4 + jj
                    for (doff, xcol, n, isc, pos) in full_tiles:
                        ii, mib, mid_ = min_r.next()
                        tl = P.dma("sync", (lambda e, mib=mib, j=j, doff=doff, n=n: e.dma_start(out=mib[:, :n], in_=MSv[:, j, doff:doff + n])), f"mi{ii}", deps=mid_ + ms_writes + conv_done[-1:])
                        b1, p1, pd1 = PS.next()
                        m1 = mm_group(p1, n, [wg[:, kc, jj * 128:(jj + 1) * 128] for kc in range(NJ)], [h[:, kc, doff:doff + n] for kc in range(NJ)], [wgt] + H_READY, pd1)
                        b2, p2, pd2 = PS.next()
                        m2 = mm_group(p2, n, [wa[:, kc, jj * 128:(jj + 1) * 128] for kc in range(NJ)], [y[:, kc, doff:doff + n] for kc in range(NJ)], [wat] + conv_done, pd2)
                        lastmm = m2
                        ti, tb_, td = tmpf_bufs.next()
                        ts = P.op("scalar", (lambda e, p1=p1, tb_=tb_, n=n: e.activation(out=tb_[:, :n], in_=p1[:, :n], func=AF.Sigmoid)), deps=[m1] + td)
                        PS.release(b1, [ts])
                        xi_, xb_, xd_ = x1_r.next()
                        tv = P.op("vector", (lambda e, p2=p2, tb_=tb_, xb_=xb_, n=n: e.tensor_tensor(out=xb_[:, :n], in0=p2[:, :n], in1=tb_[:, :n], op=ALU.mult)), deps=[ts, m2] + xd_)
                        PS.release(b2, [tv])
                        tmpf_bufs.release(ti, [tv])
                        tw_ = P.op("vector", (lambda e, xb_=xb_, mib=mib, msb=msb, jj=jj, doff=doff, n=n: e.tensor_tensor(
                            out=msb[:, jj, doff:doff + n], in0=xb_[:, :n], in1=mib[:, :n], op=ALU.add)), deps=[tv, tl] + msd)
                        x1_r.release(xi_, [tw_])
                        min_r.release(ii, [tw_])
                        evs.append(tw_)
                ws.release(r1, [lastmm])
                ws.release(r2, [lastmm])
                c_done.append(lastmm)
                tw = P.dma("sync", (lambda e, msb=msb, jg=jg: e.dma_start(out=MSv[:, jg * 4:(jg + 1) * 4, :TF], in_=msb)), "mo1", deps=evs)
                ms_r2.release(mi, [tw])
                ms2_writes.append(tw)

            Mall = y
            tM = P.dma("sync", (lambda e: e.dma_start(out=Mall, in_=MSv[:, :, :TF])), "big", deps=ms2_writes + c_done)
            xo_r = Rot([view(o_scr + i * 2048, F32, 512) for i in range(2)])
            xn_r = Rot([view(o_scr + 4096 + i * 2048, F32, 512) for i in range(2)])
            o_writes = []
            for jg in range(4):
                r1, wo_, wot = ws.get()
                lastmm = None
                for jj in range(4):
                    j = jg * 4 + jj
                    for (doff, xcol, n, isc, pos) in full_tiles:
                        oi, xob, xod = xo_r.next()
                        tl = P.dma("sync", (lambda e, xob=xob, j=j, xcol=xcol, n=n: e.dma_start(out=xob[:, :n], in_=XSv[:, j, xcol:xcol + n])), f"xo{oi}", deps=xod + XS_READY + k.prev_phase + ms2_writes[-1:])
                        b1, p1, pd1 = PS.next()
                        m1 = mm_group(p1, n, [wo_[:, kc, jj * 128:(jj + 1) * 128] for kc in range(NJ)], [Mall[:, kc, doff:doff + n] for kc in range(NJ)], [wot, tM], pd1)
                        lastmm = m1
                        ni, xnb, xnd = xn_r.next()
                        tv = P.op("vector", (lambda e, p1=p1, xob=xob, xnb=xnb, n=n, j=j, isc=isc: e.scalar_tensor_tensor(
                            out=xnb[:, :n], in0=p1[:, :n], scalar=modcol(l, 2, j, isc), in1=xob[:, :n], op0=ALU.mult, op1=ALU.add)), deps=[m1, tl] + xnd + MODT)
                        PS.release(b1, [tv])
                        xo_r.release(oi, [tv])
                        tw = P.dma("sync", (lambda e, xnb=xnb, j=j, xcol=xcol, n=n: e.dma_start(out=XSv[:, j, xcol:xcol + n], in_=xnb[:, :n])), f"xn{ni}", deps=[tv])
                        xn_r.release(ni, [tw])
                        o_writes.append(tw)
                ws.release(r1, [lastmm])
            return o_writes + [lastmm]

        def barrier(toks):
            for eng in ["tensor", "vector", "scalar", "sync"]:
                P.wait_only(eng, toks)

        def ffn(l, ptiles, w_list, moe, final):
            Tp = sum(t[1] for t in ptiles)
            xr = view(o_big, F32, NJ * 1024)[:, :NJ * Tp].rearrange("p (j t) -> p j t", j=NJ)
            h2 = h_all[:, :NJ * Tp].rearrange("p (j t) -> p j t", j=NJ)
            ag_r = Rot([view(o_big + 65536 + i * 8192, BF16, 4 * 1024)[:, :4 * Tp].rearrange("p (a t) -> p a t", a=4) for i in range(2)])
            combB = h_all[:, 16384:16384 + NE * 1024].rearrange("p (e t) -> p e t", e=NE)
            offs = []
            o = 0
            for (xcol, n, isc) in ptiles:
                offs.append(o)
                o += n
            xl = []
            for ti_, (xcol, n, isc) in enumerate(ptiles):
                tl = P.dma("sync", (lambda e, xcol=xcol, n=n, o_=offs[ti_]: e.dma_start(out=xr[:, :, o_:o_ + n], in_=XSv[:, :, xcol:xcol + n])), f"xt{ti_}", deps=k.prev_phase)
                xl.append(tl)
            h2_tok = []
            rt_prev = []
            for ti_, (xcol, n, isc) in enumerate(ptiles):
                o_ = offs[ti_]
                toks, t2 = norm_tile(l, 2, xr[:, :, o_:o_ + n], n, isc, h2[:, :, o_:o_ + n], [xl[ti_]], k.prev_phase + rt_prev)
                rt_prev = []
                h2_tok += toks
                if moe:
                    for tb in range(n // 128):
                        bi, pb, pd = PS.next()
                        tt = P.op("tensor", (lambda e, pb=pb, tb=tb: e.transpose(pb[:, 0:128], rstd_t[:, tb * 128:(tb + 1) * 128], ident_f)), deps=[t2] + pd + CONST)
                        gtb = (o_ + tb * 128) // 128
                        tc_2 = P.op("vector", (lambda e, pb=pb, gtb=gtb: e.tensor_copy(out=rT_all[:, gtb:gtb + 1], in_=pb[:, 0:1])), deps=[tt])
                        PS.release(bi, [tc_2])
                        h2_tok.append(tc_2)
                        rt_prev.append(tc_2)
            H2 = h2_tok
            import os
            mdbg = os.environ.get("MOE_DBG", "")
            if moe and mdbg == "noroute":
                tcb = P.op("vector", (lambda e: e.memset(combB, 0.5)), deps=H2)
                H2 = H2 + [tcb]
            if moe and mdbg != "noroute":
                NTB = Tp // 128
                t_ra = P.op("vector", (lambda e: e.tensor_tensor(out=rtA, in0=rt_s, in1=A2[:, l, :, 0:1].to_broadcast([128, NJ, NE]), op=ALU.mult)), deps=CONST + MODT)
                bmr = Rot([tmpf_bufs.bufs[0][:, 0:128], tmpf_bufs.bufs[1][:, 0:128]])
                bi, pbias, pd = PS.next()
                lastb = None
                for j in range(NJ):
                    mi_, bm, bmd = bmr.next()
                    tbm = P.op("vector", (lambda e, bm=bm, j=j: e.tensor_scalar(out=bm, in0=ones_f, scalar1=modcol(l, 3, j, 0), scalar2=None, op0=ALU.mult)), deps=bmd + H2 + MODT)
                    lastb = P.op("tensor", (lambda e, bm=bm, j=j: e.matmul(pbias[:, 0:NE], lhsT=bm, rhs=rt_s[:, j, :], start=(j == 0), stop=(j == NJ - 1))), deps=[tbm] + (pd if j == 0 else []))
                    bmr.release(mi_, [lastb])
                tbs = P.op("vector", (lambda e: e.tensor_copy(out=bias_sb, in_=pbias[:, 0:NE])), deps=[lastb])
                PS.release(bi, [tbs])
                bi, plg, pd = PS.next()
                lastl = None
                for tb in range(NTB):
                    for j in range(NJ):
                        lastl = P.op("tensor", (lambda e, tb=tb, j=j: e.matmul(plg[:, tb * NE:(tb + 1) * NE], lhsT=xr[:, j, tb * 128:(tb + 1) * 128], rhs=rtA[:, j, :], start=(j == 0), stop=(j == NJ - 1))),
                                     deps=[t_ra] + xl + (pd if (tb == 0 and j == 0) else []))
                S_ = NTB * NE
                rsc = rt_t
                lg = rsc[:, 0:S_].rearrange("p (b e) -> p b e", e=NE)
                eq1 = rsc[:, 64:64 + S_].rearrange("p (b e) -> p b e", e=NE)
                lg2 = rsc[:, 128:128 + S_].rearrange("p (b e) -> p b e", e=NE)
                eq2 = rsc[:, 192:192 + S_].rearrange("p (b e) -> p b e", e=NE)
                cmb = rsc[:, 256:256 + S_].rearrange("p (b e) -> p b e", e=NE)
                m1_ = rsc[:, 320:320 + NTB]
                m2_ = rsc[:, 328:328 + NTB]
                dd_ = rsc[:, 336:336 + NTB]
                e2_ = rsc[:, 344:344 + NTB]
                p1_ = rsc[:, 352:352 + NTB]
                p2_ = rsc[:, 360:360 + NTB]
                tq = None
                for tb in range(NTB):
                    tq = P.op("vector", (lambda e, tb=tb: e.scalar_tensor_tensor(out=lg[:, tb, :], in0=plg[:, tb * NE:(tb + 1) * NE], scalar=rT_all[:, tb:tb + 1], in1=bias_sb, op0=ALU.mult, op1=ALU.add)),
                              deps=[lastl, tbs] + H2)
                PS.release(bi, [tq])
                bc = lambda v: v.unsqueeze(2).to_broadcast([128, NTB, NE])
                tq = P.op("vector", (lambda e: e.tensor_reduce(out=m1_, in_=lg, axis=mybir.AxisListType.X, op=ALU.max)), deps=[tq])
                tq = P.op("vector", (lambda e: e.tensor_tensor(out=eq1, in0=lg, in1=bc(m1_), op=ALU.is_equal)), deps=[tq])
                tq = P.op("vector", (lambda e: e.scalar_tensor_tensor(out=lg2, in0=eq1, scalar=-1e30, in1=lg, op0=ALU.mult, op1=ALU.add)), deps=[tq])
                tq = P.op("vector", (lambda e: e.tensor_reduce(out=m2_, in_=lg2, axis=mybir.AxisListType.X, op=ALU.max)), deps=[tq])
                tq = P.op("vector", (lambda e: e.tensor_tensor(out=eq2, in0=lg2, in1=bc(m2_), op=ALU.is_equal)), deps=[tq])
                tq = P.op("vector", (lambda e: e.tensor_tensor(out=dd_, in0=m2_, in1=m1_, op=ALU.subtract)), deps=[tq])
                tq = P.op("scalar", (lambda e: e.activation(out=e2_, in_=dd_, func=AF.Exp)), deps=[tq])
                tq = P.op("vector", (lambda e: e.tensor_scalar(out=p2_, in0=e2_, scalar1=1.0, scalar2=None, op0=ALU.add)), deps=[tq])
                tq = P.op("vector", (lambda e: e.reciprocal(out=p1_, in_=p2_)), deps=[tq])
                tq = P.op("vector", (lambda e: e.tensor_tensor(out=p2_, in0=e2_, in1=p1_, op=ALU.mult)), deps=[tq])
                tq = P.op("vector", (lambda e: e.tensor_tensor(out=eq1, in0=eq1, in1=bc(p1_), op=ALU.mult)), deps=[tq])
                tq = P.op("vector", (lambda e: e.tensor_tensor(out=eq2, in0=eq2, in1=bc(p2_), op=ALU.mult)), deps=[tq])
                tq = P.op("vector", (lambda e: e.tensor_tensor(out=cmb, in0=eq1, in1=eq2, op=ALU.add)), deps=[tq])
                dgv = view(o_tmp + 2048, BF16, 1024).rearrange("p (e t) -> p e t", e=NE)
                cb_tok = []
                tprev = [tq]
                for tb in range(NTB):
                    td_ = P.op("vector", (lambda e, tb=tb: e.tensor_tensor(out=dgv, in0=ident_f.unsqueeze(1).to_broadcast([128, NE, 128]),
                                                                          in1=cmb[:, tb, :].unsqueeze(2).to_broadcast([128, NE, 128]), op=ALU.mult)), deps=tprev)
                    mms = []
                    for hf in range(2):
                        bi, pb, pd = PS.next()
                        tm = P.op("tensor", (lambda e, pb=pb, hf=hf: e.matmul(pb, lhsT=ones_b, rhs=dgv[:, hf * 4:(hf + 1) * 4, :], start=True, stop=True)), deps=[td_] + pd)
                        te = P.op("vector", (lambda e, pb=pb, hf=hf, tb=tb: e.tensor_copy(out=combB[:, hf * 4:(hf + 1) * 4, tb * 128:(tb + 1) * 128], in_=pb.rearrange("p (a b) -> p a b", a=4))), deps=[tm])
                        PS.release(bi, [te])
                        mms.append(tm)
                        cb_tok.append(te)
                    tprev = mms
                if mdbg == "r2":
                    cb_tok = [P.op("vector", (lambda e: e.memset(combB, 0.5)), deps=cb_tok)]
                H2 = H2 + cb_tok

            tile_list = [(offs[i], ptiles[i][1], ptiles[i][2]) for i in range(len(ptiles))]
            x_last = {}
            for ei, nfg in enumerate(w_list):
                for fg in range(nfg):
                    r1, w1t, w1k = ws.get()
                    r2, w3t, w3k = ws.get()
                    r3, w2t, w2k = ws.get()
                    gi, ag, agd = ag_r.next()
                    atoks = []
                    lastmm = None
                    for ff in range(4):
                        for (o_, n, isc) in tile_list:
                            b1, p1, pd1 = PS.next()
                            m1 = mm_group(p1, n, [w1t[:, kc, ff * 128:(ff + 1) * 128] for kc in range(NJ)], [h2[:, kc, o_:o_ + n] for kc in range(NJ)], [w1k] + H2, pd1)
                            b3, p3, pd3 = PS.next()
                            m3 = mm_group(p3, n, [w3t[:, kc, ff * 128:(ff + 1) * 128] for kc in range(NJ)], [h2[:, kc, o_:o_ + n] for kc in range(NJ)], [w3k] + H2, pd3)
                            lastmm = m3
                            qi, qb, qd = sq_bufs.next()
                            ts = P.op("scalar", (lambda e, p1=p1, qb=qb, n=n: e.activation(out=qb[:, :n], in_=p1[:, :n], func=AF.Silu)), deps=[m1] + qd)
                            PS.release(b1, [ts])
                            tv = P.op("vector", (lambda e, p3=p3, qb=qb, ag=ag, ff=ff, o_=o_, n=n: e.tensor_tensor(out=ag[:, ff, o_:o_ + n], in0=p3[:, :n], in1=qb[:, :n], op=ALU.mult)), deps=[ts, m3] + agd)
                            PS.release(b3, [tv])
                            sq_bufs.release(qi, [tv])
                            if moe:
                                tv = P.op("vector", (lambda e, ag=ag, ff=ff, o_=o_, n=n, ei=ei: e.tensor_tensor(out=ag[:, ff, o_:o_ + n], in0=ag[:, ff, o_:o_ + n], in1=combB[:, ei, o_:o_ + n], op=ALU.mult)), deps=[tv])
                            atoks.append(tv)
                    ws.release(r1, [lastmm])
                    ws.release(r2, [lastmm])
                    lastd = None
                    for i in range(NJ):
                        for (o_, n, isc) in tile_list:
                            bo, po, pdo = PS.next()
                            lastd = mm_group(po, n, [w2t[:, ff, i * 128:(i + 1) * 128] for ff in range(4)], [ag[:, ff, o_:o_ + n] for ff in range(4)], [w2k] + atoks, pdo)
                            tx = P.op("vector", (lambda e, po=po, i=i, o_=o_, n=n, isc=isc: e.scalar_tensor_tensor(
                                out=xr[:, i, o_:o_ + n], in0=po[:, :n], scalar=modcol(l, 5, i, isc), in1=xr[:, i, o_:o_ + n], op0=ALU.mult, op1=ALU.add)),
                                deps=[lastd] + ([x_last[(i, o_)]] if (i, o_) in x_last else xl + H2) + MODT)
                            PS.release(bo, [tx])
                            x_last[(i, o_)] = tx
                    ws.release(r3, [lastd])
                    ag_r.release(gi, [lastd])
            XDONE = list(x_last.values())
            if not final:
                outs = []
                for ti_, (xcol, n, isc) in enumerate(ptiles):
                    o_ = offs[ti_]
                    tw = P.dma("sync", (lambda e, xcol=xcol, n=n, o_=o_: e.dma_start(out=XSv[:, :, xcol:xcol + n], in_=xr[:, :, o_:o_ + n])), "big", deps=XDONE)
                    outs.append(tw)
                return outs
            ost = [view(o_big + 65536 + i * 8192, F32, 2048) for i in range(2)]
            ost_r = Rot(ost)
            fouts = []
            for (o_, n, isc) in tile_list:
                bi, pb, pdeps = PS.next()
                last = None
                for j in range(NJ):
                    qi, qb, qdeps = sq_bufs.next()
                    tsq = P.op("scalar", (lambda e, qb=qb, j=j, o_=o_, n=n: e.activation(out=qb[:, :n], in_=xr[:, j, o_:o_ + n], func=AF.Square)), deps=XDONE + qdeps)
                    last = P.op("tensor", (lambda e, pb=pb, qb=qb, j=j, n=n: e.matmul(pb[:, :n], lhsT=ones_b, rhs=qb[:, :n], start=(j == 0), stop=(j == NJ - 1))), deps=[tsq] + (pdeps if j == 0 else []))
                    sq_bufs.release(qi, [last])
                t1 = P.op("scalar", (lambda e, pb=pb, n=n: e.activation(out=rt_t[:, :n], in_=pb[:, :n], func=AF.Sqrt, bias=eps_t, scale=1.0 / D)), deps=[last] + fouts[-1:])
                PS.release(bi, [t1])
                t2 = P.op("vector", (lambda e, n=n: e.reciprocal(out=rstd_t[:, :n], in_=rt_t[:, :n])), deps=[t1])
                for tb in range(n // 128):
                    oi, ob, od = ost_r.next()
                    evs = []
                    for q4 in range(4):
                        bi, pb, pd = PS.next()
                        lastt = None
                        for jj in range(4):
                            j = q4 * 4 + jj
                            ti, tbuf, tdeps = tmpf_bufs.next()
                            ta = P.op("vector", (lambda e, tbuf=tbuf, j=j, o_=o_, tb=tb: e.scalar_tensor_tensor(
                                out=tbuf[:, :128], in0=xr[:, j, o_ + tb * 128:o_ + (tb + 1) * 128], scalar=nf_s[:, j:j + 1], in1=rstd_t[:, tb * 128:(tb + 1) * 128], op0=ALU.mult, op1=ALU.mult)),
                                deps=[t2] + tdeps)
                            lastt = P.op("tensor", (lambda e, pb=pb, tbuf=tbuf, jj=jj: e.transpose(pb[:, jj * 128:(jj + 1) * 128], tbuf[:, :128], ident_f)), deps=[ta] + (pd if jj == 0 else []))
                            tmpf_bufs.release(ti, [lastt])
                        if q4 % 2 == 0:
                            ev = P.op("scalar", (lambda e, pb=pb, ob=ob, q4=q4: e.activation(out=ob[:, q4 * 512:(q4 + 1) * 512], in_=pb, func=AF.Copy)), deps=[lastt] + od)
                        else:
                            ev = P.op("vector", (lambda e, pb=pb, ob=ob, q4=q4: e.tensor_copy(out=ob[:, q4 * 512:(q4 + 1) * 512], in_=pb)), deps=[lastt] + od)
                        PS.release(bi, [ev])
                        evs.append(ev)
                    r0 = o_ + tb * 128
                    tw = P.dma("sync", (lambda e, ob=ob, r0=r0: e.dma_start(out=out[r0:r0 + 128, :], in_=ob)), f"fin{oi}", deps=evs)
                    ost_r.release(oi, [tw])
                    fouts.append(tw)
            return fouts

        k.prev_phase = []
        res = mixer(0)
        if res is None:
            P.build(st)
            return nc
        barrier(res)
        k.prev_phase = res
        if dump and dump[0] == "mix0":
            t = P.dma("sync", (lambda e: e.dma_start(out=dbg.rearrange("p (j t) -> p j t", j=NJ), in_=XSv)), "dbg", deps=res)
            P.wait_only("sync", [t])
            P.build(st)
            return nc
        r1_ = ffn(0, [(0, 512, 0), (512, 192, 0)], [11], False, False)
        barrier(r1_)
        k.prev_phase = r1_
        r2_ = ffn(0, [(704, 448, 0), (1152, 256, 1)], [11], False, False)
        barrier(r2_)
        k.prev_phase = r2_
        if dump and dump[0] == "l0":
            t = P.dma("sync", (lambda e: e.dma_start(out=dbg.rearrange("p (j t) -> p j t", j=NJ), in_=XSv)), "dbg", deps=r2_)
            P.wait_only("sync", [t])
            P.build(st)
            return nc
        res = mixer(1)
        barrier(res)
        k.prev_phase = res
        if dump and dump[0] == "mix1":
            t = P.dma("sync", (lambda e: e.dma_start(out=dbg.rearrange("p (j t) -> p j t", j=NJ), in_=XSv)), "dbg", deps=res)
            P.wait_only("sync", [t])
            P.build(st)
            return nc
        fo = ffn(1, [(0, 512, 0), (512, 512, 0)], [14] * NE, True, True)
        P.wait_only("sync", fo)
        P.build(st)
    return nc


def _consts(mirror):
    half = 32
    inv = np.power(10000.0, -np.arange(half, dtype=np.float32) / half).astype(np.float32)
    loc = np.arange(1280)
    pos = (SEQ - 1 - loc) if mirror else loc
    rows = (pos // 64).astype(np.float32)
    cols = (pos % 64).astype(np.float32)
    cosT = np.zeros((128, 1280), np.float32)
    sinT = np.zeros((128, 1280), np.float32)
    for p in range(128):
        pp = rows if p < 64 else cols
        ang = pp * inv[p % 32]
        cosT[p] = np.cos(ang)
        sgn = -1.0 if (p % 64) < 32 else 1.0
        sinT[p] = sgn * np.sin(ang)
    ident = np.eye(128, dtype=np.float32)
    perm = np.zeros((128, 128), np.float32)
    for p in range(128):
        q = p + 32 if (p % 64) < 32 else p - 32
        perm[q, p] = 1.0
    jj, ii = np.meshgrid(np.arange(128), np.arange(128), indexing="ij")
    mask1 = (ii <= jj).astype(np.float32)
    mask2 = (jj <= ii).astype(np.float32)
    cmat = np.stack([ident, perm, mask1, mask2], axis=1)
    return cosT, sinT, np.ascontiguousarray(cmat)


def make_in_maps(inp, cores):
    f = lambda a: np.ascontiguousarray(np.asarray(a, dtype=np.float32))
    x, c, ctx, c_ctx = f(inp["x"]), f(inp["c"]), f(inp["ctx"]), f(inp["c_ctx"])
    shared = {
        "w_mod": f(inp["w_mod"]), "w_in": f(inp["w_in"]), "w_o_attn": f(inp["w_o_attn"]), "w_o_conv": f(inp["w_o_conv"]),
        "w_out": f(inp["w_out"]), "ffn_w1": f(inp["ffn_w1"]), "ffn_w3": f(inp["ffn_w3"]), "ffn_w2": f(inp["ffn_w2"]),
        "moe_w1": f(inp["moe_w1"]), "moe_w3": f(inp["moe_w3"]), "moe_w2": f(inp["moe_w2"]),
    }
    tr = lambda v: np.ascontiguousarray(v.reshape(-1, 128).T)
    bmodT = np.ascontiguousarray(np.stack([tr(f(inp["b_mod"])[l]) for l in range(2)], axis=1))
    n1T = np.ascontiguousarray(np.stack([tr(f(inp["norm1"])[l]) for l in range(2)], axis=1))
    n2T = np.ascontiguousarray(np.stack([tr(f(inp["norm2"])[l]) for l in range(2)], axis=1))
    nfT = tr(f(inp["norm_f"]))
    sinkB = np.ascontiguousarray(np.broadcast_to(f(inp["sink"])[None], (128, 2, NH)))
    cw = f(inp["conv_w"])
    routerT = np.ascontiguousarray(f(inp["router"])[0].reshape(NJ, 128, NE).transpose(1, 0, 2))
    maps = []
    for cid in cores:
        b, hf = cid // 2, cid % 2
        mirror = hf == 1
        xs = x[b]
        loc = xs[::-1][:1280] if mirror else xs[:1280]
        xin = np.ascontiguousarray(np.concatenate([loc[:1152], ctx[b], loc[1152:1280]], axis=0))
        cvec = np.ascontiguousarray(np.stack([tr(c[b]), tr(c_ctx)], axis=2))
        cwl = cw[:, ::-1, :] if mirror else cw
        convT = np.ascontiguousarray(np.stack([np.stack([tr(cwl[l, w]) for w in range(3)], axis=1) for l in range(2)], axis=1))
        cosT, sinT, cmat = _consts(mirror)
        m = dict(shared)
        m.update({"xin": xin, "cvec": cvec, "bmodT": bmodT, "n1T": n1T, "n2T": n2T, "nfT": nfT, "sinkB": sinkB,
                  "convT": convT, "routerT": routerT, "cosT": cosT, "sinT": sinT, "cmat": cmat})
        maps.append(m)
    return maps


def kernel(**inputs):
    nc = build_program()
    maps = make_in_maps(inputs, list(range(8)))
    res = run_bass_kernel_spmd(nc, maps, core_ids=list(range(8)))
    outp = np.zeros((4, SEQ, D), np.float32)
    for cid in range(8):
        b, hf = cid // 2, cid % 2
        o = res.results[cid]["out"]
        if hf == 0:
            outp[b, :1024] = o
        else:
            outp[b, 1024:] = o[::-1]
    return outp
```
